# Optimizing a Trainium2 kernel written in Bass

```python
import jax, jax.numpy as jnp
from jax import lax
import numpy as np

D_MODEL = 1024
BATCH = 16
SEQ = 4096
DEPTH = 1

CHUNK = 64

D_POOL = D_MODEL // 2
POOL_WINDOWS = (2, 4, 8, 16)
POOL_GROUPS = len(POOL_WINDOWS)
POOL_GROUP_DIM = D_POOL // POOL_GROUPS

D_CONV = D_MODEL // 2
CONV_WIDTH = 3

D_IN = D_POOL + 3 * D_CONV + 2 * D_MODEL

N_KEYS = 128
N_EXPERTS = N_KEYS * N_KEYS
PEER_HEADS = 8
PEER_QUERY_DIM = 256
PEER_HALF = PEER_QUERY_DIM // 2
PEER_TOPK = 16
PEER_TOKEN_BLOCK = 128

RMS_EPS = 1e-6

kernel_name = "hybrid_pool_shortconv_peer_block"


def rmsnorm(x, g):
    xf = x.astype(jnp.float32)
    y = xf * lax.rsqrt(jnp.mean(xf * xf, axis=-1, keepdims=True) + RMS_EPS)
    return (y * g.astype(jnp.float32)).astype(x.dtype)


def pool_mixer(u, group_w, scale):
    S_ = u.shape[1]
    uf = u.astype(jnp.float32)
    csum = jnp.cumsum(uf, axis=1)
    pos = jnp.arange(S_)
    outs = []
    for g, w in enumerate(POOL_WINDOWS):
        sl = slice(g * POOL_GROUP_DIM, (g + 1) * POOL_GROUP_DIM)
        c = csum[:, :, sl]
        c_shift = jnp.pad(c[:, :S_ - w, :], ((0, 0), (w, 0), (0, 0)))
        cnt = jnp.minimum(pos + 1, w).astype(jnp.float32)[None, :, None]
        outs.append((c - c_shift) / cnt - uf[:, :, sl])
    p = jnp.stack(outs, axis=2).astype(u.dtype)
    p = jnp.einsum('bsgc,gcd->bsgd', p, group_w)
    return p.reshape(u.shape) * scale


def short_conv_mixer(c_gate, b_gate, v, conv_w):
    S_ = v.shape[1]
    u = c_gate * v
    up = jnp.pad(u, ((0, 0), (CONV_WIDTH - 1, 0), (0, 0)))
    y = up[:, 0:S_, :] * conv_w[0]
    for k in range(1, CONV_WIDTH):
        y = y + up[:, k:k + S_, :] * conv_w[k]
    return b_gate * y


def peer_ffn(h, w_q, sub_keys, expert_u, expert_v):
    B_, S_, D_ = h.shape
    hb_all = h.reshape(B_ * S_ // PEER_TOKEN_BLOCK, PEER_TOKEN_BLOCK, D_)

    def block(hb):
        q = (hb @ w_q).reshape(PEER_TOKEN_BLOCK, PEER_HEADS, 2, PEER_HALF)
        s = jnp.einsum('thpc,hpkc->thpk', q, sub_keys).astype(jnp.float32)
        top_s, top_i = lax.top_k(s, PEER_TOPK)
        cand_s = top_s[:, :, 0, :, None] + top_s[:, :, 1, None, :]
        cand_i = top_i[:, :, 0, :, None] * N_KEYS + top_i[:, :, 1, None, :]
        cand_s = cand_s.reshape(PEER_TOKEN_BLOCK, PEER_HEADS, PEER_TOPK * PEER_TOPK)
        cand_i = cand_i.reshape(PEER_TOKEN_BLOCK, PEER_HEADS, PEER_TOPK * PEER_TOPK)
        fin_s, fin_pos = lax.top_k(cand_s, PEER_TOPK)
        idx = jnp.take_along_axis(cand_i, fin_pos, axis=-1)
        gates = jax.nn.softmax(fin_s, axis=-1)
        u = expert_u[idx]
        act = jax.nn.gelu(jnp.einsum('thkd,td->thk', u, hb).astype(jnp.float32), approximate=False)
        wts = (gates * act).astype(hb.dtype)
        return jnp.einsum('thk,thkd->td', wts, expert_v[idx])

    out = lax.map(block, hb_all)
    return out.reshape(B_, S_, D_)


def setup_inputs(seed: int = 0) -> dict:
    key = jax.random.key(seed)
    ks = jax.random.split(key, 16)
    f32 = jnp.float32
    nrm = lambda k, shape, s: jax.random.normal(k, shape, f32) * s
    L = DEPTH
    return {
        "x": nrm(ks[0], (BATCH, SEQ, D_MODEL), 1.0),
        "g_mix": 1.0 + nrm(ks[1], (L, D_MODEL), 0.02),
        "w_in": nrm(ks[2], (L, D_MODEL, D_IN), D_MODEL ** -0.5),
        "pool_group_w": nrm(ks[3], (L, POOL_GROUPS, POOL_GROUP_DIM, POOL_GROUP_DIM), POOL_GROUP_DIM ** -0.5),
        "pool_scale": 1.0 + nrm(ks[4], (L, D_POOL), 0.02),
        "conv_w": nrm(ks[5], (L, CONV_WIDTH, D_CONV), CONV_WIDTH ** -0.5),
        "w_branch_pool": nrm(ks[6], (L, D_POOL, D_MODEL), D_POOL ** -0.5),
        "w_branch_conv": nrm(ks[7], (L, D_CONV, D_MODEL), D_CONV ** -0.5),
        "w_out": nrm(ks[8], (L, D_MODEL, D_MODEL), D_MODEL ** -0.5),
        "g_ffn": 1.0 + nrm(ks[9], (L, D_MODEL), 0.02),
        "w_q": nrm(ks[10], (L, D_MODEL, PEER_HEADS * PEER_QUERY_DIM), D_MODEL ** -0.5),
        "sub_keys": nrm(ks[11], (L, PEER_HEADS, 2, N_KEYS, PEER_HALF), PEER_HALF ** -0.5),
        "expert_u": nrm(ks[12], (L, N_EXPERTS, D_MODEL), D_MODEL ** -0.5),
        "expert_v": nrm(ks[13], (L, N_EXPERTS, D_MODEL), PEER_HEADS ** -0.5),
        "g_final": 1.0 + nrm(ks[14], (D_MODEL,), 0.02),
    }


def reference(x, g_mix, w_in, pool_group_w, pool_scale, conv_w, w_branch_pool, w_branch_conv,
              w_out, g_ffn, w_q, sub_keys, expert_u, expert_v, g_final):
    for l in range(DEPTH):
        h = rmsnorm(x, g_mix[l])
        z = h @ w_in[l]
        o1 = D_POOL
        o2 = o1 + D_CONV
        o3 = o2 + D_CONV
        o4 = o3 + D_CONV
        o5 = o4 + D_MODEL
        z_pool, z_c, z_b, z_v = z[..., :o1], z[..., o1:o2], z[..., o2:o3], z[..., o3:o4]
        gate_a, gate_b = z[..., o4:o5], z[..., o5:]
        a = pool_mixer(z_pool, pool_group_w[l], pool_scale[l]) @ w_branch_pool[l]
        b = short_conv_mixer(z_c, z_b, z_v, conv_w[l]) @ w_branch_conv[l]
        m = jax.nn.sigmoid(gate_a) * a + jax.nn.sigmoid(gate_b) * b
        x = x + m @ w_out[l]
        h2 = rmsnorm(x, g_ffn[l])
        x = x + peer_ffn(h2, w_q[l], sub_keys[l], expert_u[l], expert_v[l])
    return rmsnorm(x, g_final)
```

```python
import numpy as np
from contextlib import ExitStack
import concourse.bass as bass
import concourse.mybir as mybir
from concourse.bass_utils import run_bass_kernel_spmd

F32 = mybir.dt.float32
BF16 = mybir.dt.bfloat16
U32 = mybir.dt.uint32
ALU = mybir.AluOpType
AF = mybir.ActivationFunctionType
AX = mybir.AxisListType

D = 1024
DIN = 4096
NEXP = 16384
NCH = 128
TB = 256
EPS = 1e-6
NBUF = 6
NSB = 8


class T:
    __slots__ = ("name", "w", "r", "dsem")

    def __init__(self, name):
        self.name = name
        self.w = None
        self.r = {}
        self.dsem = None


class Sem:
    __slots__ = ("h", "n")

    def __init__(self, h):
        self.h = h
        self.n = 0


import os
class _Stop(Exception):
    pass


_STOP = [False]


def chk(n):
    if int(os.environ.get('KSTAGE', '0')) == n:
        _STOP[0] = True


class Ctx:
    def __init__(self, nc, es):
        self.nc, self.es = nc, es
        self.E = dict(pe=nc.tensor, act=nc.scalar, dve=nc.vector, pool=nc.gpsimd, sp=nc.sync)
        self.allsems = []
        self.esem = {k: self.newsem("e_" + k) for k in ("pe", "act", "dve", "pool")}
        self.seen = {k: {} for k in self.E}

    def newsem(self, name):
        s = Sem(self.es.enter_context(self.nc.semaphore(name)))
        self.allsems.append(s)
        return s

    def _wait(self, eng, need):
        seen = self.seen[eng]
        for s, v in need.items():
            if seen.get(s, 0) < v:
                self.E[eng].wait_ge(s.h, v)
                seen[s] = v

    def _need(self, eng, reads, writes, acc, skip=None):
        need = {}

        def add(s, v):
            if s is skip:
                return
            if need.get(s, 0) < v:
                need[s] = v
        me = self.esem.get(eng)
        for t in reads:
            if t.w is not None:
                add(*t.w)
        for t in writes:
            if t.w is not None and t.w[0] is not me:
                add(*t.w)
            for s, v in t.r.items():
                if s is not me:
                    add(s, v)
        return need

    def _mark(self, s, v, reads, writes):
        for t in reads:
            if t.r.get(s, 0) < v:
                t.r[s] = v
        for t in writes:
            t.w = (s, v)
            t.r = {}

    def op(self, eng, fn, reads=(), writes=(), acc=()):
        if _STOP[0]:
            return
        self._wait(eng, self._need(eng, reads, writes, acc))
        inst = fn(self.E[eng])
        if isinstance(inst, (list, tuple)):
            inst = inst[-1]
        s = self.esem[eng]
        s.n += 1
        inst.then_inc(s.h, 1)
        self._mark(s, s.n, reads, writes)

    def dma(self, eng, fn, reads=(), writes=(), sem_of=None, group=False):
        if _STOP[0]:
            return
        t = sem_of if sem_of is not None else (writes[0] if writes else reads[0])
        if t.dsem is None:
            t.dsem = self.newsem("d_" + t.name)
        self._wait(eng, self._need(eng, reads, writes, (), skip=(t.dsem if group else None)))
        inst = fn(self.E[eng])
        t.dsem.n += 16
        inst.then_inc(t.dsem.h, 16)
        self._mark(t.dsem, t.dsem.n, reads, writes)

    def barrier(self, engs=("pe", "act", "dve", "pool", "sp")):
        for e in engs:
            self._wait(e, {s: s.n for s in self.allsems if s.n > 0})


def build(NTOK, SEQ, phase='AB'):
    _STOP[0] = False
    nc = bass.Bass("TRN2", target_bir_lowering=False)
    NBLK = NTOK // TB
    BPS = SEQ // TB

    def din(name, shape, dt=F32):
        return nc.dram_tensor(name, shape, dt, kind="ExternalInput").ap()

    x = din("x", [NTOK, D])
    w_in = din("w_in", [D, DIN])
    w_bp = din("w_bp", [512, D])
    w_bc = din("w_bc", [512, D])
    w_out = din("w_out", [D, D])
    w_q = din("w_q", [D, 2048])
    gw = din("gw", [128, 4, 128])
    pscale_d = din("pscale", [128, 4])
    cw_d = din("cw", [128, 4, 3])
    gmix_d = din("gmix", [D])
    gffn_d = din("gffn", [D])
    gfin_d = din("gfin", [D])
    keysT = din("keysT", [128, 16, 128])
    uT = din("uT", [NEXP, D])
    vv = din("v", [NEXP, D])
    y = nc.dram_tensor("y", [NTOK, D], F32, kind="ExternalOutput").ap()
    x1d = nc.dram_tensor("x1d", [NTOK, D], F32, kind="Internal").ap()
    uTb = nc.dram_tensor("uTb", [NEXP, D], BF16, kind="Internal").ap()
    vb = nc.dram_tensor("vb", [NEXP, D], BF16, kind="Internal").ap()
    wqb = nc.dram_tensor("wqb", [D, 2048], BF16, kind="Internal").ap()
    Gd = nc.dram_tensor("Gd", [2, NCH, 128, TB], BF16, kind="Internal").ap()

    with ExitStack() as es:
        K = Ctx(nc, es)

        def sbt(st, name, shape, dt):
            return st.enter_context(nc.sbuf_tensor(name, shape, dt))

        def pst(st, name, shape, dt):
            return st.enter_context(nc.psum_tensor(name, shape, dt))

        io128 = sbt(es, "io128", [128, 128], F32); Tio = T("io128")
        pidx = sbt(es, "pidx", [128, 1], F32); Tpidx = T("pidx")
        idf = sbt(es, "idf", [128, 128], F32); Tidf = T("idf")
        idb = sbt(es, "idb", [128, 128], BF16); Tidb = T("idb")
        K.op("pool", lambda e: e.iota(io128[:], pattern=[[1, 128]], base=0, channel_multiplier=0,
                                      allow_small_or_imprecise_dtypes=True), writes=[Tio])
        K.op("pool", lambda e: e.iota(pidx[:], pattern=[[0, 1]], base=0, channel_multiplier=1,
                                      allow_small_or_imprecise_dtypes=True), writes=[Tpidx])
        K.op("dve", lambda e: e.tensor_scalar(out=idf[:], in0=io128[:], scalar1=pidx[:, 0:1], scalar2=None,
                                              op0=ALU.is_equal), reads=[Tio, Tpidx], writes=[Tidf])
        K.op("dve", lambda e: e.tensor_copy(out=idb[:], in_=idf[:]), reads=[Tidf], writes=[Tidb])

        Texp = T("experts")
        Tx1d = [T("x1d%d" % b) for b in range(NBLK)]
        Tst = [T("st0"), T("st1")]

        with ExitStack() as ea:
            w_in_bf = sbt(ea, "w_in_bf", [128, 8, DIN], BF16); Twin = T("w_in")
            w_bp_bf = sbt(ea, "w_bp_bf", [128, 4, D], BF16); Twbp = T("w_bp")
            w_bc_bf = sbt(ea, "w_bc_bf", [128, 4, D], BF16); Twbc = T("w_bc")
            w_out_bf = sbt(ea, "w_out_bf", [128, 8, D], BF16); Twout = T("w_out")
            gw_bf = sbt(ea, "gw_bf", [128, 4, 128], BF16); Tgw = T("gw")
            pscale = sbt(ea, "pscale_s", [128, 4], F32); Tpsc = T("pscale")
            cw = sbt(ea, "cw_s", [128, 4, 3], F32); Tcw = T("cw")
            gmix = sbt(ea, "gmix_s", [128, D], F32); Tgmix = T("gmix")
            invcnt = sbt(ea, "invcnt", [128, 4, 16], F32); Tinv = T("invcnt")
            io16p = sbt(ea, "io16p", [128, 16], F32); Tio16p = T("io16p")
            xblk = [sbt(ea, "xblk%d" % i, [128, 2, D], F32) for i in range(2)]
            Txblk = [T("xblk0"), T("xblk1")]
            junk = sbt(ea, "junkA", [128, D], BF16); Tjunk = T("junk")
            ss = sbt(ea, "ssA", [128, 2], F32); Tss = T("ss")
            sd = sbt(ea, "sdA", [128, 2], F32); Tsd = T("sd")
            rs = sbt(ea, "rsA", [128, 2], F32); Trs = T("rs")
            hbf = sbt(ea, "hbf", [128, 2, D], BF16); Thbf = T("hbf")
            hT = sbt(ea, "hT", [128, 8, TB], BF16); ThT = T("hT")
            zp = [sbt(ea, "zp%d" % i, [128, 4, 16 + TB], F32) for i in range(2)]
            Tzp = [T("zp0"), T("zp1")]
            sA = sbt(ea, "sA", [128, 4, 16 + TB], F32); TsA = T("sA")
            sB = sbt(ea, "sB", [128, 4, 16 + TB], F32); TsB = T("sB")
            tmpc = sbt(ea, "tmpc", [128, 4, 16], F32); Ttmpc = T("tmpc")
            zc = sbt(ea, "zc", [128, 12, TB], F32); Tzc = T("zc")
            ub = [sbt(ea, "ub%d" % i, [128, 4, 2 + TB], F32) for i in range(2)]
            Tub = [T("ub0"), T("ub1")]
            yb = sbt(ea, "yb", [128, 4, TB], F32); Tyb = T("yb")
            sig = sbt(ea, "sig", [128, 16, TB], F32); Tsig = T("sig")
            pbf = sbt(ea, "pbf", [128, 4, TB], BF16); Tpbf = T("pbf")
            pmbf = sbt(ea, "pmbf", [128, 4, TB], BF16); Tpmbf = T("pmbf")
            cvbf = sbt(ea, "cvbf", [128, 4, TB], BF16); Tcvbf = T("cvbf")
            t12 = sbt(ea, "t12", [128, 2, TB], F32); Tt12 = T("t12")
            mbf = sbt(ea, "mbf", [128, 8, TB], BF16); Tmbf = T("mbf")
            tps = pst(ea, "tpsA", [128, 8, 128], BF16); Ttps = T("tps")
            PBA = [pst(ea, "pbA%d" % i, [128, 512], F32) for i in range(7)]
            TPBA = [T("pbA%d" % i) for i in range(7)]
            zpsb = [PBA[0], PBA[1]]; Tzps = [TPBA[0], TPBA[1]]
            gpsb = [PBA[2], PBA[3]]; Tgpsb = [TPBA[2], TPBA[3]]
            abps = [PBA[4], PBA[5]]; Tabps = [TPBA[4], TPBA[5]]
            xops = [PBA[6], PBA[2]]; Txops = [TPBA[6], TPBA[2]]
            gpsv = lambda g: gpsb[g // 2][:, (g % 2) * TB:(g % 2 + 1) * TB]

            for k in range(8):
                for pc in range(4):
                    K.dma("pool", lambda e, k=k, pc=pc: e.dma_start(
                        out=w_in_bf[:, k, pc * 1024:(pc + 1) * 1024],
                        in_=w_in[k * 128:(k + 1) * 128, pc * 1024:(pc + 1) * 1024]), writes=[Twin], group=True)
            K.dma("pool", lambda e: e.dma_start(out=gw_bf[:], in_=gw), writes=[Tgw])
            for g in range(4):
                K.dma("pool", lambda e, g=g: e.dma_start(out=w_bp_bf[:, g, :], in_=w_bp[g * 128:(g + 1) * 128, :]),
                      writes=[Twbp], group=True)
                K.dma("pool", lambda e, g=g: e.dma_start(out=w_bc_bf[:, g, :], in_=w_bc[g * 128:(g + 1) * 128, :]),
                      writes=[Twbc], group=True)
            for k in range(8):
                K.dma("pool", lambda e, k=k: e.dma_start(out=w_out_bf[:, k, :], in_=w_out[k * 128:(k + 1) * 128, :]),
                      writes=[Twout], group=True)
            K.dma("sp", lambda e: e.dma_start(out=pscale[:], in_=pscale_d), writes=[Tpsc])
            K.dma("sp", lambda e: e.dma_start(out=cw[:], in_=cw_d), writes=[Tcw])
            K.dma("sp", lambda e: e.dma_start(out=gmix[:], in_=gmix_d.partition_broadcast(128)), writes=[Tgmix])
            NPIECE = 8
            RP = NEXP // NPIECE
            for pc in range(NPIECE if 'B' in phase else 0):
                K.dma("pool", lambda e, pc=pc: e.dma_start(out=uTb[pc * RP:(pc + 1) * RP, :],
                                                           in_=uT[pc * RP:(pc + 1) * RP, :]),
                      writes=[Texp], group=True)
                K.dma("pool", lambda e, pc=pc: e.dma_start(out=vb[pc * RP:(pc + 1) * RP, :],
                                                           in_=vv[pc * RP:(pc + 1) * RP, :]),
                      writes=[Texp], group=True)
            K.op("dve", lambda e: e.tensor_scalar(out=io16p[:], in0=io128[:, 0:16], scalar1=1.0, scalar2=None,
                                                  op0=ALU.add), reads=[Tio], writes=[Tio16p])
            K.op("dve", lambda e: [e.tensor_scalar(out=invcnt[:, g, :], in0=io16p[:], scalar1=float(2 ** (g + 1)),
                                                   scalar2=None, op0=ALU.min) for g in range(4)],
                 reads=[Tio16p], writes=[Tinv])
            K.op("dve", lambda e: e.reciprocal(out=invcnt[:], in_=invcnt[:]), reads=[Tinv], writes=[Tinv])

            def headA(b):
                par = b % 2
                tok0 = b * TB
                X = xblk[par]
                TX = Txblk[par]
                K.dma("sp", lambda e: e.dma_start(out=X[:], in_=x[tok0:tok0 + TB, :].rearrange("(j p) d -> p j d", p=128)),
                      writes=[TX])
                for j in range(2):
                    K.op("act", lambda e, j=j: e.activation(out=junk[:], in_=X[:, j, :], func=AF.Square,
                                                            accum_out=ss[:, j:j + 1]),
                         reads=[TX], writes=[Tjunk, Tss])
                K.op("act", lambda e: e.activation(out=sd[:], in_=ss[:], func=AF.Sqrt, scale=1.0 / D, bias=EPS),
                     reads=[Tss], writes=[Tsd])
                K.op("dve", lambda e: e.reciprocal(out=rs[:], in_=sd[:]), reads=[Tsd], writes=[Trs])
                K.op("dve", lambda e: [e.scalar_tensor_tensor(out=hbf[:, j, :], in0=X[:, j, :], scalar=rs[:, j:j + 1],
                                                              in1=gmix[:], op0=ALU.mult, op1=ALU.mult)
                                       for j in range(2)],
                     reads=[TX, Trs, Tgmix], writes=[Thbf])
            chk(1)
            for b in range(NBLK):
                par = b % 2
                first = (b % BPS == 0)
                tok0 = b * TB
                X = xblk[par]
                TX = Txblk[par]
                Z = zp[par]
                TZ = Tzp[par]
                U = ub[par]
                TU = Tub[par]
                if b == 0:
                    headA(0)
                chk(2)
                for j in range(2):
                    K.op("pe", lambda e, j=j: [e.transpose(out=tps[:, k, :], in_=hbf[:, j, k * 128:(k + 1) * 128],
                                                           identity=idb[:]) for k in range(8)],
                         reads=[Thbf, Tidb], writes=[Ttps])
                    if j == 0:
                        K.op("act", lambda e, j=j: e.activation(out=hT[:, :, j * 128:(j + 1) * 128], in_=tps[:],
                                                                func=AF.Copy), writes=[ThT, Ttps])
                    else:
                        K.op("dve", lambda e, j=j: e.tensor_copy(out=hT[:, :, j * 128:(j + 1) * 128], in_=tps[:]),
                             writes=[ThT, Ttps])
                chk(3)
                if first:
                    K.op("dve", lambda e: [e.memset(Z[:, :, 0:16], 0.0), e.memset(U[:, :, 0:2], 0.0)],
                         writes=[TZ, TU])
                else:
                    K.op("dve", lambda e: [e.tensor_copy(out=Z[:, :, 0:16], in_=zp[1 - par][:, :, TB:TB + 16]),
                                           e.tensor_copy(out=U[:, :, 0:2], in_=ub[1 - par][:, :, TB:TB + 2])],
                         reads=[Tzp[1 - par], Tub[1 - par]], writes=[TZ, TU])
                chk(31)
                for oc in range(32):
                    zi = oc % 2
                    K.op("pe", lambda e, oc=oc, zi=zi: [e.matmul(zpsb[zi][:, 0:TB], lhsT=w_in_bf[:, k, oc * 128:(oc + 1) * 128],
                                                                 rhs=hT[:, k, :], start=(k == 0), stop=(k == 7))
                                                        for k in range(8)],
                         reads=[ThT, Twin], writes=[Tzps[zi]])
                    if oc == 0:
                        chk(321)
                    if oc < 4:
                        K.op("act", lambda e, oc=oc, zi=zi: e.activation(out=Z[:, oc, 16:16 + TB], in_=zpsb[zi][:, 0:TB],
                                                                         func=AF.Copy), writes=[TZ, Tzps[zi]])
                    elif oc < 16:
                        K.op("act", lambda e, oc=oc, zi=zi: e.activation(out=zc[:, oc - 4, :], in_=zpsb[zi][:, 0:TB],
                                                                         func=AF.Copy), writes=[Tzc, Tzps[zi]])
                    else:
                        K.op("act", lambda e, oc=oc, zi=zi: e.activation(out=sig[:, oc - 16, :], in_=zpsb[zi][:, 0:TB],
                                                                         func=AF.Sigmoid), writes=[Tsig, Tzps[zi]])
                    if oc == 2:
                        chk(32)
                    if oc == 0:
                        chk(322)
                    if oc == 3:
                        W1 = 16 + TB
                        K.op("dve", lambda e: e.tensor_tensor(out=sA[:, :, 1:W1], in0=Z[:, :, 1:W1], in1=Z[:, :, 0:W1 - 1],
                                                              op=ALU.add), reads=[TZ], writes=[TsA])
                        K.op("dve", lambda e: [
                            e.scalar_tensor_tensor(out=pbf[:, 0, :], in0=sA[:, 0, 16:W1], scalar=0.5, in1=Z[:, 0, 16:W1],
                                                   op0=ALU.mult, op1=ALU.subtract),
                            e.tensor_tensor(out=sB[:, 1:4, 3:W1], in0=sA[:, 1:4, 3:W1], in1=sA[:, 1:4, 1:W1 - 2],
                                            op=ALU.add)],
                             reads=[TsA, TZ], writes=[Tpbf, TsB])
                        K.op("dve", lambda e: [
                            e.scalar_tensor_tensor(out=pbf[:, 1, :], in0=sB[:, 1, 16:W1], scalar=0.25, in1=Z[:, 1, 16:W1],
                                                   op0=ALU.mult, op1=ALU.subtract),
                            e.tensor_tensor(out=sA[:, 2:4, 7:W1], in0=sB[:, 2:4, 7:W1], in1=sB[:, 2:4, 3:W1 - 4],
                                            op=ALU.add)],
                             reads=[TsB, TZ], writes=[Tpbf, TsA])
                        K.op("dve", lambda e: [
                            e.scalar_tensor_tensor(out=pbf[:, 2, :], in0=sA[:, 2, 16:W1], scalar=0.125, in1=Z[:, 2, 16:W1],
                                                   op0=ALU.mult, op1=ALU.subtract),
                            e.tensor_tensor(out=sB[:, 3, 15:W1], in0=sA[:, 3, 15:W1], in1=sA[:, 3, 7:W1 - 8],
                                            op=ALU.add)],
                             reads=[TsA, TZ], writes=[Tpbf, TsB])
                        K.op("dve", lambda e: e.scalar_tensor_tensor(out=pbf[:, 3, :], in0=sB[:, 3, 16:W1], scalar=1.0 / 16,
                                                                     in1=Z[:, 3, 16:W1], op0=ALU.mult, op1=ALU.subtract),
                             reads=[TsB, TZ], writes=[Tpbf])
                        if first:
                            srcs = [sA[:, 0, 16:32], sB[:, 1, 16:32], sA[:, 2, 16:32], sB[:, 3, 16:32]]
                            K.op("dve", lambda e: [e.tensor_tensor(out=tmpc[:, g, :], in0=srcs[g], in1=invcnt[:, g, :],
                                                                   op=ALU.mult) for g in range(4)],
                                 reads=[TsA, TsB, Tinv], writes=[Ttmpc])
                            K.op("dve", lambda e: e.tensor_tensor(out=pbf[:, :, 0:16], in0=tmpc[:], in1=Z[:, :, 16:32],
                                                                  op=ALU.subtract), reads=[Ttmpc, TZ], writes=[Tpbf])
                    if oc == 9:
                        K.op("pe", lambda e: [e.matmul(gpsv(g), lhsT=gw_bf[:, g, :], rhs=pbf[:, g, :], start=True,
                                                       stop=True) for g in range(4)],
                             reads=[Tpbf, Tgw], writes=Tgpsb)
                        K.op("dve", lambda e: [e.tensor_scalar(out=pmbf[:, g, :], in0=gpsv(g), scalar1=pscale[:, g:g + 1],
                                                               scalar2=None, op0=ALU.mult) for g in range(4)],
                             reads=[Tpsc], writes=[Tpmbf] + Tgpsb)
                    if oc == 4:
                        chk(34)
                    if oc == 15:
                        chk(35)
                        K.op("dve", lambda e: e.tensor_tensor(out=U[:, :, 2:2 + TB], in0=zc[:, 0:4, :], in1=zc[:, 8:12, :],
                                                              op=ALU.mult), reads=[Tzc], writes=[TU])
                        K.op("dve", lambda e: [e.tensor_scalar(out=yb[:, ch, :], in0=U[:, ch, 2:2 + TB],
                                                               scalar1=cw[:, ch, 2:3], scalar2=None, op0=ALU.mult)
                                               for ch in range(4)], reads=[TU, Tcw], writes=[Tyb])
                        for kk in (1, 0):
                            K.op("dve", lambda e, kk=kk: [e.scalar_tensor_tensor(out=yb[:, ch, :], in0=U[:, ch, kk:kk + TB],
                                                                                 scalar=cw[:, ch, kk:kk + 1], in1=yb[:, ch, :],
                                                                                 op0=ALU.mult, op1=ALU.add)
                                                          for ch in range(4)], reads=[TU, Tcw, Tyb], writes=[Tyb])
                        K.op("dve", lambda e: e.tensor_tensor(out=cvbf[:], in0=yb[:], in1=zc[:, 4:8, :], op=ALU.mult),
                             reads=[Tyb, Tzc], writes=[Tcvbf])
                chk(4)
                if b + 1 < NBLK:
                    headA(b + 1)
                for dc in range(8):
                    ab = abps[dc % 2]
                    Tab = Tabps[dc % 2]
                    K.op("pe", lambda e, dc=dc, ab=ab: (
                        [e.matmul(ab[:, 0:TB], lhsT=w_bp_bf[:, g, dc * 128:(dc + 1) * 128], rhs=pmbf[:, g, :],
                                  start=(g == 0), stop=(g == 3)) for g in range(4)] +
                        [e.matmul(ab[:, TB:2 * TB], lhsT=w_bc_bf[:, g, dc * 128:(dc + 1) * 128], rhs=cvbf[:, g, :],
                                  start=(g == 0), stop=(g == 3)) for g in range(4)]),
                         reads=[Tpmbf, Tcvbf, Twbp, Twbc], writes=[Tab])
                    K.op("dve", lambda e, dc=dc, ab=ab: [
                        e.tensor_tensor(out=t12[:, 0, :], in0=sig[:, dc, :], in1=ab[:, 0:TB], op=ALU.mult),
                        e.tensor_tensor(out=t12[:, 1, :], in0=sig[:, 8 + dc, :], in1=ab[:, TB:2 * TB], op=ALU.mult)],
                         reads=[Tsig], writes=[Tt12, Tab])
                    K.op("dve", lambda e, dc=dc: e.tensor_tensor(out=mbf[:, dc, :], in0=t12[:, 0, :], in1=t12[:, 1, :],
                                                                 op=ALU.add), reads=[Tt12], writes=[Tmbf])
                chk(5)
                for j in range(2):
                    for hf in range(2):
                        xi = (2 * j + hf) % 2
                        K.op("pe", lambda e, j=j, hf=hf, xi=xi: [
                            e.matmul(xops[xi][:], lhsT=mbf[:, k, j * 128:(j + 1) * 128],
                                     rhs=w_out_bf[:, k, hf * 512:(hf + 1) * 512], start=(k == 0), stop=(k == 7))
                            for k in range(8)], reads=[Tmbf, Twout], writes=[Txops[xi]])
                        K.op("dve", lambda e, j=j, hf=hf, xi=xi: e.tensor_tensor(
                            out=X[:, j, hf * 512:(hf + 1) * 512], in0=X[:, j, hf * 512:(hf + 1) * 512], in1=xops[xi][:],
                            op=ALU.add), reads=[TX], writes=[TX, Txops[xi]])
                dst = x1d if 'B' in phase else y
                K.dma("sp", lambda e: e.dma_start(out=dst[tok0:tok0 + TB, :].rearrange("(j p) d -> p j d", p=128), in_=X[:]),
                      reads=[TX], writes=[Tx1d[b]], sem_of=Tst[par])
            K.barrier()

        with ExitStack() as eb:
            if 'B' not in phase:
                return nc
            keys_bf = sbt(eb, "keys_bf", [128, 16, 128], BF16); Tkeys = T("keys")
            gffn = sbt(eb, "gffn_s", [128, D], F32); Tgffn = T("gffn")
            gfin = sbt(eb, "gfin_s", [128, D], F32); Tgfin = T("gfin")
            iob = sbt(eb, "iob", [128, 128], BF16); Tiob = T("iob")
            ecst = sbt(eb, "ecst", [128, 128], F32); Tecst = T("ecst")
            nhalf = sbt(eb, "nhalf", [128, 2], F32); Tnhalf = T("nhalf")
            wqr = [sbt(eb, "wqr%d" % i, [128, 8, 256], BF16) for i in range(2)]; Twqr = [T("wqr0"), T("wqr1")]
            xp = sbt(eb, "xp", [128, 2, D], F32); Txp = T("xp")
            xe = sbt(eb, "xe", [128, 2, D], F32); Txe = T("xe")
            ss1 = sbt(eb, "ss1", [128, 2], F32); Tss1 = T("ss1")
            ms1 = sbt(eb, "ms1", [128, 2], F32); Tms1 = T("ms1")
            rs1 = sbt(eb, "rs1", [128, 2], F32); Trs1 = T("rs1")
            ss2 = sbt(eb, "ss2", [128, 2], F32); Tss2 = T("ss2")
            ms2 = sbt(eb, "ms2", [128, 2], F32); Tms2 = T("ms2")
            rs2 = sbt(eb, "rs2", [128, 2], F32); Trs2 = T("rs2")
            h2bf = sbt(eb, "h2bf", [128, 2, D], BF16); Th2bf = T("h2bf")
            h2T = [sbt(eb, "h2T%d" % i, [128, 8, TB], BF16) for i in range(3)]; Th2T = [T("h2T%d" % i) for i in range(3)]
            qT = sbt(eb, "qT", [128, 16, TB], BF16); TqT = T("qT")
            S = sbt(eb, "S", [128, 2048], F32); TS = T("S")
            S2 = sbt(eb, "S2", [128, 2048], F32); TS2 = T("S2")
            cand = sbt(eb, "cand", [128, 2048], F32); Tcand = T("cand")
            V16 = sbt(eb, "V16", [128, 16, 16], F32); TV16 = T("V16")
            IX = sbt(eb, "IX", [128, 16, 16], U32); TIX = T("IX")
            IXf = sbt(eb, "IXf", [128, 16, 16], F32); TIXf = T("IXf")
            F16 = sbt(eb, "F16", [128, 8, 16], F32); TF16 = T("F16")
            PS = sbt(eb, "PS", [128, 8, 16], U32); TPS = T("PS")
            au = sbt(eb, "au", [128, 2, 128], U32); Tau = T("au")
            apf = sbt(eb, "apf", [128, 2, 128], F32); Tapf = T("apf")
            sh = sbt(eb, "sh", [128, 128], F32); Tsh = T("sh")
            ex = sbt(eb, "ex", [128, 128], F32); Tex = T("ex")
            Zs = sbt(eb, "Zs", [128, 8], F32); TZs = T("Zs")
            rZ = sbt(eb, "rZ", [128, 8], F32); TrZ = T("rZ")
            IJG = sbt(eb, "IJG", [128, 3, 128], F32); TIJG = T("IJG")
            IJGT = [sbt(eb, "IJGT%d" % i, [128, 3, TB], F32) for i in range(2)]; TIJGT = [T("IJGT0"), T("IJGT1")]
            Qb = [sbt(eb, "Qb%d" % i, [128, NSB, 128], BF16) for i in range(2)]; TQb = [T("Qb0"), T("Qb1")]
            Pb = [sbt(eb, "Pb%d" % i, [128, NSB, 128], BF16) for i in range(2)]; TPb = [T("Pb0"), T("Pb1")]
            Gbuf = sbt(eb, "Gbuf", [128, NCH, TB], BF16); TG = T("Gbuf")
            Gr = [sbt(eb, "Gr%d" % i, [128, TB], BF16) for i in range(NBUF)]; TGr = [T("Gr%d" % i) for i in range(NBUF)]
            TGd = [T("Gd0"), T("Gd1")]
            UT = [sbt(eb, "UT%d" % i, [128, D], BF16) for i in range(NBUF)]; TUT = [T("UT%d" % i) for i in range(NBUF)]
            Vc = [sbt(eb, "Vc%d" % i, [128, D], BF16) for i in range(NBUF)]; TVc = [T("Vc%d" % i) for i in range(NBUF)]
            gl = [sbt(eb, "gl%d" % i, [128, TB], BF16) for i in range(2)]; Tgl = [T("gl0"), T("gl1")]
            Wb = [sbt(eb, "Wb%d" % i, [128, TB], BF16) for i in range(4)]; TW = [T("W%d" % i) for i in range(4)]
            opsb = [pst(eb, "ops%d" % i, [128, 512], F32) for i in range(4)]; Tops = [T("ops%d" % i) for i in range(4)]
            tpb = pst(eb, "tpsB", [128, 8, 128], BF16); Ttpb = T("tpsB")
            MB = [pst(eb, "MB%d" % i, [128, 512], F32) for i in range(3)]
            TMB = [T("MB0"), T("MB1"), T("MB2")]
            Apsb = [MB[1], MB[2]]; TAps = [TMB[1], TMB[2]]

            Twqb = T("wqb")
            for pc in range(2):
                K.dma("pool", lambda e: e.dma_start(out=wqb[:, pc * 1024:(pc + 1) * 1024], in_=w_q[:, pc * 1024:(pc + 1) * 1024]),
                      writes=[Twqb], group=True)
            K.dma("pool", lambda e: e.dma_start(out=keys_bf[:], in_=keysT), writes=[Tkeys])
            K.dma("sp", lambda e: e.dma_start(out=gffn[:], in_=gffn_d.partition_broadcast(128)), writes=[Tgffn])
            K.dma("sp", lambda e: e.dma_start(out=gfin[:], in_=gfin_d.partition_broadcast(128)), writes=[Tgfin])
            K.op("dve", lambda e: [e.tensor_copy(out=iob[:], in_=io128[:]), e.memset(ecst[:], float(np.e)),
                                   e.memset(nhalf[:], -0.5)], reads=[Tio], writes=[Tiob, Tecst, Tnhalf])
            wqb3 = wqb.rearrange("(k p) n -> p k n", p=128)

            uTb3 = uTb.rearrange("(c p) n -> c p n", p=128)
            vb3 = vb.rearrange("(c p) n -> c p n", p=128)
            NG = NBLK * NCH

            def load_chunk(gi):
                if gi >= NG:
                    return
                c = gi % NCH
                sl = gi % NBUF
                K.dma("sp", lambda e: e.dma_start(out=UT[sl][:], in_=uTb3[c]), reads=[Texp], writes=[TUT[sl]])
                K.dma("sp", lambda e: e.dma_start(out=Vc[sl][:], in_=vb3[c]), reads=[Texp], writes=[TVc[sl]])
                gb = (gi // NCH) % 2
                K.dma("sp", lambda e: e.dma_start(out=Gr[sl][:], in_=Gd[gb, c]), reads=[TGd[gb]], writes=[TGr[sl]])

            def rstd(ssx, Tssx, msx, Tmsx, rsx, Trsx):
                K.op("pool", lambda e: e.tensor_scalar(out=msx[:], in0=ssx[:], scalar1=1.0 / D, scalar2=EPS,
                                                       op0=ALU.mult, op1=ALU.add), reads=[Tssx], writes=[Tmsx])
                K.op("pool", lambda e: e.tensor_tensor(out=rsx[:], in0=msx[:], in1=nhalf[:], op=ALU.pow),
                     reads=[Tmsx, Tnhalf], writes=[Trsx])

            def load_wq(qp):
                K.dma("sp", lambda e: e.dma_start(out=wqr[qp % 2][:], in_=wqb3[:, :, qp * 256:(qp + 1) * 256]),
                      reads=[Twqb], writes=[Twqr[qp % 2]])

            def prefetch_p1(b):
                if b >= NBLK:
                    return
                t0_ = b * TB
                K.dma("sp", lambda e: e.dma_start(out=xp[:], in_=x1d[t0_:t0_ + TB, :].rearrange("(j p) d -> p j d", p=128)),
                      reads=[Tx1d[b]], writes=[Txp])
                load_wq(0)
                load_wq(1)

            def part1(b):
                pb = b % 2
                tok0 = b * TB
                H2T = h2T[b % 3]
                TH2T = Th2T[b % 3]
                if b == 0:
                    prefetch_p1(0)
                for j in range(2):
                    K.op("act", lambda e: e.activation(out=h2bf[:, j, :], in_=xp[:, j, :], func=AF.Square,
                                                       accum_out=ss1[:, j:j + 1]), reads=[Txp], writes=[Th2bf, Tss1])
                    yield 0.1
                rstd(ss1, Tss1, ms1, Tms1, rs1, Trs1)
                yield 0.1
                for j in range(2):
                    for hf in range(2):
                        cs = slice(hf * 512, (hf + 1) * 512)
                        K.op("dve", lambda e: e.scalar_tensor_tensor(out=h2bf[:, j, cs], in0=xp[:, j, cs], scalar=rs1[:, j:j + 1],
                                                                     in1=gffn[:, cs], op0=ALU.mult, op1=ALU.mult),
                             reads=[Txp, Trs1, Tgffn], writes=[Th2bf])
                        yield 0.6
                yield 3.0
                for j in range(2):
                    K.op("pe", lambda e: [e.transpose(out=tpb[:, k, :], in_=h2bf[:, j, k * 128:(k + 1) * 128],
                                                      identity=idb[:]) for k in range(8)],
                         reads=[Th2bf, Tidb], writes=[Ttpb])
                    K.op("act", lambda e: e.activation(out=H2T[:, :, j * 128:(j + 1) * 128], in_=tpb[:], func=AF.Copy),
                         writes=[TH2T, Ttpb])
                    yield 0.3
                for qp in range(8):
                    wr = wqr[qp % 2]
                    K.op("pe", lambda e: [e.matmul(MB[0][:, hh * 256:(hh + 1) * 256], lhsT=wr[:, k, hh * 128:(hh + 1) * 128],
                                                   rhs=H2T[:, k, :], start=(k == 0), stop=(k == 7))
                                          for hh in range(2) for k in range(8)],
                         reads=[Twqr[qp % 2], TH2T], writes=[TMB[0]])
                    K.op("act", lambda e: e.activation(out=qT[:, 2 * qp:2 * qp + 2, :],
                                                       in_=MB[0][:].rearrange("p (a t) -> p a t", a=2), func=AF.Copy),
                         writes=[TqT, TMB[0]])
                    if qp + 2 < 8:
                        load_wq(qp + 2)
                    yield 0.4
                prefetch_p1(b + 1)
                Sg = lambda g: S[:, g * 128:(g + 1) * 128]
                S2g = lambda g: S2[:, g * 128:(g + 1) * 128]
                Cg = lambda h: cand[:, h * 256:(h + 1) * 256]
                C2g = lambda h: S[:, h * 256:(h + 1) * 256]
                for j in range(2):
                    for pc in range(4):
                        K.op("pe", lambda e: [e.matmul(MB[0][:, i * 128:(i + 1) * 128], lhsT=qT[:, pc * 4 + i, j * 128:(j + 1) * 128],
                                                       rhs=keys_bf[:, pc * 4 + i, :], start=True, stop=True) for i in range(4)],
                             reads=[TqT, Tkeys], writes=[TMB[0]])
                        K.op("act", lambda e: e.activation(out=S[:, pc * 512:(pc + 1) * 512], in_=MB[0][:], func=AF.Copy),
                             writes=[TS, TMB[0]])
                        yield 0.4
                    for gq in range(8):
                        gs = range(2 * gq, 2 * gq + 2)
                        K.op("dve", lambda e: [e.max(out=V16[:, g, 0:8], in_=Sg(g)) for g in gs], reads=[TS], writes=[TV16])
                        yield 0.5
                        K.op("dve", lambda e: [e.max_index(out=IX[:, g, 0:8], in_max=V16[:, g, 0:8], in_values=Sg(g)) for g in gs] +
                             [e.match_replace(out=S2g(g), in_to_replace=V16[:, g, 0:8], in_values=Sg(g), imm_value=-1e30) for g in gs],
                             reads=[TS, TV16], writes=[TIX, TS2])
                        yield 1.0
                        K.op("dve", lambda e: [e.max(out=V16[:, g, 8:16], in_=S2g(g)) for g in gs], reads=[TS2], writes=[TV16])
                        yield 0.5
                        K.op("dve", lambda e: [e.max_index(out=IX[:, g, 8:16], in_max=V16[:, g, 8:16], in_values=S2g(g)) for g in gs],
                             reads=[TS2, TV16], writes=[TIX])
                        yield 0.5
                    V16r = V16[:].rearrange("p (h two) k -> p h two k", two=2)
                    cand4 = cand[:].rearrange("p (h a b) -> p h a b", h=8, a=16)
                    for hq in range(8):
                        hs = slice(hq, hq + 1)
                        K.op("dve", lambda e: e.tensor_tensor(out=cand4[:, hs], in0=V16r[:, hs, 0, :].unsqueeze(3).to_broadcast([128, 1, 16, 16]),
                                                              in1=V16r[:, hs, 1, :].unsqueeze(2).to_broadcast([128, 1, 16, 16]),
                                                              op=ALU.add), reads=[TV16], writes=[Tcand])
                        yield 0.55
                    for hq in range(8):
                        hs = range(hq, hq + 1)
                        K.op("dve", lambda e: [e.max(out=F16[:, h, 0:8], in_=Cg(h)) for h in hs], reads=[Tcand], writes=[TF16])
                        yield 0.5
                        K.op("dve", lambda e: [e.max_index(out=PS[:, h, 0:8], in_max=F16[:, h, 0:8], in_values=Cg(h)) for h in hs] +
                             [e.match_replace(out=C2g(h), in_to_replace=F16[:, h, 0:8], in_values=Cg(h), imm_value=-1e30) for h in hs],
                             reads=[Tcand, TF16], writes=[TPS, TS])
                        yield 1.0
                        K.op("dve", lambda e: [e.max(out=F16[:, h, 8:16], in_=C2g(h)) for h in hs], reads=[TS], writes=[TF16])
                        yield 0.5
                        K.op("dve", lambda e: [e.max_index(out=PS[:, h, 8:16], in_max=F16[:, h, 8:16], in_values=C2g(h)) for h in hs],
                             reads=[TS, TF16], writes=[TPS])
                        yield 0.5
                    sh3 = sh[:].rearrange("p (h k) -> p h k", k=16)
                    ex3 = ex[:].rearrange("p (h k) -> p h k", k=16)
                    K.op("dve", lambda e: e.tensor_tensor(out=sh3, in0=F16[:], in1=F16[:, :, 0:1].to_broadcast([128, 8, 16]),
                                                          op=ALU.subtract), reads=[TF16], writes=[Tsh])
                    yield 0.3
                    yield 1.5
                    yield 1.5
                    K.op("act", lambda e: e.activation(out=ex[:], in_=sh[:], func=AF.Tanh, scale=0.5), reads=[Tsh], writes=[Tex])
                    yield 0.1
                    K.op("dve", lambda e: e.tensor_scalar(out=sh[:], in0=ex[:], scalar1=-1.0, scalar2=1.0, op0=ALU.mult, op1=ALU.add),
                         reads=[Tex], writes=[Tsh])
                    yield 0.2
                    K.op("dve", lambda e: [e.reciprocal(out=sh[:], in_=sh[:])], reads=[Tsh], writes=[Tsh])
                    yield 0.2
                    K.op("dve", lambda e: e.scalar_tensor_tensor(out=ex[:], in0=ex[:], scalar=1.0, in1=sh[:], op0=ALU.add, op1=ALU.mult),
                         reads=[Tex, Tsh], writes=[Tex])
                    yield 0.2
                    PSf = PS[:].rearrange("p h k -> p (h k)")
                    K.op("dve", lambda e: [e.tensor_single_scalar(out=au[:, 0, :], in_=PSf, scalar=4, op=ALU.arith_shift_right),
                                           e.tensor_single_scalar(out=au[:, 1, :], in_=PSf, scalar=15, op=ALU.bitwise_and)],
                         reads=[TPS], writes=[Tau])
                    yield 0.3
                    K.op("dve", lambda e: [e.tensor_copy(out=apf[:], in_=au[:]), e.tensor_copy(out=IXf[:], in_=IX[:])],
                         reads=[Tau, TIX], writes=[Tapf, TIXf])
                    yield 0.5
                    IXr = IXf[:].rearrange("p (h two) k -> p h two k", two=2)
                    eqs = [S2[:].rearrange("p (h k a) -> p h k a", h=8, k=16), cand[:].rearrange("p (h k a) -> p h k a", h=8, k=16)]
                    Teqs = [TS2, Tcand]
                    for wh in range(2):
                        pos3 = apf[:, wh, :].rearrange("p (h k) -> p h k", k=16)
                        for hq in range(8):
                            hs = slice(hq, hq + 1)
                            K.op("dve", lambda e: e.tensor_tensor(
                                out=eqs[wh][:, hs], in0=pos3[:, hs].unsqueeze(3).to_broadcast([128, 1, 16, 16]),
                                in1=io128[:, 0:16].unsqueeze(1).unsqueeze(1).to_broadcast([128, 1, 16, 16]), op=ALU.is_equal),
                                 reads=[Tapf, Tio], writes=[Teqs[wh]])
                            yield 0.55
                        for hq in range(8):
                            hs = slice(hq, hq + 1)
                            K.op("dve", lambda e: e.tensor_tensor(out=eqs[wh][:, hs], in0=eqs[wh][:, hs],
                                                                  in1=IXr[:, hs, wh, :].unsqueeze(2).to_broadcast([128, 1, 16, 16]),
                                                                  op=ALU.mult), reads=[Teqs[wh], TIXf], writes=[Teqs[wh]])
                            yield 0.55
                    for wh in range(2):
                        for hq in range(4):
                            hs = slice(2 * hq, 2 * hq + 2)
                            K.op("dve", lambda e: e.reduce_sum(out=IJG[:, wh, :].rearrange("p (h k) -> p h k", k=16)[:, hs],
                                                               in_=eqs[wh][:, hs], axis=AX.X), reads=[Teqs[wh]], writes=[TIJG])
                            yield 0.55
                        if wh == 0:
                            K.op("dve", lambda e: e.reduce_sum(out=Zs[:], in_=ex3, axis=AX.X), reads=[Tex], writes=[TZs])
                            yield 0.2
                            K.op("dve", lambda e: e.reciprocal(out=rZ[:], in_=Zs[:]), reads=[TZs], writes=[TrZ])
                            yield 0.1
                            K.op("dve", lambda e: e.tensor_tensor(out=IJG[:, 2, :].rearrange("p (h k) -> p h k", k=16), in0=ex3,
                                                                  in1=rZ[:].unsqueeze(2).to_broadcast([128, 8, 16]), op=ALU.mult),
                                 reads=[Tex, TrZ], writes=[TIJG])
                            yield 0.1
                    yield 1.5
                    yield 1.5
                    K.op("pe", lambda e: [e.transpose(out=MB[0][:, w * 128:(w + 1) * 128], in_=IJG[:, w, :], identity=idf[:])
                                          for w in range(3)], reads=[TIJG, Tidf], writes=[TMB[0]])
                    K.op("act", lambda e: e.activation(out=IJGT[pb][:, :, j * 128:(j + 1) * 128],
                                                       in_=MB[0][:, 0:384].rearrange("p (w t) -> p w t", w=3),
                                                       func=AF.Copy), writes=[TIJGT[pb], TMB[0]])
                    yield 0.3

            def part2gen(b):
                pb = b % 2
                IT = IJGT[pb]
                for sbi in range(TB // NSB):
                    sl = sbi % 2
                    t0 = sbi * NSB
                    K.op("dve", lambda e: [e.tensor_scalar(out=Qb[sl][:, i, :], in0=iob[:], scalar1=IT[:, 1, t0 + i:t0 + i + 1],
                                                           scalar2=None, op0=ALU.is_equal) for i in range(NSB)],
                         reads=[Tiob, TIJGT[pb]], writes=[TQb[sl]])
                    yield 1.3
                    K.op("dve", lambda e: [e.tensor_scalar(out=Pb[sl][:, i, :], in0=iob[:], scalar1=IT[:, 0, t0 + i:t0 + i + 1],
                                                           scalar2=IT[:, 2, t0 + i:t0 + i + 1], op0=ALU.is_equal, op1=ALU.mult)
                                           for i in range(NSB)],
                         reads=[Tiob, TIJGT[pb]], writes=[TPb[sl]])
                    yield 1.8
                    yield 0.6
                    for q4 in range(NSB // 4):
                        tt = t0 + q4 * 4
                        K.op("pe", lambda e: [e.matmul(MB[0][:, i * 128:(i + 1) * 128], lhsT=Qb[sl][:, q4 * 4 + i, :],
                                                       rhs=Pb[sl][:, q4 * 4 + i, :], start=True, stop=True) for i in range(4)],
                             reads=[TQb[sl], TPb[sl]], writes=[TMB[0]])
                        src = MB[0][:].rearrange("p (t i) -> p i t", t=4)
                        K.op("act", lambda e: e.activation(out=Gbuf[:, :, tt:tt + 4], in_=src, func=AF.Copy),
                             writes=[TG, TMB[0]])
                        yield 0.05
                yield 1.5
                for pc in range(16):
                    K.dma("act", lambda e: e.dma_start(out=Gd[pb, pc * 8:(pc + 1) * 8].rearrange("c j t -> j c t"),
                                                       in_=Gbuf[:, pc * 8:(pc + 1) * 8, :]),
                          reads=[TG], writes=[TGd[pb]], sem_of=TGd[pb], group=True)
                    yield 0.05

            def epi_adds(b):
                K.op("dve", lambda e: [e.tensor_tensor(out=xe[:, j, hf * 512:(hf + 1) * 512], in0=xe[:, j, hf * 512:(hf + 1) * 512],
                                                       in1=opsb[2 * j + hf][:], op=ALU.add)
                                       for j in range(2) for hf in range(2)], reads=[Txe], writes=[Txe] + Tops)

            def epi_rest(b):
                t0_ = b * TB
                for j in range(2):
                    K.op("act", lambda e: e.activation(out=h2bf[:, j, :], in_=xe[:, j, :], func=AF.Square,
                                                       accum_out=ss2[:, j:j + 1]), reads=[Txe], writes=[Th2bf, Tss2])
                    yield 0.1
                rstd(ss2, Tss2, ms2, Tms2, rs2, Trs2)
                yield 0.1
                for j in range(2):
                    for hf in range(2):
                        cs = slice(hf * 512, (hf + 1) * 512)
                        K.op("dve", lambda e: e.scalar_tensor_tensor(out=xe[:, j, cs], in0=xe[:, j, cs], scalar=rs2[:, j:j + 1],
                                                                     in1=gfin[:, cs], op0=ALU.mult, op1=ALU.mult),
                             reads=[Txe, Trs2, Tgfin], writes=[Txe])
                        yield 0.6
                K.dma("sp", lambda e: e.dma_start(out=y[t0_:t0_ + TB, :].rearrange("(j p) d -> p j d", p=128), in_=xe[:]),
                      reads=[Txe], sem_of=Tst[b % 2])
                yield 0.1

            def chain(*gens):
                for g in gens:
                    if g is not None:
                        for v in g:
                            yield v

            def drain(g):
                if g is not None:
                    for _ in g:
                        pass

            drain(part1(0))
            drain(part2gen(0))
            if NBLK > 1:
                drain(part1(1))
            for gi in range(NBUF - 1):
                load_chunk(gi)

            for b in range(NBLK):
                tok0 = b * TB
                if b > 0:
                    epi_adds(b - 1)
                bgs = [epi_rest(b - 1) if b > 0 else None,
                       part2gen(b + 1) if b + 1 < NBLK else None,
                       part1(b + 2) if b + 2 < NBLK else None]

                def emitA(c):
                    gi = b * NCH + c
                    sl = gi % NBUF
                    ai = c % 2
                    K.op("pe", lambda e: [e.matmul(Apsb[ai][:, 0:TB], lhsT=UT[sl][:, k * 128:(k + 1) * 128], rhs=h2T[b % 3][:, k, :],
                                                   start=(k == 0), stop=(k == 7)) for k in range(8)],
                         reads=[TUT[sl], Th2T[b % 3]], writes=[TAps[ai]])
                    K.op("act", lambda e: e.activation(out=gl[ai][:], in_=Apsb[ai][:, 0:TB], func=AF.Gelu),
                         writes=[Tgl[ai], TAps[ai]])
                    K.op("pool", lambda e: e.tensor_tensor(out=Wb[c % 4][:], in0=gl[ai][:], in1=Gr[sl][:], op=ALU.mult),
                         reads=[Tgl[ai], TGr[sl]], writes=[TW[c % 4]])

                def emitV(c):
                    gi = b * NCH + c
                    sl = gi % NBUF
                    K.op("pe", lambda e: [e.matmul(opsb[2 * j + hf][:], lhsT=Wb[c % 4][:, j * 128:(j + 1) * 128],
                                                   rhs=Vc[sl][:, hf * 512:(hf + 1) * 512], start=(c == 0), stop=(c == NCH - 1))
                                          for j in range(2) for hf in range(2)],
                         reads=[TW[c % 4], TVc[sl]], writes=Tops, acc=(Tops if c > 0 else ()))

                emitA(0)
                emitA(1)
                for c in range(NCH):
                    if c + 2 < NCH:
                        emitA(c + 2)
                    emitV(c)
                    if c == NCH - NBUF:
                        drain(bgs[0]); drain(bgs[1]); bgs[0] = bgs[1] = None
                    load_chunk(b * NCH + c + NBUF - 1)
                    if c == NCH - 24:
                        K.dma("sp", lambda e: e.dma_start(out=xe[:], in_=x1d[tok0:tok0 + TB, :].rearrange("(j p) d -> p j d", p=128)),
                              reads=[Tx1d[b]], writes=[Txe])
                    if c >= 2:
                        budget = 2.0
                        while budget > 0:
                            gidx = next((i for i in range(3) if bgs[i] is not None), None)
                            if gidx is None:
                                break
                            try:
                                budget -= next(bgs[gidx])
                            except StopIteration:
                                bgs[gidx] = None
                for g in bgs:
                    drain(g)
                chk(53)
                chk(70 + b)
                chk(54)
            epi_adds(NBLK - 1)
            for _ in epi_rest(NBLK - 1):
                pass
            K.barrier()
    return nc


def _prep_weights(inputs):
    f = lambda a: np.ascontiguousarray(np.asarray(a, dtype=np.float32))
    L = 0
    eu = np.asarray(inputs["expert_u"], dtype=np.float32)[L]
    uT = np.ascontiguousarray(eu.reshape(NCH, 128, 8, 128).transpose(0, 3, 2, 1)).reshape(NEXP, D)
    sk = np.asarray(inputs["sub_keys"], dtype=np.float32)[L]
    keysT = np.ascontiguousarray(sk.reshape(16, 128, 128).transpose(2, 0, 1))
    gwt = np.ascontiguousarray(np.asarray(inputs["pool_group_w"], dtype=np.float32)[L].transpose(1, 0, 2))
    pscale = np.ascontiguousarray(np.asarray(inputs["pool_scale"], dtype=np.float32)[L].reshape(4, 128).T)
    cwt = np.ascontiguousarray(np.asarray(inputs["conv_w"], dtype=np.float32)[L].reshape(3, 4, 128).transpose(2, 1, 0))
    return dict(
        w_in=f(inputs["w_in"][L]), w_bp=f(inputs["w_branch_pool"][L]), w_bc=f(inputs["w_branch_conv"][L]),
        w_out=f(inputs["w_out"][L]), w_q=f(inputs["w_q"][L]), gw=gwt, pscale=pscale, cw=cwt,
        gmix=f(inputs["g_mix"][L]), gffn=f(inputs["g_ffn"][L]), gfin=f(inputs["g_final"]),
        keysT=keysT, uT=uT, v=f(np.asarray(inputs["expert_v"])[L]),
    )


def run(inputs, ncores, ntok, seq, phase='AB'):
    x = np.asarray(inputs["x"], dtype=np.float32)
    B, S_, _ = x.shape
    xf = np.ascontiguousarray(x.reshape(B * S_, D))
    assert B * S_ == ncores * ntok
    wts = _prep_weights(inputs)
    nc = build(ntok, seq, phase)
    in_maps = []
    for c in range(ncores):
        m = dict(wts)
        m["x"] = xf[c * ntok:(c + 1) * ntok]
        in_maps.append(m)
    res = run_bass_kernel_spmd(nc, in_maps, core_ids=list(range(ncores)))
    out = np.concatenate([np.asarray(r["y"]) for r in res.results], axis=0)
    return out.reshape(B, S_, D).astype(np.float32)


def kernel(**inputs):
    return run(inputs, 8, 8192, 4096)
```

```python
import numpy as np
from contextlib import ExitStack
import concourse.bass as bass
import concourse.mybir as mybir
from concourse.bass_utils import run_bass_kernel_spmd

F32 = mybir.dt.float32
BF16 = mybir.dt.bfloat16
U32 = mybir.dt.uint32
ALU = mybir.AluOpType
AF = mybir.ActivationFunctionType
AX = mybir.AxisListType

D = 1024
DIN = 4096
NEXP = 16384
NCH = 128
TB = 256
EPS = 1e-6
NBUF = 6
NSB = 8


class T:
    __slots__ = ("name", "w", "r", "dsem")

    def __init__(self, name):
        self.name = name
        self.w = None
        self.r = {}
        self.dsem = None


class Sem:
    __slots__ = ("h", "n")

    def __init__(self, h):
        self.h = h
        self.n = 0


import os
class _Stop(Exception):
    pass


_STOP = [False]


def chk(n):
    if int(os.environ.get('KSTAGE', '0')) == n:
        _STOP[0] = True


class Ctx:
    def __init__(self, nc, es):
        self.nc, self.es = nc, es
        self.E = dict(pe=nc.tensor, act=nc.scalar, dve=nc.vector, pool=nc.gpsimd, sp=nc.sync)
        self.allsems = []
        self.esem = {k: self.newsem("e_" + k) for k in ("pe", "act", "dve", "pool")}
        self.seen = {k: {} for k in self.E}

    def newsem(self, name):
        s = Sem(self.es.enter_context(self.nc.semaphore(name)))
        self.allsems.append(s)
        return s

    def _wait(self, eng, need):
        seen = self.seen[eng]
        for s, v in need.items():
            if seen.get(s, 0) < v:
                self.E[eng].wait_ge(s.h, v)
                seen[s] = v

    def _need(self, eng, reads, writes, acc, skip=None):
        need = {}

        def add(s, v):
            if s is skip:
                return
            if need.get(s, 0) < v:
                need[s] = v
        me = self.esem.get(eng)
        for t in reads:
            if t.w is not None:
                add(*t.w)
        for t in writes:
            if t.w is not None and t.w[0] is not me:
                add(*t.w)
            for s, v in t.r.items():
                if s is not me:
                    add(s, v)
        return need

    def _mark(self, s, v, reads, writes):
        for t in reads:
            if t.r.get(s, 0) < v:
                t.r[s] = v
        for t in writes:
            t.w = (s, v)
            t.r = {}

    def op(self, eng, fn, reads=(), writes=(), acc=()):
        if _STOP[0]:
            return
        self._wait(eng, self._need(eng, reads, writes, acc))
        inst = fn(self.E[eng])
        if isinstance(inst, (list, tuple)):
            inst = inst[-1]
        s = self.esem[eng]
        s.n += 1
        inst.then_inc(s.h, 1)
        self._mark(s, s.n, reads, writes)

    def dma(self, eng, fn, reads=(), writes=(), sem_of=None, group=False):
        if _STOP[0]:
            return
        t = sem_of if sem_of is not None else (writes[0] if writes else reads[0])
        if t.dsem is None:
            t.dsem = self.newsem("d_" + t.name)
        self._wait(eng, self._need(eng, reads, writes, (), skip=(t.dsem if group else None)))
        inst = fn(self.E[eng])
        t.dsem.n += 16
        inst.then_inc(t.dsem.h, 16)
        self._mark(t.dsem, t.dsem.n, reads, writes)

    def barrier(self, engs=("pe", "act", "dve", "pool", "sp")):
        for e in engs:
            self._wait(e, {s: s.n for s in self.allsems if s.n > 0})


def build(NTOK, SEQ, phase='AB'):
    _STOP[0] = False
    nc = bass.Bass("TRN2", target_bir_lowering=False)
    NBLK = NTOK // TB
    BPS = SEQ // TB

    def din(name, shape, dt=F32):
        return nc.dram_tensor(name, shape, dt, kind="ExternalInput").ap()

    x = din("x", [NTOK, D])
    w_in = din("w_in", [D, DIN])
    w_bp = din("w_bp", [512, D])
    w_bc = din("w_bc", [512, D])
    w_out = din("w_out", [D, D])
    w_q = din("w_q", [D, 2048])
    gw = din("gw", [128, 4, 128])
    pscale_d = din("pscale", [128, 4])
    cw_d = din("cw", [128, 4, 3])
    gmix_d = din("gmix", [D])
    gffn_d = din("gffn", [D])
    gfin_d = din("gfin", [D])
    keysT = din("keysT", [128, 16, 128])
    uT = din("uT", [NEXP, D])
    vv = din("v", [NEXP, D])
    y = nc.dram_tensor("y", [NTOK, D], F32, kind="ExternalOutput").ap()
    x1d = nc.dram_tensor("x1d", [NTOK, D], F32, kind="Internal").ap()
    uTb = nc.dram_tensor("uTb", [NEXP, D], BF16, kind="Internal").ap()
    vb = nc.dram_tensor("vb", [NEXP, D], BF16, kind="Internal").ap()
    wqb = nc.dram_tensor("wqb", [D, 2048], BF16, kind="Internal").ap()

    with ExitStack() as es:
        K = Ctx(nc, es)

        def sbt(st, name, shape, dt):
            return st.enter_context(nc.sbuf_tensor(name, shape, dt))

        def pst(st, name, shape, dt):
            return st.enter_context(nc.psum_tensor(name, shape, dt))

        io128 = sbt(es, "io128", [128, 128], F32); Tio = T("io128")
        pidx = sbt(es, "pidx", [128, 1], F32); Tpidx = T("pidx")
        idf = sbt(es, "idf", [128, 128], F32); Tidf = T("idf")
        idb = sbt(es, "idb", [128, 128], BF16); Tidb = T("idb")
        K.op("pool", lambda e: e.iota(io128[:], pattern=[[1, 128]], base=0, channel_multiplier=0,
                                      allow_small_or_imprecise_dtypes=True), writes=[Tio])
        K.op("pool", lambda e: e.iota(pidx[:], pattern=[[0, 1]], base=0, channel_multiplier=1,
                                      allow_small_or_imprecise_dtypes=True), writes=[Tpidx])
        K.op("dve", lambda e: e.tensor_scalar(out=idf[:], in0=io128[:], scalar1=pidx[:, 0:1], scalar2=None,
                                              op0=ALU.is_equal), reads=[Tio, Tpidx], writes=[Tidf])
        K.op("dve", lambda e: e.tensor_copy(out=idb[:], in_=idf[:]), reads=[Tidf], writes=[Tidb])

        Texp = T("experts")
        Tx1d = [T("x1d%d" % b) for b in range(NBLK)]
        Tst = [T("st0"), T("st1")]

        with ExitStack() as ea:
            w_in_bf = sbt(ea, "w_in_bf", [128, 8, DIN], BF16); Twin = T("w_in")
            w_bp_bf = sbt(ea, "w_bp_bf", [128, 4, D], BF16); Twbp = T("w_bp")
            w_bc_bf = sbt(ea, "w_bc_bf", [128, 4, D], BF16); Twbc = T("w_bc")
            w_out_bf = sbt(ea, "w_out_bf", [128, 8, D], BF16); Twout = T("w_out")
            gw_bf = sbt(ea, "gw_bf", [128, 4, 128], BF16); Tgw = T("gw")
            pscale = sbt(ea, "pscale_s", [128, 4], F32); Tpsc = T("pscale")
            cw = sbt(ea, "cw_s", [128, 4, 3], F32); Tcw = T("cw")
            gmix = sbt(ea, "gmix_s", [128, D], F32); Tgmix = T("gmix")
            invcnt = sbt(ea, "invcnt", [128, 4, 16], F32); Tinv = T("invcnt")
            io16p = sbt(ea, "io16p", [128, 16], F32); Tio16p = T("io16p")
            xblk = [sbt(ea, "xblk%d" % i, [128, 2, D], F32) for i in range(2)]
            Txblk = [T("xblk0"), T("xblk1")]
            junk = sbt(ea, "junkA", [128, D], BF16); Tjunk = T("junk")
            ss = sbt(ea, "ssA", [128, 2], F32); Tss = T("ss")
            sd = sbt(ea, "sdA", [128, 2], F32); Tsd = T("sd")
            rs = sbt(ea, "rsA", [128, 2], F32); Trs = T("rs")
            hbf = sbt(ea, "hbf", [128, 2, D], BF16); Thbf = T("hbf")
            hT = sbt(ea, "hT", [128, 8, TB], BF16); ThT = T("hT")
            zp = [sbt(ea, "zp%d" % i, [128, 4, 16 + TB], F32) for i in range(2)]
            Tzp = [T("zp0"), T("zp1")]
            sA = sbt(ea, "sA", [128, 4, 16 + TB], F32); TsA = T("sA")
            sB = sbt(ea, "sB", [128, 4, 16 + TB], F32); TsB = T("sB")
            tmpc = sbt(ea, "tmpc", [128, 4, 16], F32); Ttmpc = T("tmpc")
            zc = sbt(ea, "zc", [128, 12, TB], F32); Tzc = T("zc")
            ub = [sbt(ea, "ub%d" % i, [128, 4, 2 + TB], F32) for i in range(2)]
            Tub = [T("ub0"), T("ub1")]
            yb = sbt(ea, "yb", [128, 4, TB], F32); Tyb = T("yb")
            sig = sbt(ea, "sig", [128, 16, TB], F32); Tsig = T("sig")
            pbf = sbt(ea, "pbf", [128, 4, TB], BF16); Tpbf = T("pbf")
            pmbf = sbt(ea, "pmbf", [128, 4, TB], BF16); Tpmbf = T("pmbf")
            cvbf = sbt(ea, "cvbf", [128, 4, TB], BF16); Tcvbf = T("cvbf")
            t12 = sbt(ea, "t12", [128, 2, TB], F32); Tt12 = T("t12")
            mbf = sbt(ea, "mbf", [128, 8, TB], BF16); Tmbf = T("mbf")
            tps = pst(ea, "tpsA", [128, 8, 128], BF16); Ttps = T("tps")
            PBA = [pst(ea, "pbA%d" % i, [128, 512], F32) for i in range(7)]
            TPBA = [T("pbA%d" % i) for i in range(7)]
            zpsb = [PBA[0], PBA[1]]; Tzps = [TPBA[0], TPBA[1]]
            gpsb = [PBA[2], PBA[3]]; Tgpsb = [TPBA[2], TPBA[3]]
            abps = [PBA[4], PBA[5]]; Tabps = [TPBA[4], TPBA[5]]
            xops = [PBA[6], PBA[2]]; Txops = [TPBA[6], TPBA[2]]
            gpsv = lambda g: gpsb[g // 2][:, (g % 2) * TB:(g % 2 + 1) * TB]

            for k in range(8):
                for pc in range(4):
                    K.dma("pool", lambda e, k=k, pc=pc: e.dma_start(
                        out=w_in_bf[:, k, pc * 1024:(pc + 1) * 1024],
                        in_=w_in[k * 128:(k + 1) * 128, pc * 1024:(pc + 1) * 1024]), writes=[Twin], group=True)
            K.dma("pool", lambda e: e.dma_start(out=gw_bf[:], in_=gw), writes=[Tgw])
            for g in range(4):
                K.dma("pool", lambda e, g=g: e.dma_start(out=w_bp_bf[:, g, :], in_=w_bp[g * 128:(g + 1) * 128, :]),
                      writes=[Twbp], group=True)
                K.dma("pool", lambda e, g=g: e.dma_start(out=w_bc_bf[:, g, :], in_=w_bc[g * 128:(g + 1) * 128, :]),
                      writes=[Twbc], group=True)
            for k in range(8):
                K.dma("pool", lambda e, k=k: e.dma_start(out=w_out_bf[:, k, :], in_=w_out[k * 128:(k + 1) * 128, :]),
                      writes=[Twout], group=True)
            K.dma("sp", lambda e: e.dma_start(out=pscale[:], in_=pscale_d), writes=[Tpsc])
            K.dma("sp", lambda e: e.dma_start(out=cw[:], in_=cw_d), writes=[Tcw])
            K.dma("sp", lambda e: e.dma_start(out=gmix[:], in_=gmix_d.partition_broadcast(128)), writes=[Tgmix])
            NPIECE = 8
            RP = NEXP // NPIECE
            for pc in range(NPIECE if 'B' in phase else 0):
                K.dma("pool", lambda e, pc=pc: e.dma_start(out=uTb[pc * RP:(pc + 1) * RP, :],
                                                           in_=uT[pc * RP:(pc + 1) * RP, :]),
                      writes=[Texp], group=True)
                K.dma("pool", lambda e, pc=pc: e.dma_start(out=vb[pc * RP:(pc + 1) * RP, :],
                                                           in_=vv[pc * RP:(pc + 1) * RP, :]),
                      writes=[Texp], group=True)
            K.op("dve", lambda e: e.tensor_scalar(out=io16p[:], in0=io128[:, 0:16], scalar1=1.0, scalar2=None,
                                                  op0=ALU.add), reads=[Tio], writes=[Tio16p])
            K.op("dve", lambda e: [e.tensor_scalar(out=invcnt[:, g, :], in0=io16p[:], scalar1=float(2 ** (g + 1)),
                                                   scalar2=None, op0=ALU.min) for g in range(4)],
                 reads=[Tio16p], writes=[Tinv])
            K.op("dve", lambda e: e.reciprocal(out=invcnt[:], in_=invcnt[:]), reads=[Tinv], writes=[Tinv])

            def headA_load(b):
                par = b % 2
                tok0 = b * TB
                K.dma("sp", lambda e: e.dma_start(out=xblk[par][:], in_=x[tok0:tok0 + TB, :].rearrange("(j p) d -> p j d", p=128)),
                      writes=[Txblk[par]])

            def headA(b):
                par = b % 2
                X = xblk[par]
                TX = Txblk[par]
                for j in range(2):
                    K.op("act", lambda e, j=j: e.activation(out=junk[:], in_=X[:, j, :], func=AF.Square,
                                                            accum_out=ss[:, j:j + 1]),
                         reads=[TX], writes=[Tjunk, Tss])
                K.op("act", lambda e: e.activation(out=sd[:], in_=ss[:], func=AF.Sqrt, scale=1.0 / D, bias=EPS),
                     reads=[Tss], writes=[Tsd])
                K.op("dve", lambda e: e.reciprocal(out=rs[:], in_=sd[:]), reads=[Tsd], writes=[Trs])
                K.op("dve", lambda e: [e.scalar_tensor_tensor(out=hbf[:, j, :], in0=X[:, j, :], scalar=rs[:, j:j + 1],
                                                              in1=gmix[:], op0=ALU.mult, op1=ALU.mult)
                                       for j in range(2)],
                     reads=[TX, Trs, Tgmix], writes=[Thbf])
            chk(1)
            for b in range(NBLK):
                par = b % 2
                first = (b % BPS == 0)
                tok0 = b * TB
                X = xblk[par]
                TX = Txblk[par]
                Z = zp[par]
                TZ = Tzp[par]
                U = ub[par]
                TU = Tub[par]
                if b == 0:
                    headA_load(0)
                    headA(0)
                if b + 1 < NBLK:
                    headA_load(b + 1)
                chk(2)
                for j in range(2):
                    K.op("pe", lambda e, j=j: [e.transpose(out=tps[:, k, :], in_=hbf[:, j, k * 128:(k + 1) * 128],
                                                           identity=idb[:]) for k in range(8)],
                         reads=[Thbf, Tidb], writes=[Ttps])
                    if j == 0:
                        K.op("act", lambda e, j=j: e.activation(out=hT[:, :, j * 128:(j + 1) * 128], in_=tps[:],
                                                                func=AF.Copy), writes=[ThT, Ttps])
                    else:
                        K.op("dve", lambda e, j=j: e.tensor_copy(out=hT[:, :, j * 128:(j + 1) * 128], in_=tps[:]),
                             writes=[ThT, Ttps])
                chk(3)
                if first:
                    K.op("dve", lambda e: [e.memset(Z[:, :, 0:16], 0.0), e.memset(U[:, :, 0:2], 0.0)],
                         writes=[TZ, TU])
                else:
                    K.op("dve", lambda e: [e.tensor_copy(out=Z[:, :, 0:16], in_=zp[1 - par][:, :, TB:TB + 16]),
                                           e.tensor_copy(out=U[:, :, 0:2], in_=ub[1 - par][:, :, TB:TB + 2])],
                         reads=[Tzp[1 - par], Tub[1 - par]], writes=[TZ, TU])
                chk(31)
                for oc in range(32):
                    zi = oc % 2
                    K.op("pe", lambda e, oc=oc, zi=zi: [e.matmul(zpsb[zi][:, 0:TB], lhsT=w_in_bf[:, k, oc * 128:(oc + 1) * 128],
                                                                 rhs=hT[:, k, :], start=(k == 0), stop=(k == 7))
                                                        for k in range(8)],
                         reads=[ThT, Twin], writes=[Tzps[zi]])
                    if oc == 0:
                        chk(321)
                    if oc < 4:
                        K.op("act", lambda e, oc=oc, zi=zi: e.activation(out=Z[:, oc, 16:16 + TB], in_=zpsb[zi][:, 0:TB],
                                                                         func=AF.Copy), writes=[TZ, Tzps[zi]])
                    elif oc < 16:
                        K.op("act", lambda e, oc=oc, zi=zi: e.activation(out=zc[:, oc - 4, :], in_=zpsb[zi][:, 0:TB],
                                                                         func=AF.Copy), writes=[Tzc, Tzps[zi]])
                    else:
                        K.op("act", lambda e, oc=oc, zi=zi: e.activation(out=sig[:, oc - 16, :], in_=zpsb[zi][:, 0:TB],
                                                                         func=AF.Sigmoid), writes=[Tsig, Tzps[zi]])
                    if oc == 2:
                        chk(32)
                    if oc == 0:
                        chk(322)
                    if oc == 3:
                        W1 = 16 + TB
                        K.op("dve", lambda e: e.tensor_tensor(out=sA[:, :, 1:W1], in0=Z[:, :, 1:W1], in1=Z[:, :, 0:W1 - 1],
                                                              op=ALU.add), reads=[TZ], writes=[TsA])
                        K.op("dve", lambda e: [
                            e.scalar_tensor_tensor(out=pbf[:, 0, :], in0=sA[:, 0, 16:W1], scalar=0.5, in1=Z[:, 0, 16:W1],
                                                   op0=ALU.mult, op1=ALU.subtract),
                            e.tensor_tensor(out=sB[:, 1:4, 3:W1], in0=sA[:, 1:4, 3:W1], in1=sA[:, 1:4, 1:W1 - 2],
                                            op=ALU.add)],
                             reads=[TsA, TZ], writes=[Tpbf, TsB])
                        K.op("dve", lambda e: [
                            e.scalar_tensor_tensor(out=pbf[:, 1, :], in0=sB[:, 1, 16:W1], scalar=0.25, in1=Z[:, 1, 16:W1],
                                                   op0=ALU.mult, op1=ALU.subtract),
                            e.tensor_tensor(out=sA[:, 2:4, 7:W1], in0=sB[:, 2:4, 7:W1], in1=sB[:, 2:4, 3:W1 - 4],
                                            op=ALU.add)],
                             reads=[TsB, TZ], writes=[Tpbf, TsA])
                        K.op("dve", lambda e: [
                            e.scalar_tensor_tensor(out=pbf[:, 2, :], in0=sA[:, 2, 16:W1], scalar=0.125, in1=Z[:, 2, 16:W1],
                                                   op0=ALU.mult, op1=ALU.subtract),
                            e.tensor_tensor(out=sB[:, 3, 15:W1], in0=sA[:, 3, 15:W1], in1=sA[:, 3, 7:W1 - 8],
                                            op=ALU.add)],
                             reads=[TsA, TZ], writes=[Tpbf, TsB])
                        K.op("dve", lambda e: e.scalar_tensor_tensor(out=pbf[:, 3, :], in0=sB[:, 3, 16:W1], scalar=1.0 / 16,
                                                                     in1=Z[:, 3, 16:W1], op0=ALU.mult, op1=ALU.subtract),
                             reads=[TsB, TZ], writes=[Tpbf])
                        if first:
                            srcs = [sA[:, 0, 16:32], sB[:, 1, 16:32], sA[:, 2, 16:32], sB[:, 3, 16:32]]
                            K.op("dve", lambda e: [e.tensor_tensor(out=tmpc[:, g, :], in0=srcs[g], in1=invcnt[:, g, :],
                                                                   op=ALU.mult) for g in range(4)],
                                 reads=[TsA, TsB, Tinv], writes=[Ttmpc])
                            K.op("dve", lambda e: e.tensor_tensor(out=pbf[:, :, 0:16], in0=tmpc[:], in1=Z[:, :, 16:32],
                                                                  op=ALU.subtract), reads=[Ttmpc, TZ], writes=[Tpbf])
                    if oc == 9:
                        K.op("pe", lambda e: [e.matmul(gpsv(g), lhsT=gw_bf[:, g, :], rhs=pbf[:, g, :], start=True,
                                                       stop=True) for g in range(4)],
                             reads=[Tpbf, Tgw], writes=Tgpsb)
                        K.op("dve", lambda e: [e.tensor_scalar(out=pmbf[:, g, :], in0=gpsv(g), scalar1=pscale[:, g:g + 1],
                                                               scalar2=None, op0=ALU.mult) for g in range(4)],
                             reads=[Tpsc], writes=[Tpmbf] + Tgpsb)
                    if oc == 4:
                        chk(34)
                    if oc == 15:
                        chk(35)
                        K.op("dve", lambda e: e.tensor_tensor(out=U[:, :, 2:2 + TB], in0=zc[:, 0:4, :], in1=zc[:, 8:12, :],
                                                              op=ALU.mult), reads=[Tzc], writes=[TU])
                        K.op("dve", lambda e: [e.tensor_scalar(out=yb[:, ch, :], in0=U[:, ch, 2:2 + TB],
                                                               scalar1=cw[:, ch, 2:3], scalar2=None, op0=ALU.mult)
                                               for ch in range(4)], reads=[TU, Tcw], writes=[Tyb])
                        for kk in (1, 0):
                            K.op("dve", lambda e, kk=kk: [e.scalar_tensor_tensor(out=yb[:, ch, :], in0=U[:, ch, kk:kk + TB],
                                                                                 scalar=cw[:, ch, kk:kk + 1], in1=yb[:, ch, :],
                                                                                 op0=ALU.mult, op1=ALU.add)
                                                          for ch in range(4)], reads=[TU, Tcw, Tyb], writes=[Tyb])
                        K.op("dve", lambda e: e.tensor_tensor(out=cvbf[:], in0=yb[:], in1=zc[:, 4:8, :], op=ALU.mult),
                             reads=[Tyb, Tzc], writes=[Tcvbf])
                chk(4)
                for dc in range(8):
                    ab = abps[dc % 2]
                    Tab = Tabps[dc % 2]
                    K.op("pe", lambda e, dc=dc, ab=ab: (
                        [e.matmul(ab[:, 0:TB], lhsT=w_bp_bf[:, g, dc * 128:(dc + 1) * 128], rhs=pmbf[:, g, :],
                                  start=(g == 0), stop=(g == 3)) for g in range(4)] +
                        [e.matmul(ab[:, TB:2 * TB], lhsT=w_bc_bf[:, g, dc * 128:(dc + 1) * 128], rhs=cvbf[:, g, :],
                                  start=(g == 0), stop=(g == 3)) for g in range(4)]),
                         reads=[Tpmbf, Tcvbf, Twbp, Twbc], writes=[Tab])
                    K.op("dve", lambda e, dc=dc, ab=ab: [
                        e.tensor_tensor(out=t12[:, 0, :], in0=sig[:, dc, :], in1=ab[:, 0:TB], op=ALU.mult),
                        e.tensor_tensor(out=t12[:, 1, :], in0=sig[:, 8 + dc, :], in1=ab[:, TB:2 * TB], op=ALU.mult)],
                         reads=[Tsig], writes=[Tt12, Tab])
                    K.op("dve", lambda e, dc=dc: e.tensor_tensor(out=mbf[:, dc, :], in0=t12[:, 0, :], in1=t12[:, 1, :],
                                                                 op=ALU.add), reads=[Tt12], writes=[Tmbf])
                chk(5)
                if b + 1 < NBLK:
                    headA(b + 1)
                for j in range(2):
                    for hf in range(2):
                        xi = (2 * j + hf) % 2
                        K.op("pe", lambda e, j=j, hf=hf, xi=xi: [
                            e.matmul(xops[xi][:], lhsT=mbf[:, k, j * 128:(j + 1) * 128],
                                     rhs=w_out_bf[:, k, hf * 512:(hf + 1) * 512], start=(k == 0), stop=(k == 7))
                            for k in range(8)], reads=[Tmbf, Twout], writes=[Txops[xi]])
                        K.op("dve", lambda e, j=j, hf=hf, xi=xi: e.tensor_tensor(
                            out=X[:, j, hf * 512:(hf + 1) * 512], in0=X[:, j, hf * 512:(hf + 1) * 512], in1=xops[xi][:],
                            op=ALU.add), reads=[TX], writes=[TX, Txops[xi]])
                dst = x1d if 'B' in phase else y
                K.dma("sp", lambda e: e.dma_start(out=dst[tok0:tok0 + TB, :].rearrange("(j p) d -> p j d", p=128), in_=X[:]),
                      reads=[TX], writes=[Tx1d[b]], sem_of=Tst[par])
            K.barrier()

        with ExitStack() as eb:
            if 'B' not in phase:
                return nc
            keys_bf = sbt(eb, "keys_bf", [128, 16, 128], BF16); Tkeys = T("keys")
            gffn = sbt(eb, "gffn_s", [128, D], F32); Tgffn = T("gffn")
            gfin = sbt(eb, "gfin_s", [128, D], F32); Tgfin = T("gfin")
            iob = sbt(eb, "iob", [128, 128], BF16); Tiob = T("iob")
            ecst = sbt(eb, "ecst", [128, 128], F32); Tecst = T("ecst")
            nhalf = sbt(eb, "nhalf", [128, 2], F32); Tnhalf = T("nhalf")
            wqr = [sbt(eb, "wqr%d" % i, [128, 8, 256], BF16) for i in range(2)]; Twqr = [T("wqr0"), T("wqr1")]
            xp = sbt(eb, "xp", [128, 2, D], F32); Txp = T("xp")
            xe = sbt(eb, "xe", [128, 2, D], F32); Txe = T("xe")
            ss1 = sbt(eb, "ss1", [128, 2], F32); Tss1 = T("ss1")
            ms1 = sbt(eb, "ms1", [128, 2], F32); Tms1 = T("ms1")
            rs1 = sbt(eb, "rs1", [128, 2], F32); Trs1 = T("rs1")
            ss2 = sbt(eb, "ss2", [128, 2], F32); Tss2 = T("ss2")
            ms2 = sbt(eb, "ms2", [128, 2], F32); Tms2 = T("ms2")
            rs2 = sbt(eb, "rs2", [128, 2], F32); Trs2 = T("rs2")
            h2bf = sbt(eb, "h2bf", [128, 2, D], BF16); Th2bf = T("h2bf")
            h2T = [sbt(eb, "h2T%d" % i, [128, 8, TB], BF16) for i in range(2)]; Th2T = [T("h2T0"), T("h2T1")]
            qT = sbt(eb, "qT", [128, 16, TB], BF16); TqT = T("qT")
            S = sbt(eb, "S", [128, 2048], F32); TS = T("S")
            S2 = sbt(eb, "S2", [128, 2048], F32); TS2 = T("S2")
            cand = sbt(eb, "cand", [128, 2048], F32); Tcand = T("cand")
            V16 = sbt(eb, "V16", [128, 16, 16], F32); TV16 = T("V16")
            IX = sbt(eb, "IX", [128, 16, 16], U32); TIX = T("IX")
            IXf = sbt(eb, "IXf", [128, 16, 16], F32); TIXf = T("IXf")
            F16 = sbt(eb, "F16", [128, 8, 16], F32); TF16 = T("F16")
            PS = sbt(eb, "PS", [128, 8, 16], U32); TPS = T("PS")
            au = sbt(eb, "au", [128, 2, 128], U32); Tau = T("au")
            apf = sbt(eb, "apf", [128, 2, 128], F32); Tapf = T("apf")
            sh = sbt(eb, "sh", [128, 128], F32); Tsh = T("sh")
            ex = sbt(eb, "ex", [128, 128], F32); Tex = T("ex")
            Zs = sbt(eb, "Zs", [128, 8], F32); TZs = T("Zs")
            rZ = sbt(eb, "rZ", [128, 8], F32); TrZ = T("rZ")
            IJG = sbt(eb, "IJG", [128, 3, 128], F32); TIJG = T("IJG")
            IJGT = [sbt(eb, "IJGT%d" % i, [128, 3, TB], F32) for i in range(2)]; TIJGT = [T("IJGT0"), T("IJGT1")]
            Qb = [sbt(eb, "Qb%d" % i, [128, NSB, 128], BF16) for i in range(2)]; TQb = [T("Qb0"), T("Qb1")]
            Pb = [sbt(eb, "Pb%d" % i, [128, NSB, 128], BF16) for i in range(2)]; TPb = [T("Pb0"), T("Pb1")]
            Gbuf = sbt(eb, "Gbuf", [128, NCH, TB], BF16); TG = T("Gbuf")
            negJ = sbt(eb, "negJ", [128, TB], F32); TnegJ = T("negJ")
            tmpA = sbt(eb, "tmpA", [128, 2, 128], F32); TtmpA = T("tmpA")
            TQbA = [T("QbA0"), T("QbA1")]
            UT = [sbt(eb, "UT%d" % i, [128, D], BF16) for i in range(NBUF)]; TUT = [T("UT%d" % i) for i in range(NBUF)]
            Vc = [sbt(eb, "Vc%d" % i, [128, D], BF16) for i in range(NBUF)]; TVc = [T("Vc%d" % i) for i in range(NBUF)]
            gl = [sbt(eb, "gl%d" % i, [128, TB], BF16) for i in range(2)]; Tgl = [T("gl0"), T("gl1")]
            Wb = [sbt(eb, "Wb%d" % i, [128, TB], BF16) for i in range(4)]; TW = [T("W%d" % i) for i in range(4)]
            opsb = [pst(eb, "ops%d" % i, [128, 512], F32) for i in range(4)]; Tops = [T("ops%d" % i) for i in range(4)]
            tpb = pst(eb, "tpsB", [128, 8, 128], BF16); Ttpb = T("tpsB")
            MB = [pst(eb, "MB%d" % i, [128, 512], F32) for i in range(3)]
            TMB = [T("MB0"), T("MB1"), T("MB2")]
            Apsb = [MB[1], MB[2]]; TAps = [TMB[1], TMB[2]]

            Twqb = T("wqb")
            for pc in range(2):
                K.dma("pool", lambda e: e.dma_start(out=wqb[:, pc * 1024:(pc + 1) * 1024], in_=w_q[:, pc * 1024:(pc + 1) * 1024]),
                      writes=[Twqb], group=True)
            K.dma("pool", lambda e: e.dma_start(out=keys_bf[:], in_=keysT), writes=[Tkeys])
            K.dma("sp", lambda e: e.dma_start(out=gffn[:], in_=gffn_d.partition_broadcast(128)), writes=[Tgffn])
            K.dma("sp", lambda e: e.dma_start(out=gfin[:], in_=gfin_d.partition_broadcast(128)), writes=[Tgfin])
            K.op("dve", lambda e: [e.tensor_copy(out=iob[:], in_=io128[:]), e.memset(ecst[:], float(np.e)),
                                   e.memset(nhalf[:], -0.5)], reads=[Tio], writes=[Tiob, Tecst, Tnhalf])
            wqb3 = wqb.rearrange("(k p) n -> p k n", p=128)

            uTb3 = uTb.rearrange("(c p) n -> c p n", p=128)
            vb3 = vb.rearrange("(c p) n -> c p n", p=128)
            NG = NBLK * NCH

            def load_chunk(gi):
                if gi >= NG:
                    return
                c = gi % NCH
                sl = gi % NBUF
                K.dma("sp", lambda e: e.dma_start(out=UT[sl][:], in_=uTb3[c]), reads=[Texp], writes=[TUT[sl]])
                K.dma("sp", lambda e: e.dma_start(out=Vc[sl][:], in_=vb3[c]), reads=[Texp], writes=[TVc[sl]])

            def rstd(ssx, Tssx, msx, Tmsx, rsx, Trsx):
                K.op("pool", lambda e: e.tensor_scalar(out=msx[:], in0=ssx[:], scalar1=1.0 / D, scalar2=EPS,
                                                       op0=ALU.mult, op1=ALU.add), reads=[Tssx], writes=[Tmsx])
                K.op("pool", lambda e: e.tensor_tensor(out=rsx[:], in0=msx[:], in1=nhalf[:], op=ALU.pow),
                     reads=[Tmsx, Tnhalf], writes=[Trsx])

            def load_wq(qp):
                K.dma("sp", lambda e: e.dma_start(out=wqr[qp % 2][:], in_=wqb3[:, :, qp * 256:(qp + 1) * 256]),
                      reads=[Twqb], writes=[Twqr[qp % 2]])

            def prefetch_p1(b):
                if b >= NBLK:
                    return
                t0_ = b * TB
                K.dma("sp", lambda e: e.dma_start(out=xp[:], in_=x1d[t0_:t0_ + TB, :].rearrange("(j p) d -> p j d", p=128)),
                      reads=[Tx1d[b]], writes=[Txp])
                load_wq(0)
                load_wq(1)

            def part1(b):
                pb = b % 2
                tok0 = b * TB
                H2T = h2T[pb]
                TH2T = Th2T[pb]
                if b == 0:
                    prefetch_p1(0)
                for j in range(2):
                    K.op("act", lambda e: e.activation(out=h2bf[:, j, :], in_=xp[:, j, :], func=AF.Square,
                                                       accum_out=ss1[:, j:j + 1]), reads=[Txp], writes=[Th2bf, Tss1])
                    yield 0.1
                rstd(ss1, Tss1, ms1, Tms1, rs1, Trs1)
                yield 0.1
                for j in range(2):
                    for hf in range(2):
                        cs = slice(hf * 512, (hf + 1) * 512)
                        K.op("dve", lambda e: e.scalar_tensor_tensor(out=h2bf[:, j, cs], in0=xp[:, j, cs], scalar=rs1[:, j:j + 1],
                                                                     in1=gffn[:, cs], op0=ALU.mult, op1=ALU.mult),
                             reads=[Txp, Trs1, Tgffn], writes=[Th2bf])
                        yield 0.6
                yield 3.0
                for j in range(2):
                    K.op("pe", lambda e: [e.transpose(out=tpb[:, k, :], in_=h2bf[:, j, k * 128:(k + 1) * 128],
                                                      identity=idb[:]) for k in range(8)],
                         reads=[Th2bf, Tidb], writes=[Ttpb])
                    K.op("act", lambda e: e.activation(out=H2T[:, :, j * 128:(j + 1) * 128], in_=tpb[:], func=AF.Copy),
                         writes=[TH2T, Ttpb])
                    yield 0.3
                for qp in range(8):
                    wr = wqr[qp % 2]
                    K.op("pe", lambda e: [e.matmul(MB[0][:, hh * 256:(hh + 1) * 256], lhsT=wr[:, k, hh * 128:(hh + 1) * 128],
                                                   rhs=H2T[:, k, :], start=(k == 0), stop=(k == 7))
                                          for hh in range(2) for k in range(8)],
                         reads=[Twqr[qp % 2], TH2T], writes=[TMB[0]])
                    K.op("act", lambda e: e.activation(out=qT[:, 2 * qp:2 * qp + 2, :],
                                                       in_=MB[0][:].rearrange("p (a t) -> p a t", a=2), func=AF.Copy),
                         writes=[TqT, TMB[0]])
                    if qp + 2 < 8:
                        load_wq(qp + 2)
                    yield 0.4
                prefetch_p1(b + 1)
                Sg = lambda g: S[:, g * 128:(g + 1) * 128]
                S2g = lambda g: S2[:, g * 128:(g + 1) * 128]
                Cg = lambda h: cand[:, h * 256:(h + 1) * 256]
                C2g = lambda h: S[:, h * 256:(h + 1) * 256]
                for j in range(2):
                    for pc in range(4):
                        K.op("pe", lambda e: [e.matmul(MB[0][:, i * 128:(i + 1) * 128], lhsT=qT[:, pc * 4 + i, j * 128:(j + 1) * 128],
                                                       rhs=keys_bf[:, pc * 4 + i, :], start=True, stop=True) for i in range(4)],
                             reads=[TqT, Tkeys], writes=[TMB[0]])
                        K.op("act", lambda e: e.activation(out=S[:, pc * 512:(pc + 1) * 512], in_=MB[0][:], func=AF.Copy),
                             writes=[TS, TMB[0]])
                        yield 0.4
                    for gq in range(8):
                        gs = range(2 * gq, 2 * gq + 2)
                        K.op("dve", lambda e: [e.max(out=V16[:, g, 0:8], in_=Sg(g)) for g in gs], reads=[TS], writes=[TV16])
                        yield 0.5
                        K.op("dve", lambda e: [e.max_index(out=IX[:, g, 0:8], in_max=V16[:, g, 0:8], in_values=Sg(g)) for g in gs] +
                             [e.match_replace(out=S2g(g), in_to_replace=V16[:, g, 0:8], in_values=Sg(g), imm_value=-1e30) for g in gs],
                             reads=[TS, TV16], writes=[TIX, TS2])
                        yield 1.0
                        K.op("dve", lambda e: [e.max(out=V16[:, g, 8:16], in_=S2g(g)) for g in gs], reads=[TS2], writes=[TV16])
                        yield 0.5
                        K.op("dve", lambda e: [e.max_index(out=IX[:, g, 8:16], in_max=V16[:, g, 8:16], in_values=S2g(g)) for g in gs],
                             reads=[TS2, TV16], writes=[TIX])
                        yield 0.5
                    V16r = V16[:].rearrange("p (h two) k -> p h two k", two=2)
                    cand4 = cand[:].rearrange("p (h a b) -> p h a b", h=8, a=16)
                    for hq in range(8):
                        hs = slice(hq, hq + 1)
                        K.op("dve", lambda e: e.tensor_tensor(out=cand4[:, hs], in0=V16r[:, hs, 0, :].unsqueeze(3).to_broadcast([128, 1, 16, 16]),
                                                              in1=V16r[:, hs, 1, :].unsqueeze(2).to_broadcast([128, 1, 16, 16]),
                                                              op=ALU.add), reads=[TV16], writes=[Tcand])
                        yield 0.55
                    for hq in range(8):
                        hs = range(hq, hq + 1)
                        K.op("dve", lambda e: [e.max(out=F16[:, h, 0:8], in_=Cg(h)) for h in hs], reads=[Tcand], writes=[TF16])
                        yield 0.5
                        K.op("dve", lambda e: [e.max_index(out=PS[:, h, 0:8], in_max=F16[:, h, 0:8], in_values=Cg(h)) for h in hs] +
                             [e.match_replace(out=C2g(h), in_to_replace=F16[:, h, 0:8], in_values=Cg(h), imm_value=-1e30) for h in hs],
                             reads=[Tcand, TF16], writes=[TPS, TS])
                        yield 1.0
                        K.op("dve", lambda e: [e.max(out=F16[:, h, 8:16], in_=C2g(h)) for h in hs], reads=[TS], writes=[TF16])
                        yield 0.5
                        K.op("dve", lambda e: [e.max_index(out=PS[:, h, 8:16], in_max=F16[:, h, 8:16], in_values=C2g(h)) for h in hs],
                             reads=[TS, TF16], writes=[TPS])
                        yield 0.5
                    sh3 = sh[:].rearrange("p (h k) -> p h k", k=16)
                    ex3 = ex[:].rearrange("p (h k) -> p h k", k=16)
                    K.op("dve", lambda e: e.tensor_tensor(out=sh3, in0=F16[:], in1=F16[:, :, 0:1].to_broadcast([128, 8, 16]),
                                                          op=ALU.subtract), reads=[TF16], writes=[Tsh])
                    yield 0.3
                    yield 1.5
                    yield 1.5
                    K.op("act", lambda e: e.activation(out=ex[:], in_=sh[:], func=AF.Tanh, scale=0.5), reads=[Tsh], writes=[Tex])
                    yield 0.1
                    K.op("dve", lambda e: e.tensor_scalar(out=sh[:], in0=ex[:], scalar1=-1.0, scalar2=1.0, op0=ALU.mult, op1=ALU.add),
                         reads=[Tex], writes=[Tsh])
                    yield 0.2
                    K.op("dve", lambda e: [e.reciprocal(out=sh[:], in_=sh[:])], reads=[Tsh], writes=[Tsh])
                    yield 0.2
                    K.op("dve", lambda e: e.scalar_tensor_tensor(out=ex[:], in0=ex[:], scalar=1.0, in1=sh[:], op0=ALU.add, op1=ALU.mult),
                         reads=[Tex, Tsh], writes=[Tex])
                    yield 0.2
                    PSf = PS[:].rearrange("p h k -> p (h k)")
                    K.op("dve", lambda e: [e.tensor_single_scalar(out=au[:, 0, :], in_=PSf, scalar=4, op=ALU.arith_shift_right),
                                           e.tensor_single_scalar(out=au[:, 1, :], in_=PSf, scalar=15, op=ALU.bitwise_and)],
                         reads=[TPS], writes=[Tau])
                    yield 0.3
                    K.op("dve", lambda e: [e.tensor_copy(out=apf[:], in_=au[:]), e.tensor_copy(out=IXf[:], in_=IX[:])],
                         reads=[Tau, TIX], writes=[Tapf, TIXf])
                    yield 0.5
                    IXr = IXf[:].rearrange("p (h two) k -> p h two k", two=2)
                    eqs = [S2[:].rearrange("p (h k a) -> p h k a", h=8, k=16), cand[:].rearrange("p (h k a) -> p h k a", h=8, k=16)]
                    Teqs = [TS2, Tcand]
                    for wh in range(2):
                        pos3 = apf[:, wh, :].rearrange("p (h k) -> p h k", k=16)
                        for hq in range(8):
                            hs = slice(hq, hq + 1)
                            K.op("dve", lambda e: e.tensor_tensor(
                                out=eqs[wh][:, hs], in0=pos3[:, hs].unsqueeze(3).to_broadcast([128, 1, 16, 16]),
                                in1=io128[:, 0:16].unsqueeze(1).unsqueeze(1).to_broadcast([128, 1, 16, 16]), op=ALU.is_equal),
                                 reads=[Tapf, Tio], writes=[Teqs[wh]])
                            yield 0.55
                        for hq in range(8):
                            hs = slice(hq, hq + 1)
                            K.op("dve", lambda e: e.tensor_tensor(out=eqs[wh][:, hs], in0=eqs[wh][:, hs],
                                                                  in1=IXr[:, hs, wh, :].unsqueeze(2).to_broadcast([128, 1, 16, 16]),
                                                                  op=ALU.mult), reads=[Teqs[wh], TIXf], writes=[Teqs[wh]])
                            yield 0.55
                    for wh in range(2):
                        for hq in range(4):
                            hs = slice(2 * hq, 2 * hq + 2)
                            K.op("dve", lambda e: e.reduce_sum(out=IJG[:, wh, :].rearrange("p (h k) -> p h k", k=16)[:, hs],
                                                               in_=eqs[wh][:, hs], axis=AX.X), reads=[Teqs[wh]], writes=[TIJG])
                            yield 0.55
                        if wh == 0:
                            K.op("dve", lambda e: e.reduce_sum(out=Zs[:], in_=ex3, axis=AX.X), reads=[Tex], writes=[TZs])
                            yield 0.2
                            K.op("dve", lambda e: e.reciprocal(out=rZ[:], in_=Zs[:]), reads=[TZs], writes=[TrZ])
                            yield 0.1
                            K.op("dve", lambda e: e.tensor_tensor(out=IJG[:, 2, :].rearrange("p (h k) -> p h k", k=16), in0=ex3,
                                                                  in1=rZ[:].unsqueeze(2).to_broadcast([128, 8, 16]), op=ALU.mult),
                                 reads=[Tex, TrZ], writes=[TIJG])
                            yield 0.1
                    yield 1.5
                    yield 1.5
                    K.op("pe", lambda e: [e.transpose(out=MB[0][:, w * 128:(w + 1) * 128], in_=IJG[:, w, :], identity=idf[:])
                                          for w in range(3)], reads=[TIJG, Tidf], writes=[TMB[0]])
                    K.op("act", lambda e: e.activation(out=IJGT[pb][:, :, j * 128:(j + 1) * 128],
                                                       in_=MB[0][:, 0:384].rearrange("p (w t) -> p w t", w=3),
                                                       func=AF.Copy), writes=[TIJGT[pb], TMB[0]])
                    yield 0.3

            def part2(b):
                pb = b % 2
                IT = IJGT[pb]
                NA = 2
                K.op("dve", lambda e: e.tensor_scalar(out=negJ[:], in0=IT[:, 1, :], scalar1=-1.0, scalar2=None, op0=ALU.mult),
                     reads=[TIJGT[pb]], writes=[TnegJ])
                for sbi in range(TB // NSB):
                    sl = sbi % 2
                    t0 = sbi * NSB
                    K.op("act", lambda e: [e.activation(out=tmpA[:, i, :], in_=io128[:], func=AF.Abs,
                                                        bias=negJ[:, t0 + i:t0 + i + 1], scale=1.0) for i in range(NA)],
                         reads=[Tio, TnegJ], writes=[TtmpA])
                    K.op("act", lambda e: [e.activation(out=Qb[sl][:, i, :], in_=tmpA[:, i, :], func=AF.Relu, scale=-1.0, bias=1.0)
                                           for i in range(NA)], reads=[TtmpA], writes=[TQbA[sl]])
                    K.op("dve", lambda e: [e.tensor_scalar(out=Qb[sl][:, i, :], in0=iob[:], scalar1=IT[:, 1, t0 + i:t0 + i + 1],
                                                           scalar2=None, op0=ALU.is_equal) for i in range(NA, NSB)] +
                         [e.tensor_scalar(out=Pb[sl][:, i, :], in0=iob[:], scalar1=IT[:, 0, t0 + i:t0 + i + 1],
                                          scalar2=IT[:, 2, t0 + i:t0 + i + 1], op0=ALU.is_equal, op1=ALU.mult)
                          for i in range(NSB)],
                         reads=[Tiob, TIJGT[pb]], writes=[TQb[sl], TPb[sl]])
                    for q4 in range(NSB // 4):
                        cnt = sbi * (NSB // 4) + q4
                        mi = cnt % 3
                        tt = t0 + q4 * 4
                        K.op("pe", lambda e: [e.matmul(MB[mi][:, i * 128:(i + 1) * 128], lhsT=Qb[sl][:, q4 * 4 + i, :],
                                                       rhs=Pb[sl][:, q4 * 4 + i, :], start=True, stop=True) for i in range(4)],
                             reads=[TQb[sl], TQbA[sl], TPb[sl]], writes=[TMB[mi]])
                        src = MB[mi][:].rearrange("p (t i) -> p i t", t=4)
                        K.op("act", lambda e: e.activation(out=Gbuf[:, :, tt:tt + 4], in_=src, func=AF.Copy),
                             writes=[TG, TMB[mi]])

            for gi in range(NBUF - 1):
                load_chunk(gi)
            for _ in part1(0):
                pass
            chk(51)

            def epi_adds(b):
                K.op("dve", lambda e: [e.tensor_tensor(out=xe[:, j, hf * 512:(hf + 1) * 512], in0=xe[:, j, hf * 512:(hf + 1) * 512],
                                                       in1=opsb[2 * j + hf][:], op=ALU.add)
                                       for j in range(2) for hf in range(2)], reads=[Txe], writes=[Txe] + Tops)

            def epi_rest(b):
                t0_ = b * TB
                for j in range(2):
                    K.op("act", lambda e: e.activation(out=h2bf[:, j, :], in_=xe[:, j, :], func=AF.Square,
                                                       accum_out=ss2[:, j:j + 1]), reads=[Txe], writes=[Th2bf, Tss2])
                    yield 0.1
                rstd(ss2, Tss2, ms2, Tms2, rs2, Trs2)
                yield 0.1
                for j in range(2):
                    for hf in range(2):
                        cs = slice(hf * 512, (hf + 1) * 512)
                        K.op("dve", lambda e: e.scalar_tensor_tensor(out=xe[:, j, cs], in0=xe[:, j, cs], scalar=rs2[:, j:j + 1],
                                                                     in1=gfin[:, cs], op0=ALU.mult, op1=ALU.mult),
                             reads=[Txe, Trs2, Tgfin], writes=[Txe])
                        yield 0.6
                K.dma("sp", lambda e: e.dma_start(out=y[t0_:t0_ + TB, :].rearrange("(j p) d -> p j d", p=128), in_=xe[:]),
                      reads=[Txe], sem_of=Tst[b % 2])
                yield 0.1

            def chain(*gens):
                for g in gens:
                    if g is not None:
                        for v in g:
                            yield v

            for b in range(NBLK):
                tok0 = b * TB
                pb = b % 2
                part2(b)
                chk(52)
                chk(60 + b)
                if b > 0:
                    epi_adds(b - 1)
                bg = chain(epi_rest(b - 1) if b > 0 else None, part1(b + 1) if b + 1 < NBLK else None)

                def emitA(c):
                    gi = b * NCH + c
                    sl = gi % NBUF
                    ai = c % 2
                    K.op("pe", lambda e: [e.matmul(Apsb[ai][:, 0:TB], lhsT=UT[sl][:, k * 128:(k + 1) * 128], rhs=h2T[pb][:, k, :],
                                                   start=(k == 0), stop=(k == 7)) for k in range(8)],
                         reads=[TUT[sl], Th2T[pb]], writes=[TAps[ai]])
                    K.op("act", lambda e: e.activation(out=gl[ai][:], in_=Apsb[ai][:, 0:TB], func=AF.Gelu),
                         writes=[Tgl[ai], TAps[ai]])
                    K.op("pool", lambda e: e.tensor_tensor(out=Wb[c % 4][:], in0=gl[ai][:], in1=Gbuf[:, c, :], op=ALU.mult),
                         reads=[Tgl[ai], TG], writes=[TW[c % 4]])

                def emitV(c):
                    gi = b * NCH + c
                    sl = gi % NBUF
                    K.op("pe", lambda e: [e.matmul(opsb[2 * j + hf][:], lhsT=Wb[c % 4][:, j * 128:(j + 1) * 128],
                                                   rhs=Vc[sl][:, hf * 512:(hf + 1) * 512], start=(c == 0), stop=(c == NCH - 1))
                                          for j in range(2) for hf in range(2)],
                         reads=[TW[c % 4], TVc[sl]], writes=Tops, acc=(Tops if c > 0 else ()))

                emitA(0)
                emitA(1)
                for c in range(NCH):
                    if c + 2 < NCH:
                        emitA(c + 2)
                    emitV(c)
                    load_chunk(b * NCH + c + NBUF - 1)
                    if c == NCH - 24:
                        K.dma("sp", lambda e: e.dma_start(out=xe[:], in_=x1d[tok0:tok0 + TB, :].rearrange("(j p) d -> p j d", p=128)),
                              reads=[Tx1d[b]], writes=[Txe])
                    if bg is not None and c >= 2:
                        budget = 1.45
                        while budget > 0:
                            try:
                                budget -= next(bg)
                            except StopIteration:
                                bg = None
                                break
                if bg is not None:
                    for _ in bg:
                        pass

                chk(53)
                chk(70 + b)
                chk(54)
            epi_adds(NBLK - 1)
            for _ in epi_rest(NBLK - 1):
                pass
            K.barrier()
    return nc


def _prep_weights(inputs):
    f = lambda a: np.ascontiguousarray(np.asarray(a, dtype=np.float32))
    L = 0
    eu = np.asarray(inputs["expert_u"], dtype=np.float32)[L]
    uT = np.ascontiguousarray(eu.reshape(NCH, 128, 8, 128).transpose(0, 3, 2, 1)).reshape(NEXP, D)
    sk = np.asarray(inputs["sub_keys"], dtype=np.float32)[L]
    keysT = np.ascontiguousarray(sk.reshape(16, 128, 128).transpose(2, 0, 1))
    gwt = np.ascontiguousarray(np.asarray(inputs["pool_group_w"], dtype=np.float32)[L].transpose(1, 0, 2))
    pscale = np.ascontiguousarray(np.asarray(inputs["pool_scale"], dtype=np.float32)[L].reshape(4, 128).T)
    cwt = np.ascontiguousarray(np.asarray(inputs["conv_w"], dtype=np.float32)[L].reshape(3, 4, 128).transpose(2, 1, 0))
    return dict(
        w_in=f(inputs["w_in"][L]), w_bp=f(inputs["w_branch_pool"][L]), w_bc=f(inputs["w_branch_conv"][L]),
        w_out=f(inputs["w_out"][L]), w_q=f(inputs["w_q"][L]), gw=gwt, pscale=pscale, cw=cwt,
        gmix=f(inputs["g_mix"][L]), gffn=f(inputs["g_ffn"][L]), gfin=f(inputs["g_final"]),
        keysT=keysT, uT=uT, v=f(np.asarray(inputs["expert_v"])[L]),
    )


def run(inputs, ncores, ntok, seq, phase='AB'):
    x = np.asarray(inputs["x"], dtype=np.float32)
    B, S_, _ = x.shape
    xf = np.ascontiguousarray(x.reshape(B * S_, D))
    assert B * S_ == ncores * ntok
    wts = _prep_weights(inputs)
    nc = build(ntok, seq, phase)
    in_maps = []
    for c in range(ncores):
        m = dict(wts)
        m["x"] = xf[c * ntok:(c + 1) * ntok]
        in_maps.append(m)
    res = run_bass_kernel_spmd(nc, in_maps, core_ids=list(range(ncores)))
    out = np.concatenate([np.asarray(r["y"]) for r in res.results], axis=0)
    return out.reshape(B, S_, D).astype(np.float32)


def kernel(**inputs):
    return run(inputs, 8, 8192, 4096)
```

```python
import numpy as np
from contextlib import ExitStack
import concourse.bass as bass
import concourse.mybir as mybir
from concourse.bass_utils import run_bass_kernel_spmd

F32 = mybir.dt.float32
BF16 = mybir.dt.bfloat16
U32 = mybir.dt.uint32
ALU = mybir.AluOpType
AF = mybir.ActivationFunctionType
AX = mybir.AxisListType

D = 1024
DIN = 4096
NEXP = 16384
NCH = 128
TB = 256
EPS = 1e-6
NBUF = 6
NSB = 8


class T:
    __slots__ = ("name", "w", "r", "dsem")

    def __init__(self, name):
        self.name = name
        self.w = None
        self.r = {}
        self.dsem = None


class Sem:
    __slots__ = ("h", "n")

    def __init__(self, h):
        self.h = h
        self.n = 0


import os
class _Stop(Exception):
    pass


_STOP = [False]


def chk(n):
    if int(os.environ.get('KSTAGE', '0')) == n:
        _STOP[0] = True


class Ctx:
    def __init__(self, nc, es):
        self.nc, self.es = nc, es
        self.E = dict(pe=nc.tensor, act=nc.scalar, dve=nc.vector, pool=nc.gpsimd, sp=nc.sync)
        self.allsems = []
        self.esem = {k: self.newsem("e_" + k) for k in ("pe", "act", "dve", "pool")}
        self.seen = {k: {} for k in self.E}

    def newsem(self, name):
        s = Sem(self.es.enter_context(self.nc.semaphore(name)))
        self.allsems.append(s)
        return s

    def _wait(self, eng, need):
        seen = self.seen[eng]
        for s, v in need.items():
            if seen.get(s, 0) < v:
                self.E[eng].wait_ge(s.h, v)
                seen[s] = v

    def _need(self, eng, reads, writes, acc, skip=None):
        need = {}

        def add(s, v):
            if s is skip:
                return
            if need.get(s, 0) < v:
                need[s] = v
        me = self.esem.get(eng)
        for t in reads:
            if t.w is not None:
                add(*t.w)
        for t in writes:
            if t.w is not None and t.w[0] is not me:
                add(*t.w)
            for s, v in t.r.items():
                if s is not me:
                    add(s, v)
        return need

    def _mark(self, s, v, reads, writes):
        for t in reads:
            if t.r.get(s, 0) < v:
                t.r[s] = v
        for t in writes:
            t.w = (s, v)
            t.r = {}

    def op(self, eng, fn, reads=(), writes=(), acc=()):
        if _STOP[0]:
            return
        self._wait(eng, self._need(eng, reads, writes, acc))
        inst = fn(self.E[eng])
        if isinstance(inst, (list, tuple)):
            inst = inst[-1]
        s = self.esem[eng]
        s.n += 1
        inst.then_inc(s.h, 1)
        self._mark(s, s.n, reads, writes)

    def dma(self, eng, fn, reads=(), writes=(), sem_of=None, group=False):
        if _STOP[0]:
            return
        t = sem_of if sem_of is not None else (writes[0] if writes else reads[0])
        if t.dsem is None:
            t.dsem = self.newsem("d_" + t.name)
        self._wait(eng, self._need(eng, reads, writes, (), skip=(t.dsem if group else None)))
        inst = fn(self.E[eng])
        t.dsem.n += 16
        inst.then_inc(t.dsem.h, 16)
        self._mark(t.dsem, t.dsem.n, reads, writes)

    def barrier(self, engs=("pe", "act", "dve", "pool", "sp")):
        for e in engs:
            self._wait(e, {s: s.n for s in self.allsems if s.n > 0})


def build(NTOK, SEQ, phase='AB'):
    _STOP[0] = False
    nc = bass.Bass("TRN2", target_bir_lowering=False)
    NBLK = NTOK // TB
    BPS = SEQ // TB

    def din(name, shape, dt=F32):
        return nc.dram_tensor(name, shape, dt, kind="ExternalInput").ap()

    x = din("x", [NTOK, D])
    w_in = din("w_in", [D, DIN])
    w_bp = din("w_bp", [512, D])
    w_bc = din("w_bc", [512, D])
    w_out = din("w_out", [D, D])
    w_q = din("w_q", [D, 2048])
    gw = din("gw", [128, 4, 128])
    pscale_d = din("pscale", [128, 4])
    cw_d = din("cw", [128, 4, 3])
    gmix_d = din("gmix", [D])
    gffn_d = din("gffn", [D])
    gfin_d = din("gfin", [D])
    keysT = din("keysT", [128, 16, 128])
    uT = din("uT", [NEXP, D])
    vv = din("v", [NEXP, D])
    y = nc.dram_tensor("y", [NTOK, D], F32, kind="ExternalOutput").ap()
    x1d = nc.dram_tensor("x1d", [NTOK, D], F32, kind="Internal").ap()
    uTb = nc.dram_tensor("uTb", [NEXP, D], BF16, kind="Internal").ap()
    vb = nc.dram_tensor("vb", [NEXP, D], BF16, kind="Internal").ap()
    wqb = nc.dram_tensor("wqb", [D, 2048], BF16, kind="Internal").ap()

    with ExitStack() as es:
        K = Ctx(nc, es)

        def sbt(st, name, shape, dt):
            return st.enter_context(nc.sbuf_tensor(name, shape, dt))

        def pst(st, name, shape, dt):
            return st.enter_context(nc.psum_tensor(name, shape, dt))

        io128 = sbt(es, "io128", [128, 128], F32); Tio = T("io128")
        pidx = sbt(es, "pidx", [128, 1], F32); Tpidx = T("pidx")
        idf = sbt(es, "idf", [128, 128], F32); Tidf = T("idf")
        idb = sbt(es, "idb", [128, 128], BF16); Tidb = T("idb")
        K.op("pool", lambda e: e.iota(io128[:], pattern=[[1, 128]], base=0, channel_multiplier=0,
                                      allow_small_or_imprecise_dtypes=True), writes=[Tio])
        K.op("pool", lambda e: e.iota(pidx[:], pattern=[[0, 1]], base=0, channel_multiplier=1,
                                      allow_small_or_imprecise_dtypes=True), writes=[Tpidx])
        K.op("dve", lambda e: e.tensor_scalar(out=idf[:], in0=io128[:], scalar1=pidx[:, 0:1], scalar2=None,
                                              op0=ALU.is_equal), reads=[Tio, Tpidx], writes=[Tidf])
        K.op("dve", lambda e: e.tensor_copy(out=idb[:], in_=idf[:]), reads=[Tidf], writes=[Tidb])

        Texp = T("experts")
        Tx1d = [T("x1d%d" % b) for b in range(NBLK)]
        Tst = [T("st0"), T("st1")]

        with ExitStack() as ea:
            w_in_bf = sbt(ea, "w_in_bf", [128, 8, DIN], BF16); Twin = T("w_in")
            w_bp_bf = sbt(ea, "w_bp_bf", [128, 4, D], BF16); Twbp = T("w_bp")
            w_bc_bf = sbt(ea, "w_bc_bf", [128, 4, D], BF16); Twbc = T("w_bc")
            w_out_bf = sbt(ea, "w_out_bf", [128, 8, D], BF16); Twout = T("w_out")
            gw_bf = sbt(ea, "gw_bf", [128, 4, 128], BF16); Tgw = T("gw")
            pscale = sbt(ea, "pscale_s", [128, 4], F32); Tpsc = T("pscale")
            cw = sbt(ea, "cw_s", [128, 4, 3], F32); Tcw = T("cw")
            gmix = sbt(ea, "gmix_s", [128, D], F32); Tgmix = T("gmix")
            invcnt = sbt(ea, "invcnt", [128, 4, 16], F32); Tinv = T("invcnt")
            io16p = sbt(ea, "io16p", [128, 16], F32); Tio16p = T("io16p")
            xblk = [sbt(ea, "xblk%d" % i, [128, 2, D], F32) for i in range(2)]
            Txblk = [T("xblk0"), T("xblk1")]
            junk = sbt(ea, "junkA", [128, D], BF16); Tjunk = T("junk")
            ss = sbt(ea, "ssA", [128, 2], F32); Tss = T("ss")
            sd = sbt(ea, "sdA", [128, 2], F32); Tsd = T("sd")
            rs = sbt(ea, "rsA", [128, 2], F32); Trs = T("rs")
            hbf = sbt(ea, "hbf", [128, 2, D], BF16); Thbf = T("hbf")
            hT = sbt(ea, "hT", [128, 8, TB], BF16); ThT = T("hT")
            zp = [sbt(ea, "zp%d" % i, [128, 4, 16 + TB], F32) for i in range(2)]
            Tzp = [T("zp0"), T("zp1")]
            sA = sbt(ea, "sA", [128, 4, 16 + TB], F32); TsA = T("sA")
            sB = sbt(ea, "sB", [128, 4, 16 + TB], F32); TsB = T("sB")
            tmpc = sbt(ea, "tmpc", [128, 4, 16], F32); Ttmpc = T("tmpc")
            zc = sbt(ea, "zc", [128, 12, TB], F32); Tzc = T("zc")
            ub = [sbt(ea, "ub%d" % i, [128, 4, 2 + TB], F32) for i in range(2)]
            Tub = [T("ub0"), T("ub1")]
            yb = sbt(ea, "yb", [128, 4, TB], F32); Tyb = T("yb")
            sig = sbt(ea, "sig", [128, 16, TB], F32); Tsig = T("sig")
            pbf = sbt(ea, "pbf", [128, 4, TB], BF16); Tpbf = T("pbf")
            pmbf = sbt(ea, "pmbf", [128, 4, TB], BF16); Tpmbf = T("pmbf")
            cvbf = sbt(ea, "cvbf", [128, 4, TB], BF16); Tcvbf = T("cvbf")
            t12 = sbt(ea, "t12", [128, 2, TB], F32); Tt12 = T("t12")
            mbf = sbt(ea, "mbf", [128, 8, TB], BF16); Tmbf = T("mbf")
            tps = pst(ea, "tpsA", [128, 8, 128], BF16); Ttps = T("tps")
            PBA = [pst(ea, "pbA%d" % i, [128, 512], F32) for i in range(7)]
            TPBA = [T("pbA%d" % i) for i in range(7)]
            zpsb = [PBA[0], PBA[1]]; Tzps = [TPBA[0], TPBA[1]]
            gpsb = [PBA[2], PBA[3]]; Tgpsb = [TPBA[2], TPBA[3]]
            abps = [PBA[4], PBA[5]]; Tabps = [TPBA[4], TPBA[5]]
            xops = [PBA[6], PBA[2]]; Txops = [TPBA[6], TPBA[2]]
            gpsv = lambda g: gpsb[g // 2][:, (g % 2) * TB:(g % 2 + 1) * TB]

            for k in range(8):
                for pc in range(4):
                    K.dma("pool", lambda e, k=k, pc=pc: e.dma_start(
                        out=w_in_bf[:, k, pc * 1024:(pc + 1) * 1024],
                        in_=w_in[k * 128:(k + 1) * 128, pc * 1024:(pc + 1) * 1024]), writes=[Twin], group=True)
            K.dma("pool", lambda e: e.dma_start(out=gw_bf[:], in_=gw), writes=[Tgw])
            for g in range(4):
                K.dma("pool", lambda e, g=g: e.dma_start(out=w_bp_bf[:, g, :], in_=w_bp[g * 128:(g + 1) * 128, :]),
                      writes=[Twbp], group=True)
                K.dma("pool", lambda e, g=g: e.dma_start(out=w_bc_bf[:, g, :], in_=w_bc[g * 128:(g + 1) * 128, :]),
                      writes=[Twbc], group=True)
            for k in range(8):
                K.dma("pool", lambda e, k=k: e.dma_start(out=w_out_bf[:, k, :], in_=w_out[k * 128:(k + 1) * 128, :]),
                      writes=[Twout], group=True)
            K.dma("sp", lambda e: e.dma_start(out=pscale[:], in_=pscale_d), writes=[Tpsc])
            K.dma("sp", lambda e: e.dma_start(out=cw[:], in_=cw_d), writes=[Tcw])
            K.dma("sp", lambda e: e.dma_start(out=gmix[:], in_=gmix_d.partition_broadcast(128)), writes=[Tgmix])
            NPIECE = 8
            RP = NEXP // NPIECE
            for pc in range(NPIECE if 'B' in phase else 0):
                K.dma("pool", lambda e, pc=pc: e.dma_start(out=uTb[pc * RP:(pc + 1) * RP, :],
                                                           in_=uT[pc * RP:(pc + 1) * RP, :]),
                      writes=[Texp], group=True)
                K.dma("pool", lambda e, pc=pc: e.dma_start(out=vb[pc * RP:(pc + 1) * RP, :],
                                                           in_=vv[pc * RP:(pc + 1) * RP, :]),
                      writes=[Texp], group=True)
            K.op("dve", lambda e: e.tensor_scalar(out=io16p[:], in0=io128[:, 0:16], scalar1=1.0, scalar2=None,
                                                  op0=ALU.add), reads=[Tio], writes=[Tio16p])
            K.op("dve", lambda e: [e.tensor_scalar(out=invcnt[:, g, :], in0=io16p[:], scalar1=float(2 ** (g + 1)),
                                                   scalar2=None, op0=ALU.min) for g in range(4)],
                 reads=[Tio16p], writes=[Tinv])
            K.op("dve", lambda e: e.reciprocal(out=invcnt[:], in_=invcnt[:]), reads=[Tinv], writes=[Tinv])

            def headA_load(b):
                par = b % 2
                tok0 = b * TB
                K.dma("sp", lambda e: e.dma_start(out=xblk[par][:], in_=x[tok0:tok0 + TB, :].rearrange("(j p) d -> p j d", p=128)),
                      writes=[Txblk[par]])

            def headA(b):
                par = b % 2
                X = xblk[par]
                TX = Txblk[par]
                for j in range(2):
                    K.op("act", lambda e, j=j: e.activation(out=junk[:], in_=X[:, j, :], func=AF.Square,
                                                            accum_out=ss[:, j:j + 1]),
                         reads=[TX], writes=[Tjunk, Tss])
                K.op("act", lambda e: e.activation(out=sd[:], in_=ss[:], func=AF.Sqrt, scale=1.0 / D, bias=EPS),
                     reads=[Tss], writes=[Tsd])
                K.op("dve", lambda e: e.reciprocal(out=rs[:], in_=sd[:]), reads=[Tsd], writes=[Trs])
                K.op("dve", lambda e: [e.scalar_tensor_tensor(out=hbf[:, j, :], in0=X[:, j, :], scalar=rs[:, j:j + 1],
                                                              in1=gmix[:], op0=ALU.mult, op1=ALU.mult)
                                       for j in range(2)],
                     reads=[TX, Trs, Tgmix], writes=[Thbf])
            chk(1)
            for b in range(NBLK):
                par = b % 2
                first = (b % BPS == 0)
                tok0 = b * TB
                X = xblk[par]
                TX = Txblk[par]
                Z = zp[par]
                TZ = Tzp[par]
                U = ub[par]
                TU = Tub[par]
                if b == 0:
                    headA_load(0)
                    headA(0)
                if b + 1 < NBLK:
                    headA_load(b + 1)
                chk(2)
                for j in range(2):
                    K.op("pe", lambda e, j=j: [e.transpose(out=tps[:, k, :], in_=hbf[:, j, k * 128:(k + 1) * 128],
                                                           identity=idb[:]) for k in range(8)],
                         reads=[Thbf, Tidb], writes=[Ttps])
                    if j == 0:
                        K.op("act", lambda e, j=j: e.activation(out=hT[:, :, j * 128:(j + 1) * 128], in_=tps[:],
                                                                func=AF.Copy), writes=[ThT, Ttps])
                    else:
                        K.op("dve", lambda e, j=j: e.tensor_copy(out=hT[:, :, j * 128:(j + 1) * 128], in_=tps[:]),
                             writes=[ThT, Ttps])
                chk(3)
                if first:
                    K.op("dve", lambda e: [e.memset(Z[:, :, 0:16], 0.0), e.memset(U[:, :, 0:2], 0.0)],
                         writes=[TZ, TU])
                else:
                    K.op("dve", lambda e: [e.tensor_copy(out=Z[:, :, 0:16], in_=zp[1 - par][:, :, TB:TB + 16]),
                                           e.tensor_copy(out=U[:, :, 0:2], in_=ub[1 - par][:, :, TB:TB + 2])],
                         reads=[Tzp[1 - par], Tub[1 - par]], writes=[TZ, TU])
                chk(31)
                for oc in range(32):
                    zi = oc % 2
                    K.op("pe", lambda e, oc=oc, zi=zi: [e.matmul(zpsb[zi][:, 0:TB], lhsT=w_in_bf[:, k, oc * 128:(oc + 1) * 128],
                                                                 rhs=hT[:, k, :], start=(k == 0), stop=(k == 7))
                                                        for k in range(8)],
                         reads=[ThT, Twin], writes=[Tzps[zi]])
                    if oc == 0:
                        chk(321)
                    if oc < 4:
                        K.op("act", lambda e, oc=oc, zi=zi: e.activation(out=Z[:, oc, 16:16 + TB], in_=zpsb[zi][:, 0:TB],
                                                                         func=AF.Copy), writes=[TZ, Tzps[zi]])
                    elif oc < 16:
                        K.op("act", lambda e, oc=oc, zi=zi: e.activation(out=zc[:, oc - 4, :], in_=zpsb[zi][:, 0:TB],
                                                                         func=AF.Copy), writes=[Tzc, Tzps[zi]])
                    else:
                        K.op("act", lambda e, oc=oc, zi=zi: e.activation(out=sig[:, oc - 16, :], in_=zpsb[zi][:, 0:TB],
                                                                         func=AF.Sigmoid), writes=[Tsig, Tzps[zi]])
                    if oc == 2:
                        chk(32)
                    if oc == 0:
                        chk(322)
                    if oc == 3:
                        W1 = 16 + TB
                        K.op("dve", lambda e: e.tensor_tensor(out=sA[:, :, 1:W1], in0=Z[:, :, 1:W1], in1=Z[:, :, 0:W1 - 1],
                                                              op=ALU.add), reads=[TZ], writes=[TsA])
                        K.op("dve", lambda e: [
                            e.scalar_tensor_tensor(out=pbf[:, 0, :], in0=sA[:, 0, 16:W1], scalar=0.5, in1=Z[:, 0, 16:W1],
                                                   op0=ALU.mult, op1=ALU.subtract),
                            e.tensor_tensor(out=sB[:, 1:4, 3:W1], in0=sA[:, 1:4, 3:W1], in1=sA[:, 1:4, 1:W1 - 2],
                                            op=ALU.add)],
                             reads=[TsA, TZ], writes=[Tpbf, TsB])
                        K.op("dve", lambda e: [
                            e.scalar_tensor_tensor(out=pbf[:, 1, :], in0=sB[:, 1, 16:W1], scalar=0.25, in1=Z[:, 1, 16:W1],
                                                   op0=ALU.mult, op1=ALU.subtract),
                            e.tensor_tensor(out=sA[:, 2:4, 7:W1], in0=sB[:, 2:4, 7:W1], in1=sB[:, 2:4, 3:W1 - 4],
                                            op=ALU.add)],
                             reads=[TsB, TZ], writes=[Tpbf, TsA])
                        K.op("dve", lambda e: [
                            e.scalar_tensor_tensor(out=pbf[:, 2, :], in0=sA[:, 2, 16:W1], scalar=0.125, in1=Z[:, 2, 16:W1],
                                                   op0=ALU.mult, op1=ALU.subtract),
                            e.tensor_tensor(out=sB[:, 3, 15:W1], in0=sA[:, 3, 15:W1], in1=sA[:, 3, 7:W1 - 8],
                                            op=ALU.add)],
                             reads=[TsA, TZ], writes=[Tpbf, TsB])
                        K.op("dve", lambda e: e.scalar_tensor_tensor(out=pbf[:, 3, :], in0=sB[:, 3, 16:W1], scalar=1.0 / 16,
                                                                     in1=Z[:, 3, 16:W1], op0=ALU.mult, op1=ALU.subtract),
                             reads=[TsB, TZ], writes=[Tpbf])
                        if first:
                            srcs = [sA[:, 0, 16:32], sB[:, 1, 16:32], sA[:, 2, 16:32], sB[:, 3, 16:32]]
                            K.op("dve", lambda e: [e.tensor_tensor(out=tmpc[:, g, :], in0=srcs[g], in1=invcnt[:, g, :],
                                                                   op=ALU.mult) for g in range(4)],
                                 reads=[TsA, TsB, Tinv], writes=[Ttmpc])
                            K.op("dve", lambda e: e.tensor_tensor(out=pbf[:, :, 0:16], in0=tmpc[:], in1=Z[:, :, 16:32],
                                                                  op=ALU.subtract), reads=[Ttmpc, TZ], writes=[Tpbf])
                    if oc == 9:
                        K.op("pe", lambda e: [e.matmul(gpsv(g), lhsT=gw_bf[:, g, :], rhs=pbf[:, g, :], start=True,
                                                       stop=True) for g in range(4)],
                             reads=[Tpbf, Tgw], writes=Tgpsb)
                        K.op("dve", lambda e: [e.tensor_scalar(out=pmbf[:, g, :], in0=gpsv(g), scalar1=pscale[:, g:g + 1],
                                                               scalar2=None, op0=ALU.mult) for g in range(4)],
                             reads=[Tpsc], writes=[Tpmbf] + Tgpsb)
                    if oc == 4:
                        chk(34)
                    if oc == 15:
                        chk(35)
                        K.op("dve", lambda e: e.tensor_tensor(out=U[:, :, 2:2 + TB], in0=zc[:, 0:4, :], in1=zc[:, 8:12, :],
                                                              op=ALU.mult), reads=[Tzc], writes=[TU])
                        K.op("dve", lambda e: [e.tensor_scalar(out=yb[:, ch, :], in0=U[:, ch, 2:2 + TB],
                                                               scalar1=cw[:, ch, 2:3], scalar2=None, op0=ALU.mult)
                                               for ch in range(4)], reads=[TU, Tcw], writes=[Tyb])
                        for kk in (1, 0):
                            K.op("dve", lambda e, kk=kk: [e.scalar_tensor_tensor(out=yb[:, ch, :], in0=U[:, ch, kk:kk + TB],
                                                                                 scalar=cw[:, ch, kk:kk + 1], in1=yb[:, ch, :],
                                                                                 op0=ALU.mult, op1=ALU.add)
                                                          for ch in range(4)], reads=[TU, Tcw, Tyb], writes=[Tyb])
                        K.op("dve", lambda e: e.tensor_tensor(out=cvbf[:], in0=yb[:], in1=zc[:, 4:8, :], op=ALU.mult),
                             reads=[Tyb, Tzc], writes=[Tcvbf])
                chk(4)
                for dc in range(8):
                    ab = abps[dc % 2]
                    Tab = Tabps[dc % 2]
                    K.op("pe", lambda e, dc=dc, ab=ab: (
                        [e.matmul(ab[:, 0:TB], lhsT=w_bp_bf[:, g, dc * 128:(dc + 1) * 128], rhs=pmbf[:, g, :],
                                  start=(g == 0), stop=(g == 3)) for g in range(4)] +
                        [e.matmul(ab[:, TB:2 * TB], lhsT=w_bc_bf[:, g, dc * 128:(dc + 1) * 128], rhs=cvbf[:, g, :],
                                  start=(g == 0), stop=(g == 3)) for g in range(4)]),
                         reads=[Tpmbf, Tcvbf, Twbp, Twbc], writes=[Tab])
                    K.op("dve", lambda e, dc=dc, ab=ab: [
                        e.tensor_tensor(out=t12[:, 0, :], in0=sig[:, dc, :], in1=ab[:, 0:TB], op=ALU.mult),
                        e.tensor_tensor(out=t12[:, 1, :], in0=sig[:, 8 + dc, :], in1=ab[:, TB:2 * TB], op=ALU.mult)],
                         reads=[Tsig], writes=[Tt12, Tab])
                    K.op("dve", lambda e, dc=dc: e.tensor_tensor(out=mbf[:, dc, :], in0=t12[:, 0, :], in1=t12[:, 1, :],
                                                                 op=ALU.add), reads=[Tt12], writes=[Tmbf])
                chk(5)
                if b + 1 < NBLK:
                    headA(b + 1)
                for j in range(2):
                    for hf in range(2):
                        xi = (2 * j + hf) % 2
                        K.op("pe", lambda e, j=j, hf=hf, xi=xi: [
                            e.matmul(xops[xi][:], lhsT=mbf[:, k, j * 128:(j + 1) * 128],
                                     rhs=w_out_bf[:, k, hf * 512:(hf + 1) * 512], start=(k == 0), stop=(k == 7))
                            for k in range(8)], reads=[Tmbf, Twout], writes=[Txops[xi]])
                        K.op("dve", lambda e, j=j, hf=hf, xi=xi: e.tensor_tensor(
                            out=X[:, j, hf * 512:(hf + 1) * 512], in0=X[:, j, hf * 512:(hf + 1) * 512], in1=xops[xi][:],
                            op=ALU.add), reads=[TX], writes=[TX, Txops[xi]])
                dst = x1d if 'B' in phase else y
                K.dma("sp", lambda e: e.dma_start(out=dst[tok0:tok0 + TB, :].rearrange("(j p) d -> p j d", p=128), in_=X[:]),
                      reads=[TX], writes=[Tx1d[b]], sem_of=Tst[par])
            K.barrier()

        with ExitStack() as eb:
            if 'B' not in phase:
                return nc
            keys_bf = sbt(eb, "keys_bf", [128, 16, 128], BF16); Tkeys = T("keys")
            gffn = sbt(eb, "gffn_s", [128, D], F32); Tgffn = T("gffn")
            gfin = sbt(eb, "gfin_s", [128, D], F32); Tgfin = T("gfin")
            iob = sbt(eb, "iob", [128, 128], BF16); Tiob = T("iob")
            ecst = sbt(eb, "ecst", [128, 128], F32); Tecst = T("ecst")
            nhalf = sbt(eb, "nhalf", [128, 2], F32); Tnhalf = T("nhalf")
            wqr = [sbt(eb, "wqr%d" % i, [128, 8, 256], BF16) for i in range(2)]; Twqr = [T("wqr0"), T("wqr1")]
            xp = sbt(eb, "xp", [128, 2, D], F32); Txp = T("xp")
            xe = sbt(eb, "xe", [128, 2, D], F32); Txe = T("xe")
            ss1 = sbt(eb, "ss1", [128, 2], F32); Tss1 = T("ss1")
            ms1 = sbt(eb, "ms1", [128, 2], F32); Tms1 = T("ms1")
            rs1 = sbt(eb, "rs1", [128, 2], F32); Trs1 = T("rs1")
            ss2 = sbt(eb, "ss2", [128, 2], F32); Tss2 = T("ss2")
            ms2 = sbt(eb, "ms2", [128, 2], F32); Tms2 = T("ms2")
            rs2 = sbt(eb, "rs2", [128, 2], F32); Trs2 = T("rs2")
            h2bf = sbt(eb, "h2bf", [128, 2, D], BF16); Th2bf = T("h2bf")
            h2T = [sbt(eb, "h2T%d" % i, [128, 8, TB], BF16) for i in range(2)]; Th2T = [T("h2T0"), T("h2T1")]
            qT = sbt(eb, "qT", [128, 16, TB], BF16); TqT = T("qT")
            S = sbt(eb, "S", [128, 2048], F32); TS = T("S")
            S2 = sbt(eb, "S2", [128, 2048], F32); TS2 = T("S2")
            cand = sbt(eb, "cand", [128, 2048], F32); Tcand = T("cand")
            V16 = sbt(eb, "V16", [128, 16, 16], F32); TV16 = T("V16")
            IX = sbt(eb, "IX", [128, 16, 16], U32); TIX = T("IX")
            IXf = sbt(eb, "IXf", [128, 16, 16], F32); TIXf = T("IXf")
            F16 = sbt(eb, "F16", [128, 8, 16], F32); TF16 = T("F16")
            PS = sbt(eb, "PS", [128, 8, 16], U32); TPS = T("PS")
            au = sbt(eb, "au", [128, 2, 128], U32); Tau = T("au")
            apf = sbt(eb, "apf", [128, 2, 128], F32); Tapf = T("apf")
            sh = sbt(eb, "sh", [128, 128], F32); Tsh = T("sh")
            ex = sbt(eb, "ex", [128, 128], F32); Tex = T("ex")
            Zs = sbt(eb, "Zs", [128, 8], F32); TZs = T("Zs")
            rZ = sbt(eb, "rZ", [128, 8], F32); TrZ = T("rZ")
            IJG = sbt(eb, "IJG", [128, 3, 128], F32); TIJG = T("IJG")
            IJGT = [sbt(eb, "IJGT%d" % i, [128, 3, TB], F32) for i in range(2)]; TIJGT = [T("IJGT0"), T("IJGT1")]
            Qb = [sbt(eb, "Qb%d" % i, [128, NSB, 128], BF16) for i in range(2)]; TQb = [T("Qb0"), T("Qb1")]
            Pb = [sbt(eb, "Pb%d" % i, [128, NSB, 128], BF16) for i in range(2)]; TPb = [T("Pb0"), T("Pb1")]
            Gbuf = sbt(eb, "Gbuf", [128, NCH, TB], BF16); TG = T("Gbuf")
            UT = [sbt(eb, "UT%d" % i, [128, D], BF16) for i in range(NBUF)]; TUT = [T("UT%d" % i) for i in range(NBUF)]
            Vc = [sbt(eb, "Vc%d" % i, [128, D], BF16) for i in range(NBUF)]; TVc = [T("Vc%d" % i) for i in range(NBUF)]
            gl = [sbt(eb, "gl%d" % i, [128, TB], BF16) for i in range(2)]; Tgl = [T("gl0"), T("gl1")]
            Wb = [sbt(eb, "Wb%d" % i, [128, TB], BF16) for i in range(4)]; TW = [T("W%d" % i) for i in range(4)]
            opsb = [pst(eb, "ops%d" % i, [128, 512], F32) for i in range(4)]; Tops = [T("ops%d" % i) for i in range(4)]
            tpb = pst(eb, "tpsB", [128, 8, 128], BF16); Ttpb = T("tpsB")
            MB = [pst(eb, "MB%d" % i, [128, 512], F32) for i in range(3)]
            TMB = [T("MB0"), T("MB1"), T("MB2")]
            Apsb = [MB[1], MB[2]]; TAps = [TMB[1], TMB[2]]

            Twqb = T("wqb")
            for pc in range(2):
                K.dma("pool", lambda e: e.dma_start(out=wqb[:, pc * 1024:(pc + 1) * 1024], in_=w_q[:, pc * 1024:(pc + 1) * 1024]),
                      writes=[Twqb], group=True)
            K.dma("pool", lambda e: e.dma_start(out=keys_bf[:], in_=keysT), writes=[Tkeys])
            K.dma("sp", lambda e: e.dma_start(out=gffn[:], in_=gffn_d.partition_broadcast(128)), writes=[Tgffn])
            K.dma("sp", lambda e: e.dma_start(out=gfin[:], in_=gfin_d.partition_broadcast(128)), writes=[Tgfin])
            K.op("dve", lambda e: [e.tensor_copy(out=iob[:], in_=io128[:]), e.memset(ecst[:], float(np.e)),
                                   e.memset(nhalf[:], -0.5)], reads=[Tio], writes=[Tiob, Tecst, Tnhalf])
            wqb3 = wqb.rearrange("(k p) n -> p k n", p=128)

            uTb3 = uTb.rearrange("(c p) n -> c p n", p=128)
            vb3 = vb.rearrange("(c p) n -> c p n", p=128)
            NG = NBLK * NCH

            def load_chunk(gi):
                if gi >= NG:
                    return
                c = gi % NCH
                sl = gi % NBUF
                K.dma("sp", lambda e: e.dma_start(out=UT[sl][:], in_=uTb3[c]), reads=[Texp], writes=[TUT[sl]])
                K.dma("sp", lambda e: e.dma_start(out=Vc[sl][:], in_=vb3[c]), reads=[Texp], writes=[TVc[sl]])

            def rstd(ssx, Tssx, msx, Tmsx, rsx, Trsx):
                K.op("pool", lambda e: e.tensor_scalar(out=msx[:], in0=ssx[:], scalar1=1.0 / D, scalar2=EPS,
                                                       op0=ALU.mult, op1=ALU.add), reads=[Tssx], writes=[Tmsx])
                K.op("pool", lambda e: e.tensor_tensor(out=rsx[:], in0=msx[:], in1=nhalf[:], op=ALU.pow),
                     reads=[Tmsx, Tnhalf], writes=[Trsx])

            def load_wq(qp):
                K.dma("sp", lambda e: e.dma_start(out=wqr[qp % 2][:], in_=wqb3[:, :, qp * 256:(qp + 1) * 256]),
                      reads=[Twqb], writes=[Twqr[qp % 2]])

            def prefetch_p1(b):
                if b >= NBLK:
                    return
                t0_ = b * TB
                K.dma("sp", lambda e: e.dma_start(out=xp[:], in_=x1d[t0_:t0_ + TB, :].rearrange("(j p) d -> p j d", p=128)),
                      reads=[Tx1d[b]], writes=[Txp])
                load_wq(0)
                load_wq(1)

            def part1(b):
                pb = b % 2
                tok0 = b * TB
                H2T = h2T[pb]
                TH2T = Th2T[pb]
                if b == 0:
                    prefetch_p1(0)
                for j in range(2):
                    K.op("act", lambda e: e.activation(out=h2bf[:, j, :], in_=xp[:, j, :], func=AF.Square,
                                                       accum_out=ss1[:, j:j + 1]), reads=[Txp], writes=[Th2bf, Tss1])
                    yield 0.1
                rstd(ss1, Tss1, ms1, Tms1, rs1, Trs1)
                yield 0.1
                for j in range(2):
                    for hf in range(2):
                        cs = slice(hf * 512, (hf + 1) * 512)
                        K.op("dve", lambda e: e.scalar_tensor_tensor(out=h2bf[:, j, cs], in0=xp[:, j, cs], scalar=rs1[:, j:j + 1],
                                                                     in1=gffn[:, cs], op0=ALU.mult, op1=ALU.mult),
                             reads=[Txp, Trs1, Tgffn], writes=[Th2bf])
                        yield 0.6
                yield 3.0
                for j in range(2):
                    K.op("pe", lambda e: [e.transpose(out=tpb[:, k, :], in_=h2bf[:, j, k * 128:(k + 1) * 128],
                                                      identity=idb[:]) for k in range(8)],
                         reads=[Th2bf, Tidb], writes=[Ttpb])
                    K.op("act", lambda e: e.activation(out=H2T[:, :, j * 128:(j + 1) * 128], in_=tpb[:], func=AF.Copy),
                         writes=[TH2T, Ttpb])
                    yield 0.3
                for qp in range(8):
                    wr = wqr[qp % 2]
                    K.op("pe", lambda e: [e.matmul(MB[0][:, hh * 256:(hh + 1) * 256], lhsT=wr[:, k, hh * 128:(hh + 1) * 128],
                                                   rhs=H2T[:, k, :], start=(k == 0), stop=(k == 7))
                                          for hh in range(2) for k in range(8)],
                         reads=[Twqr[qp % 2], TH2T], writes=[TMB[0]])
                    K.op("act", lambda e: e.activation(out=qT[:, 2 * qp:2 * qp + 2, :],
                                                       in_=MB[0][:].rearrange("p (a t) -> p a t", a=2), func=AF.Copy),
                         writes=[TqT, TMB[0]])
                    if qp + 2 < 8:
                        load_wq(qp + 2)
                    yield 0.4
                prefetch_p1(b + 1)
                Sg = lambda g: S[:, g * 128:(g + 1) * 128]
                S2g = lambda g: S2[:, g * 128:(g + 1) * 128]
                Cg = lambda h: cand[:, h * 256:(h + 1) * 256]
                C2g = lambda h: S[:, h * 256:(h + 1) * 256]
                for j in range(2):
                    for pc in range(4):
                        K.op("pe", lambda e: [e.matmul(MB[0][:, i * 128:(i + 1) * 128], lhsT=qT[:, pc * 4 + i, j * 128:(j + 1) * 128],
                                                       rhs=keys_bf[:, pc * 4 + i, :], start=True, stop=True) for i in range(4)],
                             reads=[TqT, Tkeys], writes=[TMB[0]])
                        K.op("act", lambda e: e.activation(out=S[:, pc * 512:(pc + 1) * 512], in_=MB[0][:], func=AF.Copy),
                             writes=[TS, TMB[0]])
                        yield 0.4
                    for gq in range(8):
                        gs = range(2 * gq, 2 * gq + 2)
                        K.op("dve", lambda e: [e.max(out=V16[:, g, 0:8], in_=Sg(g)) for g in gs], reads=[TS], writes=[TV16])
                        yield 0.5
                        K.op("dve", lambda e: [e.max_index(out=IX[:, g, 0:8], in_max=V16[:, g, 0:8], in_values=Sg(g)) for g in gs] +
                             [e.match_replace(out=S2g(g), in_to_replace=V16[:, g, 0:8], in_values=Sg(g), imm_value=-1e30) for g in gs],
                             reads=[TS, TV16], writes=[TIX, TS2])
                        yield 1.0
                        K.op("dve", lambda e: [e.max(out=V16[:, g, 8:16], in_=S2g(g)) for g in gs], reads=[TS2], writes=[TV16])
                        yield 0.5
                        K.op("dve", lambda e: [e.max_index(out=IX[:, g, 8:16], in_max=V16[:, g, 8:16], in_values=S2g(g)) for g in gs],
                             reads=[TS2, TV16], writes=[TIX])
                        yield 0.5
                    V16r = V16[:].rearrange("p (h two) k -> p h two k", two=2)
                    cand4 = cand[:].rearrange("p (h a b) -> p h a b", h=8, a=16)
                    for hq in range(8):
                        hs = slice(hq, hq + 1)
                        K.op("dve", lambda e: e.tensor_tensor(out=cand4[:, hs], in0=V16r[:, hs, 0, :].unsqueeze(3).to_broadcast([128, 1, 16, 16]),
                                                              in1=V16r[:, hs, 1, :].unsqueeze(2).to_broadcast([128, 1, 16, 16]),
                                                              op=ALU.add), reads=[TV16], writes=[Tcand])
                        yield 0.55
                    for hq in range(8):
                        hs = range(hq, hq + 1)
                        K.op("dve", lambda e: [e.max(out=F16[:, h, 0:8], in_=Cg(h)) for h in hs], reads=[Tcand], writes=[TF16])
                        yield 0.5
                        K.op("dve", lambda e: [e.max_index(out=PS[:, h, 0:8], in_max=F16[:, h, 0:8], in_values=Cg(h)) for h in hs] +
                             [e.match_replace(out=C2g(h), in_to_replace=F16[:, h, 0:8], in_values=Cg(h), imm_value=-1e30) for h in hs],
                             reads=[Tcand, TF16], writes=[TPS, TS])
                        yield 1.0
                        K.op("dve", lambda e: [e.max(out=F16[:, h, 8:16], in_=C2g(h)) for h in hs], reads=[TS], writes=[TF16])
                        yield 0.5
                        K.op("dve", lambda e: [e.max_index(out=PS[:, h, 8:16], in_max=F16[:, h, 8:16], in_values=C2g(h)) for h in hs],
                             reads=[TS, TF16], writes=[TPS])
                        yield 0.5
                    sh3 = sh[:].rearrange("p (h k) -> p h k", k=16)
                    ex3 = ex[:].rearrange("p (h k) -> p h k", k=16)
                    K.op("dve", lambda e: e.tensor_tensor(out=sh3, in0=F16[:], in1=F16[:, :, 0:1].to_broadcast([128, 8, 16]),
                                                          op=ALU.subtract), reads=[TF16], writes=[Tsh])
                    yield 0.3
                    yield 1.5
                    yield 1.5
                    K.op("act", lambda e: e.activation(out=ex[:], in_=sh[:], func=AF.Tanh, scale=0.5), reads=[Tsh], writes=[Tex])
                    yield 0.1
                    K.op("dve", lambda e: e.tensor_scalar(out=sh[:], in0=ex[:], scalar1=-1.0, scalar2=1.0, op0=ALU.mult, op1=ALU.add),
                         reads=[Tex], writes=[Tsh])
                    yield 0.2
                    K.op("dve", lambda e: [e.reciprocal(out=sh[:], in_=sh[:])], reads=[Tsh], writes=[Tsh])
                    yield 0.2
                    K.op("dve", lambda e: e.scalar_tensor_tensor(out=ex[:], in0=ex[:], scalar=1.0, in1=sh[:], op0=ALU.add, op1=ALU.mult),
                         reads=[Tex, Tsh], writes=[Tex])
                    yield 0.2
                    PSf = PS[:].rearrange("p h k -> p (h k)")
                    K.op("dve", lambda e: [e.tensor_single_scalar(out=au[:, 0, :], in_=PSf, scalar=4, op=ALU.arith_shift_right),
                                           e.tensor_single_scalar(out=au[:, 1, :], in_=PSf, scalar=15, op=ALU.bitwise_and)],
                         reads=[TPS], writes=[Tau])
                    yield 0.3
                    K.op("dve", lambda e: [e.tensor_copy(out=apf[:], in_=au[:]), e.tensor_copy(out=IXf[:], in_=IX[:])],
                         reads=[Tau, TIX], writes=[Tapf, TIXf])
                    yield 0.5
                    IXr = IXf[:].rearrange("p (h two) k -> p h two k", two=2)
                    eqs = [S2[:].rearrange("p (h k a) -> p h k a", h=8, k=16), cand[:].rearrange("p (h k a) -> p h k a", h=8, k=16)]
                    Teqs = [TS2, Tcand]
                    for wh in range(2):
                        pos3 = apf[:, wh, :].rearrange("p (h k) -> p h k", k=16)
                        for hq in range(8):
                            hs = slice(hq, hq + 1)
                            K.op("dve", lambda e: e.tensor_tensor(
                                out=eqs[wh][:, hs], in0=pos3[:, hs].unsqueeze(3).to_broadcast([128, 1, 16, 16]),
                                in1=io128[:, 0:16].unsqueeze(1).unsqueeze(1).to_broadcast([128, 1, 16, 16]), op=ALU.is_equal),
                                 reads=[Tapf, Tio], writes=[Teqs[wh]])
                            yield 0.55
                        for hq in range(8):
                            hs = slice(hq, hq + 1)
                            K.op("dve", lambda e: e.tensor_tensor(out=eqs[wh][:, hs], in0=eqs[wh][:, hs],
                                                                  in1=IXr[:, hs, wh, :].unsqueeze(2).to_broadcast([128, 1, 16, 16]),
                                                                  op=ALU.mult), reads=[Teqs[wh], TIXf], writes=[Teqs[wh]])
                            yield 0.55
                    for wh in range(2):
                        for hq in range(4):
                            hs = slice(2 * hq, 2 * hq + 2)
                            K.op("dve", lambda e: e.reduce_sum(out=IJG[:, wh, :].rearrange("p (h k) -> p h k", k=16)[:, hs],
                                                               in_=eqs[wh][:, hs], axis=AX.X), reads=[Teqs[wh]], writes=[TIJG])
                            yield 0.55
                        if wh == 0:
                            K.op("dve", lambda e: e.reduce_sum(out=Zs[:], in_=ex3, axis=AX.X), reads=[Tex], writes=[TZs])
                            yield 0.2
                            K.op("dve", lambda e: e.reciprocal(out=rZ[:], in_=Zs[:]), reads=[TZs], writes=[TrZ])
                            yield 0.1
                            K.op("dve", lambda e: e.tensor_tensor(out=IJG[:, 2, :].rearrange("p (h k) -> p h k", k=16), in0=ex3,
                                                                  in1=rZ[:].unsqueeze(2).to_broadcast([128, 8, 16]), op=ALU.mult),
                                 reads=[Tex, TrZ], writes=[TIJG])
                            yield 0.1
                    yield 1.5
                    yield 1.5
                    K.op("pe", lambda e: [e.transpose(out=MB[0][:, w * 128:(w + 1) * 128], in_=IJG[:, w, :], identity=idf[:])
                                          for w in range(3)], reads=[TIJG, Tidf], writes=[TMB[0]])
                    K.op("act", lambda e: e.activation(out=IJGT[pb][:, :, j * 128:(j + 1) * 128],
                                                       in_=MB[0][:, 0:384].rearrange("p (w t) -> p w t", w=3),
                                                       func=AF.Copy), writes=[TIJGT[pb], TMB[0]])
                    yield 0.3

            def part2(b):
                pb = b % 2
                IT = IJGT[pb]
                for sbi in range(TB // NSB):
                    sl = sbi % 2
                    t0 = sbi * NSB
                    K.op("dve", lambda e: [e.tensor_scalar(out=Qb[sl][:, i, :], in0=iob[:], scalar1=IT[:, 1, t0 + i:t0 + i + 1],
                                                           scalar2=None, op0=ALU.is_equal) for i in range(NSB)] +
                         [e.tensor_scalar(out=Pb[sl][:, i, :], in0=iob[:], scalar1=IT[:, 0, t0 + i:t0 + i + 1],
                                          scalar2=IT[:, 2, t0 + i:t0 + i + 1], op0=ALU.is_equal, op1=ALU.mult)
                          for i in range(NSB)],
                         reads=[Tiob, TIJGT[pb]], writes=[TQb[sl], TPb[sl]])
                    for q4 in range(NSB // 4):
                        cnt = sbi * (NSB // 4) + q4
                        mi = cnt % 3
                        tt = t0 + q4 * 4
                        K.op("pe", lambda e: [e.matmul(MB[mi][:, i * 128:(i + 1) * 128], lhsT=Qb[sl][:, q4 * 4 + i, :],
                                                       rhs=Pb[sl][:, q4 * 4 + i, :], start=True, stop=True) for i in range(4)],
                             reads=[TQb[sl], TPb[sl]], writes=[TMB[mi]])
                        src = MB[mi][:].rearrange("p (t i) -> p i t", t=4)
                        K.op("act", lambda e: e.activation(out=Gbuf[:, :, tt:tt + 4], in_=src, func=AF.Copy),
                             writes=[TG, TMB[mi]])

            for gi in range(NBUF - 1):
                load_chunk(gi)
            for _ in part1(0):
                pass
            chk(51)

            def epi_adds(b):
                K.op("dve", lambda e: [e.tensor_tensor(out=xe[:, j, hf * 512:(hf + 1) * 512], in0=xe[:, j, hf * 512:(hf + 1) * 512],
                                                       in1=opsb[2 * j + hf][:], op=ALU.add)
                                       for j in range(2) for hf in range(2)], reads=[Txe], writes=[Txe] + Tops)

            def epi_rest(b):
                t0_ = b * TB
                for j in range(2):
                    K.op("act", lambda e: e.activation(out=h2bf[:, j, :], in_=xe[:, j, :], func=AF.Square,
                                                       accum_out=ss2[:, j:j + 1]), reads=[Txe], writes=[Th2bf, Tss2])
                    yield 0.1
                rstd(ss2, Tss2, ms2, Tms2, rs2, Trs2)
                yield 0.1
                for j in range(2):
                    for hf in range(2):
                        cs = slice(hf * 512, (hf + 1) * 512)
                        K.op("dve", lambda e: e.scalar_tensor_tensor(out=xe[:, j, cs], in0=xe[:, j, cs], scalar=rs2[:, j:j + 1],
                                                                     in1=gfin[:, cs], op0=ALU.mult, op1=ALU.mult),
                             reads=[Txe, Trs2, Tgfin], writes=[Txe])
                        yield 0.6
                K.dma("sp", lambda e: e.dma_start(out=y[t0_:t0_ + TB, :].rearrange("(j p) d -> p j d", p=128), in_=xe[:]),
                      reads=[Txe], sem_of=Tst[b % 2])
                yield 0.1

            def chain(*gens):
                for g in gens:
                    if g is not None:
                        for v in g:
                            yield v

            for b in range(NBLK):
                tok0 = b * TB
                pb = b % 2
                part2(b)
                chk(52)
                chk(60 + b)
                if b > 0:
                    epi_adds(b - 1)
                bg = chain(epi_rest(b - 1) if b > 0 else None, part1(b + 1) if b + 1 < NBLK else None)

                def emitA(c):
                    gi = b * NCH + c
                    sl = gi % NBUF
                    ai = c % 2
                    K.op("pe", lambda e: [e.matmul(Apsb[ai][:, 0:TB], lhsT=UT[sl][:, k * 128:(k + 1) * 128], rhs=h2T[pb][:, k, :],
                                                   start=(k == 0), stop=(k == 7)) for k in range(8)],
                         reads=[TUT[sl], Th2T[pb]], writes=[TAps[ai]])
                    K.op("act", lambda e: e.activation(out=gl[ai][:], in_=Apsb[ai][:, 0:TB], func=AF.Gelu),
                         writes=[Tgl[ai], TAps[ai]])
                    K.op("pool", lambda e: e.tensor_tensor(out=Wb[c % 4][:], in0=gl[ai][:], in1=Gbuf[:, c, :], op=ALU.mult),
                         reads=[Tgl[ai], TG], writes=[TW[c % 4]])

                def emitV(c):
                    gi = b * NCH + c
                    sl = gi % NBUF
                    K.op("pe", lambda e: [e.matmul(opsb[2 * j + hf][:], lhsT=Wb[c % 4][:, j * 128:(j + 1) * 128],
                                                   rhs=Vc[sl][:, hf * 512:(hf + 1) * 512], start=(c == 0), stop=(c == NCH - 1))
                                          for j in range(2) for hf in range(2)],
                         reads=[TW[c % 4], TVc[sl]], writes=Tops, acc=(Tops if c > 0 else ()))

                emitA(0)
                emitA(1)
                for c in range(NCH):
                    if c + 2 < NCH:
                        emitA(c + 2)
                    emitV(c)
                    load_chunk(b * NCH + c + NBUF - 1)
                    if c == NCH - 24:
                        K.dma("sp", lambda e: e.dma_start(out=xe[:], in_=x1d[tok0:tok0 + TB, :].rearrange("(j p) d -> p j d", p=128)),
                              reads=[Tx1d[b]], writes=[Txe])
                    if bg is not None and c >= 2:
                        budget = 1.45
                        while budget > 0:
                            try:
                                budget -= next(bg)
                            except StopIteration:
                                bg = None
                                break
                if bg is not None:
                    for _ in bg:
                        pass

                chk(53)
                chk(70 + b)
                chk(54)
            epi_adds(NBLK - 1)
            for _ in epi_rest(NBLK - 1):
                pass
            K.barrier()
    return nc


def _prep_weights(inputs):
    f = lambda a: np.ascontiguousarray(np.asarray(a, dtype=np.float32))
    L = 0
    eu = np.asarray(inputs["expert_u"], dtype=np.float32)[L]
    uT = np.ascontiguousarray(eu.reshape(NCH, 128, 8, 128).transpose(0, 3, 2, 1)).reshape(NEXP, D)
    sk = np.asarray(inputs["sub_keys"], dtype=np.float32)[L]
    keysT = np.ascontiguousarray(sk.reshape(16, 128, 128).transpose(2, 0, 1))
    gwt = np.ascontiguousarray(np.asarray(inputs["pool_group_w"], dtype=np.float32)[L].transpose(1, 0, 2))
    pscale = np.ascontiguousarray(np.asarray(inputs["pool_scale"], dtype=np.float32)[L].reshape(4, 128).T)
    cwt = np.ascontiguousarray(np.asarray(inputs["conv_w"], dtype=np.float32)[L].reshape(3, 4, 128).transpose(2, 1, 0))
    return dict(
        w_in=f(inputs["w_in"][L]), w_bp=f(inputs["w_branch_pool"][L]), w_bc=f(inputs["w_branch_conv"][L]),
        w_out=f(inputs["w_out"][L]), w_q=f(inputs["w_q"][L]), gw=gwt, pscale=pscale, cw=cwt,
        gmix=f(inputs["g_mix"][L]), gffn=f(inputs["g_ffn"][L]), gfin=f(inputs["g_final"]),
        keysT=keysT, uT=uT, v=f(np.asarray(inputs["expert_v"])[L]),
    )


def run(inputs, ncores, ntok, seq, phase='AB'):
    x = np.asarray(inputs["x"], dtype=np.float32)
    B, S_, _ = x.shape
    xf = np.ascontiguousarray(x.reshape(B * S_, D))
    assert B * S_ == ncores * ntok
    wts = _prep_weights(inputs)
    nc = build(ntok, seq, phase)
    in_maps = []
    for c in range(ncores):
        m = dict(wts)
        m["x"] = xf[c * ntok:(c + 1) * ntok]
        in_maps.append(m)
    res = run_bass_kernel_spmd(nc, in_maps, core_ids=list(range(ncores)))
    out = np.concatenate([np.asarray(r["y"]) for r in res.results], axis=0)
    return out.reshape(B, S_, D).astype(np.float32)


def kernel(**inputs):
    return run(inputs, 8, 8192, 4096)
```

```python
import numpy as np
from contextlib import ExitStack
import concourse.bass as bass
import concourse.mybir as mybir
from concourse.bass_utils import run_bass_kernel_spmd

F32 = mybir.dt.float32
BF16 = mybir.dt.bfloat16
U32 = mybir.dt.uint32
ALU = mybir.AluOpType
AF = mybir.ActivationFunctionType
AX = mybir.AxisListType

D = 1024
DIN = 4096
NEXP = 16384
NCH = 128
TB = 256
EPS = 1e-6
NBUF = 6
NSB = 8


class T:
    __slots__ = ("name", "w", "r", "dsem")

    def __init__(self, name):
        self.name = name
        self.w = None
        self.r = {}
        self.dsem = None


class Sem:
    __slots__ = ("h", "n")

    def __init__(self, h):
        self.h = h
        self.n = 0


import os
class _Stop(Exception):
    pass


_STOP = [False]


def chk(n):
    return


class Ctx:
    def __init__(self, nc, es):
        self.nc, self.es = nc, es
        self.E = dict(pe=nc.tensor, act=nc.scalar, dve=nc.vector, pool=nc.gpsimd, sp=nc.sync)
        self.allsems = []
        self.esem = {k: self.newsem("e_" + k) for k in ("pe", "act", "dve", "pool")}
        self.seen = {k: {} for k in self.E}

    def newsem(self, name):
        s = Sem(self.es.enter_context(self.nc.semaphore(name)))
        self.allsems.append(s)
        return s

    def _wait(self, eng, need):
        seen = self.seen[eng]
        for s, v in need.items():
            if seen.get(s, 0) < v:
                self.E[eng].wait_ge(s.h, v)
                seen[s] = v

    def _need(self, eng, reads, writes, acc, skip=None):
        need = {}

        def add(s, v):
            if s is skip:
                return
            if need.get(s, 0) < v:
                need[s] = v
        me = self.esem.get(eng)
        for t in reads:
            if t.w is not None:
                add(*t.w)
        for t in writes:
            if t.w is not None and t.w[0] is not me:
                add(*t.w)
            for s, v in t.r.items():
                if s is not me:
                    add(s, v)
        return need

    def _mark(self, s, v, reads, writes):
        for t in reads:
            if t.r.get(s, 0) < v:
                t.r[s] = v
        for t in writes:
            t.w = (s, v)
            t.r = {}

    def op(self, eng, fn, reads=(), writes=(), acc=()):
        if _STOP[0]:
            return
        self._wait(eng, self._need(eng, reads, writes, acc))
        inst = fn(self.E[eng])
        if isinstance(inst, (list, tuple)):
            inst = inst[-1]
        s = self.esem[eng]
        s.n += 1
        inst.then_inc(s.h, 1)
        self._mark(s, s.n, reads, writes)

    def dma(self, eng, fn, reads=(), writes=(), sem_of=None, group=False):
        if _STOP[0]:
            return
        t = sem_of if sem_of is not None else (writes[0] if writes else reads[0])
        if t.dsem is None:
            t.dsem = self.newsem("d_" + t.name)
        self._wait(eng, self._need(eng, reads, writes, (), skip=(t.dsem if group else None)))
        inst = fn(self.E[eng])
        t.dsem.n += 16
        inst.then_inc(t.dsem.h, 16)
        self._mark(t.dsem, t.dsem.n, reads, writes)

    def barrier(self, engs=("pe", "act", "dve", "pool", "sp")):
        for e in engs:
            self._wait(e, {s: s.n for s in self.allsems if s.n > 0})


def build(NTOK, SEQ, phase='AB'):
    _STOP[0] = False
    nc = bass.Bass("TRN2", target_bir_lowering=False)
    NBLK = NTOK // TB
    BPS = SEQ // TB

    def din(name, shape, dt=F32):
        return nc.dram_tensor(name, shape, dt, kind="ExternalInput").ap()

    x = din("x", [NTOK, D])
    w_in = din("w_in", [D, DIN])
    w_bp = din("w_bp", [512, D])
    w_bc = din("w_bc", [512, D])
    w_out = din("w_out", [D, D])
    w_q = din("w_q", [D, 2048])
    gw = din("gw", [128, 4, 128])
    pscale_d = din("pscale", [128, 4])
    cw_d = din("cw", [128, 4, 3])
    gmix_d = din("gmix", [D])
    gffn_d = din("gffn", [D])
    gfin_d = din("gfin", [D])
    keysT = din("keysT", [128, 16, 128])
    uT = din("uT", [NEXP, D])
    vv = din("v", [NEXP, D])
    y = nc.dram_tensor("y", [NTOK, D], F32, kind="ExternalOutput").ap()
    x1d = nc.dram_tensor("x1d", [NTOK, D], F32, kind="Internal").ap()
    uTb = nc.dram_tensor("uTb", [NEXP, D], BF16, kind="Internal").ap()
    vb = nc.dram_tensor("vb", [NEXP, D], BF16, kind="Internal").ap()
    wqb = nc.dram_tensor("wqb", [D, 2048], BF16, kind="Internal").ap()

    with ExitStack() as es:
        K = Ctx(nc, es)

        def sbt(st, name, shape, dt):
            return st.enter_context(nc.sbuf_tensor(name, shape, dt))

        def pst(st, name, shape, dt):
            return st.enter_context(nc.psum_tensor(name, shape, dt))

        io128 = sbt(es, "io128", [128, 128], F32); Tio = T("io128")
        pidx = sbt(es, "pidx", [128, 1], F32); Tpidx = T("pidx")
        idf = sbt(es, "idf", [128, 128], F32); Tidf = T("idf")
        idb = sbt(es, "idb", [128, 128], BF16); Tidb = T("idb")
        K.op("pool", lambda e: e.iota(io128[:], pattern=[[1, 128]], base=0, channel_multiplier=0,
                                      allow_small_or_imprecise_dtypes=True), writes=[Tio])
        K.op("pool", lambda e: e.iota(pidx[:], pattern=[[0, 1]], base=0, channel_multiplier=1,
                                      allow_small_or_imprecise_dtypes=True), writes=[Tpidx])
        K.op("dve", lambda e: e.tensor_scalar(out=idf[:], in0=io128[:], scalar1=pidx[:, 0:1], scalar2=None,
                                              op0=ALU.is_equal), reads=[Tio, Tpidx], writes=[Tidf])
        K.op("dve", lambda e: e.tensor_copy(out=idb[:], in_=idf[:]), reads=[Tidf], writes=[Tidb])

        Texp = T("experts")
        Tx1d = [T("x1d%d" % b) for b in range(NBLK)]
        Tst = [T("st0"), T("st1")]

        with ExitStack() as ea:
            w_in_bf = sbt(ea, "w_in_bf", [128, 8, DIN], BF16); Twin = T("w_in")
            w_bp_bf = sbt(ea, "w_bp_bf", [128, 4, D], BF16); Twbp = T("w_bp")
            w_bc_bf = sbt(ea, "w_bc_bf", [128, 4, D], BF16); Twbc = T("w_bc")
            w_out_bf = sbt(ea, "w_out_bf", [128, 8, D], BF16); Twout = T("w_out")
            gw_bf = sbt(ea, "gw_bf", [128, 4, 128], BF16); Tgw = T("gw")
            pscale = sbt(ea, "pscale_s", [128, 4], F32); Tpsc = T("pscale")
            cw = sbt(ea, "cw_s", [128, 4, 3], F32); Tcw = T("cw")
            gmix = sbt(ea, "gmix_s", [128, D], F32); Tgmix = T("gmix")
            invcnt = sbt(ea, "invcnt", [128, 4, 16], F32); Tinv = T("invcnt")
            io16p = sbt(ea, "io16p", [128, 16], F32); Tio16p = T("io16p")
            xblk = [sbt(ea, "xblk%d" % i, [128, 2, D], F32) for i in range(2)]
            Txblk = [T("xblk0"), T("xblk1")]
            junk = sbt(ea, "junkA", [128, D], BF16); Tjunk = T("junk")
            ss = sbt(ea, "ssA", [128, 2], F32); Tss = T("ss")
            sd = sbt(ea, "sdA", [128, 2], F32); Tsd = T("sd")
            rs = sbt(ea, "rsA", [128, 2], F32); Trs = T("rs")
            hbf = sbt(ea, "hbf", [128, 2, D], BF16); Thbf = T("hbf")
            hT = sbt(ea, "hT", [128, 8, TB], BF16); ThT = T("hT")
            zp = [sbt(ea, "zp%d" % i, [128, 4, 16 + TB], F32) for i in range(2)]
            Tzp = [T("zp0"), T("zp1")]
            sA = sbt(ea, "sA", [128, 4, 16 + TB], F32); TsA = T("sA")
            sB = sbt(ea, "sB", [128, 4, 16 + TB], F32); TsB = T("sB")
            tmpc = sbt(ea, "tmpc", [128, 4, 16], F32); Ttmpc = T("tmpc")
            zc = sbt(ea, "zc", [128, 12, TB], F32); Tzc = T("zc")
            ub = [sbt(ea, "ub%d" % i, [128, 4, 2 + TB], F32) for i in range(2)]
            Tub = [T("ub0"), T("ub1")]
            yb = sbt(ea, "yb", [128, 4, TB], F32); Tyb = T("yb")
            sig = sbt(ea, "sig", [128, 16, TB], F32); Tsig = T("sig")
            pbf = sbt(ea, "pbf", [128, 4, TB], BF16); Tpbf = T("pbf")
            pmbf = sbt(ea, "pmbf", [128, 4, TB], BF16); Tpmbf = T("pmbf")
            cvbf = sbt(ea, "cvbf", [128, 4, TB], BF16); Tcvbf = T("cvbf")
            t12 = sbt(ea, "t12", [128, 2, TB], F32); Tt12 = T("t12")
            mbf = sbt(ea, "mbf", [128, 8, TB], BF16); Tmbf = T("mbf")
            tps = pst(ea, "tpsA", [128, 8, 128], BF16); Ttps = T("tps")
            PBA = [pst(ea, "pbA%d" % i, [128, 512], F32) for i in range(7)]
            TPBA = [T("pbA%d" % i) for i in range(7)]
            zpsb = [PBA[0], PBA[1]]; Tzps = [TPBA[0], TPBA[1]]
            gpsb = [PBA[2], PBA[3]]; Tgpsb = [TPBA[2], TPBA[3]]
            abps = [PBA[4], PBA[5]]; Tabps = [TPBA[4], TPBA[5]]
            xops = [PBA[6], PBA[2]]; Txops = [TPBA[6], TPBA[2]]
            gpsv = lambda g: gpsb[g // 2][:, (g % 2) * TB:(g % 2 + 1) * TB]

            for k in range(8):
                for pc in range(4):
                    K.dma("pool", lambda e, k=k, pc=pc: e.dma_start(
                        out=w_in_bf[:, k, pc * 1024:(pc + 1) * 1024],
                        in_=w_in[k * 128:(k + 1) * 128, pc * 1024:(pc + 1) * 1024]), writes=[Twin], group=True)
            K.dma("pool", lambda e: e.dma_start(out=gw_bf[:], in_=gw), writes=[Tgw])
            for g in range(4):
                K.dma("pool", lambda e, g=g: e.dma_start(out=w_bp_bf[:, g, :], in_=w_bp[g * 128:(g + 1) * 128, :]),
                      writes=[Twbp], group=True)
                K.dma("pool", lambda e, g=g: e.dma_start(out=w_bc_bf[:, g, :], in_=w_bc[g * 128:(g + 1) * 128, :]),
                      writes=[Twbc], group=True)
            for k in range(8):
                K.dma("pool", lambda e, k=k: e.dma_start(out=w_out_bf[:, k, :], in_=w_out[k * 128:(k + 1) * 128, :]),
                      writes=[Twout], group=True)
            K.dma("sp", lambda e: e.dma_start(out=pscale[:], in_=pscale_d), writes=[Tpsc])
            K.dma("sp", lambda e: e.dma_start(out=cw[:], in_=cw_d), writes=[Tcw])
            K.dma("sp", lambda e: e.dma_start(out=gmix[:], in_=gmix_d.partition_broadcast(128)), writes=[Tgmix])
            NPIECE = 8
            RP = NEXP // NPIECE
            for pc in range(NPIECE if 'B' in phase else 0):
                K.dma("pool", lambda e, pc=pc: e.dma_start(out=uTb[pc * RP:(pc + 1) * RP, :],
                                                           in_=uT[pc * RP:(pc + 1) * RP, :]),
                      writes=[Texp], group=True)
                K.dma("pool", lambda e, pc=pc: e.dma_start(out=vb[pc * RP:(pc + 1) * RP, :],
                                                           in_=vv[pc * RP:(pc + 1) * RP, :]),
                      writes=[Texp], group=True)
            K.op("dve", lambda e: e.tensor_scalar(out=io16p[:], in0=io128[:, 0:16], scalar1=1.0, scalar2=None,
                                                  op0=ALU.add), reads=[Tio], writes=[Tio16p])
            K.op("dve", lambda e: [e.tensor_scalar(out=invcnt[:, g, :], in0=io16p[:], scalar1=float(2 ** (g + 1)),
                                                   scalar2=None, op0=ALU.min) for g in range(4)],
                 reads=[Tio16p], writes=[Tinv])
            K.op("dve", lambda e: e.reciprocal(out=invcnt[:], in_=invcnt[:]), reads=[Tinv], writes=[Tinv])

            def headA_load(b):
                par = b % 2
                tok0 = b * TB
                K.dma("sp", lambda e: e.dma_start(out=xblk[par][:], in_=x[tok0:tok0 + TB, :].rearrange("(j p) d -> p j d", p=128)),
                      writes=[Txblk[par]])

            def headA(b):
                par = b % 2
                X = xblk[par]
                TX = Txblk[par]
                for j in range(2):
                    K.op("act", lambda e, j=j: e.activation(out=junk[:], in_=X[:, j, :], func=AF.Square,
                                                            accum_out=ss[:, j:j + 1]),
                         reads=[TX], writes=[Tjunk, Tss])
                K.op("act", lambda e: e.activation(out=sd[:], in_=ss[:], func=AF.Sqrt, scale=1.0 / D, bias=EPS),
                     reads=[Tss], writes=[Tsd])
                K.op("dve", lambda e: e.reciprocal(out=rs[:], in_=sd[:]), reads=[Tsd], writes=[Trs])
                K.op("dve", lambda e: [e.scalar_tensor_tensor(out=hbf[:, j, :], in0=X[:, j, :], scalar=rs[:, j:j + 1],
                                                              in1=gmix[:], op0=ALU.mult, op1=ALU.mult)
                                       for j in range(2)],
                     reads=[TX, Trs, Tgmix], writes=[Thbf])
            chk(1)
            for b in range(NBLK):
                par = b % 2
                first = (b % BPS == 0)
                tok0 = b * TB
                X = xblk[par]
                TX = Txblk[par]
                Z = zp[par]
                TZ = Tzp[par]
                U = ub[par]
                TU = Tub[par]
                if b == 0:
                    headA_load(0)
                    headA(0)
                if b + 1 < NBLK:
                    headA_load(b + 1)
                chk(2)
                for j in range(2):
                    K.op("pe", lambda e, j=j: [e.transpose(out=tps[:, k, :], in_=hbf[:, j, k * 128:(k + 1) * 128],
                                                           identity=idb[:]) for k in range(8)],
                         reads=[Thbf, Tidb], writes=[Ttps])
                    if j == 0:
                        K.op("act", lambda e, j=j: e.activation(out=hT[:, :, j * 128:(j + 1) * 128], in_=tps[:],
                                                                func=AF.Copy), writes=[ThT, Ttps])
                    else:
                        K.op("dve", lambda e, j=j: e.tensor_copy(out=hT[:, :, j * 128:(j + 1) * 128], in_=tps[:]),
                             writes=[ThT, Ttps])
                chk(3)
                if first:
                    K.op("dve", lambda e: [e.memset(Z[:, :, 0:16], 0.0), e.memset(U[:, :, 0:2], 0.0)],
                         writes=[TZ, TU])
                else:
                    K.op("dve", lambda e: [e.tensor_copy(out=Z[:, :, 0:16], in_=zp[1 - par][:, :, TB:TB + 16]),
                                           e.tensor_copy(out=U[:, :, 0:2], in_=ub[1 - par][:, :, TB:TB + 2])],
                         reads=[Tzp[1 - par], Tub[1 - par]], writes=[TZ, TU])
                chk(31)
                for oc in range(32):
                    zi = oc % 2
                    K.op("pe", lambda e, oc=oc, zi=zi: [e.matmul(zpsb[zi][:, 0:TB], lhsT=w_in_bf[:, k, oc * 128:(oc + 1) * 128],
                                                                 rhs=hT[:, k, :], start=(k == 0), stop=(k == 7))
                                                        for k in range(8)],
                         reads=[ThT, Twin], writes=[Tzps[zi]])
                    if oc == 0:
                        chk(321)
                    if oc < 4:
                        K.op("act", lambda e, oc=oc, zi=zi: e.activation(out=Z[:, oc, 16:16 + TB], in_=zpsb[zi][:, 0:TB],
                                                                         func=AF.Copy), writes=[TZ, Tzps[zi]])
                    elif oc < 16:
                        K.op("act", lambda e, oc=oc, zi=zi: e.activation(out=zc[:, oc - 4, :], in_=zpsb[zi][:, 0:TB],
                                                                         func=AF.Copy), writes=[Tzc, Tzps[zi]])
                    else:
                        K.op("act", lambda e, oc=oc, zi=zi: e.activation(out=sig[:, oc - 16, :], in_=zpsb[zi][:, 0:TB],
                                                                         func=AF.Sigmoid), writes=[Tsig, Tzps[zi]])
                    if oc == 2:
                        chk(32)
                    if oc == 0:
                        chk(322)
                    if oc == 3:
                        W1 = 16 + TB
                        K.op("dve", lambda e: e.tensor_tensor(out=sA[:, :, 1:W1], in0=Z[:, :, 1:W1], in1=Z[:, :, 0:W1 - 1],
                                                              op=ALU.add), reads=[TZ], writes=[TsA])
                        K.op("dve", lambda e: [
                            e.scalar_tensor_tensor(out=pbf[:, 0, :], in0=sA[:, 0, 16:W1], scalar=0.5, in1=Z[:, 0, 16:W1],
                                                   op0=ALU.mult, op1=ALU.subtract),
                            e.tensor_tensor(out=sB[:, 1:4, 3:W1], in0=sA[:, 1:4, 3:W1], in1=sA[:, 1:4, 1:W1 - 2],
                                            op=ALU.add)],
                             reads=[TsA, TZ], writes=[Tpbf, TsB])
                        K.op("dve", lambda e: [
                            e.scalar_tensor_tensor(out=pbf[:, 1, :], in0=sB[:, 1, 16:W1], scalar=0.25, in1=Z[:, 1, 16:W1],
                                                   op0=ALU.mult, op1=ALU.subtract),
                            e.tensor_tensor(out=sA[:, 2:4, 7:W1], in0=sB[:, 2:4, 7:W1], in1=sB[:, 2:4, 3:W1 - 4],
                                            op=ALU.add)],
                             reads=[TsB, TZ], writes=[Tpbf, TsA])
                        K.op("dve", lambda e: [
                            e.scalar_tensor_tensor(out=pbf[:, 2, :], in0=sA[:, 2, 16:W1], scalar=0.125, in1=Z[:, 2, 16:W1],
                                                   op0=ALU.mult, op1=ALU.subtract),
                            e.tensor_tensor(out=sB[:, 3, 15:W1], in0=sA[:, 3, 15:W1], in1=sA[:, 3, 7:W1 - 8],
                                            op=ALU.add)],
                             reads=[TsA, TZ], writes=[Tpbf, TsB])
                        K.op("dve", lambda e: e.scalar_tensor_tensor(out=pbf[:, 3, :], in0=sB[:, 3, 16:W1], scalar=1.0 / 16,
                                                                     in1=Z[:, 3, 16:W1], op0=ALU.mult, op1=ALU.subtract),
                             reads=[TsB, TZ], writes=[Tpbf])
                        if first:
                            srcs = [sA[:, 0, 16:32], sB[:, 1, 16:32], sA[:, 2, 16:32], sB[:, 3, 16:32]]
                            K.op("dve", lambda e: [e.tensor_tensor(out=tmpc[:, g, :], in0=srcs[g], in1=invcnt[:, g, :],
                                                                   op=ALU.mult) for g in range(4)],
                                 reads=[TsA, TsB, Tinv], writes=[Ttmpc])
                            K.op("dve", lambda e: e.tensor_tensor(out=pbf[:, :, 0:16], in0=tmpc[:], in1=Z[:, :, 16:32],
                                                                  op=ALU.subtract), reads=[Ttmpc, TZ], writes=[Tpbf])
                    if oc == 9:
                        K.op("pe", lambda e: [e.matmul(gpsv(g), lhsT=gw_bf[:, g, :], rhs=pbf[:, g, :], start=True,
                                                       stop=True) for g in range(4)],
                             reads=[Tpbf, Tgw], writes=Tgpsb)
                        K.op("dve", lambda e: [e.tensor_scalar(out=pmbf[:, g, :], in0=gpsv(g), scalar1=pscale[:, g:g + 1],
                                                               scalar2=None, op0=ALU.mult) for g in range(4)],
                             reads=[Tpsc], writes=[Tpmbf] + Tgpsb)
                    if oc == 4:
                        chk(34)
                    if oc == 15:
                        chk(35)
                        K.op("dve", lambda e: e.tensor_tensor(out=U[:, :, 2:2 + TB], in0=zc[:, 0:4, :], in1=zc[:, 8:12, :],
                                                              op=ALU.mult), reads=[Tzc], writes=[TU])
                        K.op("dve", lambda e: [e.tensor_scalar(out=yb[:, ch, :], in0=U[:, ch, 2:2 + TB],
                                                               scalar1=cw[:, ch, 2:3], scalar2=None, op0=ALU.mult)
                                               for ch in range(4)], reads=[TU, Tcw], writes=[Tyb])
                        for kk in (1, 0):
                            K.op("dve", lambda e, kk=kk: [e.scalar_tensor_tensor(out=yb[:, ch, :], in0=U[:, ch, kk:kk + TB],
                                                                                 scalar=cw[:, ch, kk:kk + 1], in1=yb[:, ch, :],
                                                                                 op0=ALU.mult, op1=ALU.add)
                                                          for ch in range(4)], reads=[TU, Tcw, Tyb], writes=[Tyb])
                        K.op("dve", lambda e: e.tensor_tensor(out=cvbf[:], in0=yb[:], in1=zc[:, 4:8, :], op=ALU.mult),
                             reads=[Tyb, Tzc], writes=[Tcvbf])
                chk(4)
                for dc in range(8):
                    ab = abps[dc % 2]
                    Tab = Tabps[dc % 2]
                    K.op("pe", lambda e, dc=dc, ab=ab: (
                        [e.matmul(ab[:, 0:TB], lhsT=w_bp_bf[:, g, dc * 128:(dc + 1) * 128], rhs=pmbf[:, g, :],
                                  start=(g == 0), stop=(g == 3)) for g in range(4)] +
                        [e.matmul(ab[:, TB:2 * TB], lhsT=w_bc_bf[:, g, dc * 128:(dc + 1) * 128], rhs=cvbf[:, g, :],
                                  start=(g == 0), stop=(g == 3)) for g in range(4)]),
                         reads=[Tpmbf, Tcvbf, Twbp, Twbc], writes=[Tab])
                    K.op("dve", lambda e, dc=dc, ab=ab: [
                        e.tensor_tensor(out=t12[:, 0, :], in0=sig[:, dc, :], in1=ab[:, 0:TB], op=ALU.mult),
                        e.tensor_tensor(out=t12[:, 1, :], in0=sig[:, 8 + dc, :], in1=ab[:, TB:2 * TB], op=ALU.mult)],
                         reads=[Tsig], writes=[Tt12, Tab])
                    K.op("dve", lambda e, dc=dc: e.tensor_tensor(out=mbf[:, dc, :], in0=t12[:, 0, :], in1=t12[:, 1, :],
                                                                 op=ALU.add), reads=[Tt12], writes=[Tmbf])
                chk(5)
                if b + 1 < NBLK:
                    headA(b + 1)
                for j in range(2):
                    for hf in range(2):
                        xi = (2 * j + hf) % 2
                        K.op("pe", lambda e, j=j, hf=hf, xi=xi: [
                            e.matmul(xops[xi][:], lhsT=mbf[:, k, j * 128:(j + 1) * 128],
                                     rhs=w_out_bf[:, k, hf * 512:(hf + 1) * 512], start=(k == 0), stop=(k == 7))
                            for k in range(8)], reads=[Tmbf, Twout], writes=[Txops[xi]])
                        K.op("dve", lambda e, j=j, hf=hf, xi=xi: e.tensor_tensor(
                            out=X[:, j, hf * 512:(hf + 1) * 512], in0=X[:, j, hf * 512:(hf + 1) * 512], in1=xops[xi][:],
                            op=ALU.add), reads=[TX], writes=[TX, Txops[xi]])
                dst = x1d if 'B' in phase else y
                K.dma("sp", lambda e: e.dma_start(out=dst[tok0:tok0 + TB, :].rearrange("(j p) d -> p j d", p=128), in_=X[:]),
                      reads=[TX], writes=[Tx1d[b]], sem_of=Tst[par])
            K.barrier()

        with ExitStack() as eb:
            if 'B' not in phase:
                return nc
            keys_bf = sbt(eb, "keys_bf", [128, 16, 128], BF16); Tkeys = T("keys")
            gffn = sbt(eb, "gffn_s", [128, D], F32); Tgffn = T("gffn")
            gfin = sbt(eb, "gfin_s", [128, D], F32); Tgfin = T("gfin")
            iob = sbt(eb, "iob", [128, 128], BF16); Tiob = T("iob")
            ecst = sbt(eb, "ecst", [128, 128], F32); Tecst = T("ecst")
            nhalf = sbt(eb, "nhalf", [128, 2], F32); Tnhalf = T("nhalf")
            wqr = [sbt(eb, "wqr%d" % i, [128, 8, 256], BF16) for i in range(2)]; Twqr = [T("wqr0"), T("wqr1")]
            xp = sbt(eb, "xp", [128, 2, D], F32); Txp = T("xp")
            xe = sbt(eb, "xe", [128, 2, D], F32); Txe = T("xe")
            ss1 = sbt(eb, "ss1", [128, 2], F32); Tss1 = T("ss1")
            ms1 = sbt(eb, "ms1", [128, 2], F32); Tms1 = T("ms1")
            rs1 = sbt(eb, "rs1", [128, 2], F32); Trs1 = T("rs1")
            ss2 = sbt(eb, "ss2", [128, 2], F32); Tss2 = T("ss2")
            ms2 = sbt(eb, "ms2", [128, 2], F32); Tms2 = T("ms2")
            rs2 = sbt(eb, "rs2", [128, 2], F32); Trs2 = T("rs2")
            h2bf = sbt(eb, "h2bf", [128, 2, D], BF16); Th2bf = T("h2bf")
            h2T = [sbt(eb, "h2T%d" % i, [128, 8, TB], BF16) for i in range(2)]; Th2T = [T("h2T0"), T("h2T1")]
            qT = sbt(eb, "qT", [128, 16, TB], BF16); TqT = T("qT")
            S = sbt(eb, "S", [128, 2048], F32); TS = T("S")
            S2 = sbt(eb, "S2", [128, 2048], F32); TS2 = T("S2")
            cand = sbt(eb, "cand", [128, 2048], F32); Tcand = T("cand")
            V16 = sbt(eb, "V16", [128, 16, 16], F32); TV16 = T("V16")
            IX = sbt(eb, "IX", [128, 16, 16], U32); TIX = T("IX")
            IXf = sbt(eb, "IXf", [128, 16, 16], F32); TIXf = T("IXf")
            F16 = sbt(eb, "F16", [128, 8, 16], F32); TF16 = T("F16")
            PS = sbt(eb, "PS", [128, 8, 16], U32); TPS = T("PS")
            au = sbt(eb, "au", [128, 2, 128], U32); Tau = T("au")
            apf = sbt(eb, "apf", [128, 2, 128], F32); Tapf = T("apf")
            sh = sbt(eb, "sh", [128, 128], F32); Tsh = T("sh")
            ex = sbt(eb, "ex", [128, 128], F32); Tex = T("ex")
            Zs = sbt(eb, "Zs", [128, 8], F32); TZs = T("Zs")
            rZ = sbt(eb, "rZ", [128, 8], F32); TrZ = T("rZ")
            IJG = sbt(eb, "IJG", [128, 3, 128], F32); TIJG = T("IJG")
            IJGT = [sbt(eb, "IJGT%d" % i, [128, 3, TB], F32) for i in range(2)]; TIJGT = [T("IJGT0"), T("IJGT1")]
            Qb = [sbt(eb, "Qb%d" % i, [128, NSB, 128], BF16) for i in range(2)]; TQb = [T("Qb0"), T("Qb1")]
            Pb = [sbt(eb, "Pb%d" % i, [128, NSB, 128], BF16) for i in range(2)]; TPb = [T("Pb0"), T("Pb1")]
            Gbuf = sbt(eb, "Gbuf", [128, NCH, TB], BF16); TG = T("Gbuf")
            UT = [sbt(eb, "UT%d" % i, [128, D], BF16) for i in range(NBUF)]; TUT = [T("UT%d" % i) for i in range(NBUF)]
            Vc = [sbt(eb, "Vc%d" % i, [128, D], BF16) for i in range(NBUF)]; TVc = [T("Vc%d" % i) for i in range(NBUF)]
            gl = [sbt(eb, "gl%d" % i, [128, TB], BF16) for i in range(2)]; Tgl = [T("gl0"), T("gl1")]
            Wb = [sbt(eb, "Wb%d" % i, [128, TB], BF16) for i in range(4)]; TW = [T("W%d" % i) for i in range(4)]
            opsb = [pst(eb, "ops%d" % i, [128, 512], F32) for i in range(4)]; Tops = [T("ops%d" % i) for i in range(4)]
            tpb = pst(eb, "tpsB", [128, 8, 128], BF16); Ttpb = T("tpsB")
            MB = [pst(eb, "MB%d" % i, [128, 512], F32) for i in range(3)]
            TMB = [T("MB0"), T("MB1"), T("MB2")]
            Apsb = [MB[1], MB[2]]; TAps = [TMB[1], TMB[2]]

            Twqb = T("wqb")
            for pc in range(2):
                K.dma("pool", lambda e: e.dma_start(out=wqb[:, pc * 1024:(pc + 1) * 1024], in_=w_q[:, pc * 1024:(pc + 1) * 1024]),
                      writes=[Twqb], group=True)
            K.dma("pool", lambda e: e.dma_start(out=keys_bf[:], in_=keysT), writes=[Tkeys])
            K.dma("sp", lambda e: e.dma_start(out=gffn[:], in_=gffn_d.partition_broadcast(128)), writes=[Tgffn])
            K.dma("sp", lambda e: e.dma_start(out=gfin[:], in_=gfin_d.partition_broadcast(128)), writes=[Tgfin])
            K.op("dve", lambda e: [e.tensor_copy(out=iob[:], in_=io128[:]), e.memset(ecst[:], float(np.e)),
                                   e.memset(nhalf[:], -0.5)], reads=[Tio], writes=[Tiob, Tecst, Tnhalf])
            wqb3 = wqb.rearrange("(k p) n -> p k n", p=128)

            uTb3 = uTb.rearrange("(c p) n -> c p n", p=128)
            vb3 = vb.rearrange("(c p) n -> c p n", p=128)
            NG = NBLK * NCH

            def load_chunk(gi):
                if gi >= NG:
                    return
                c = gi % NCH
                sl = gi % NBUF
                K.dma("sp", lambda e: e.dma_start(out=UT[sl][:], in_=uTb3[c]), reads=[Texp], writes=[TUT[sl]])
                K.dma("sp", lambda e: e.dma_start(out=Vc[sl][:], in_=vb3[c]), reads=[Texp], writes=[TVc[sl]])

            def rstd(ssx, Tssx, msx, Tmsx, rsx, Trsx):
                K.op("pool", lambda e: e.tensor_scalar(out=msx[:], in0=ssx[:], scalar1=1.0 / D, scalar2=EPS,
                                                       op0=ALU.mult, op1=ALU.add), reads=[Tssx], writes=[Tmsx])
                K.op("pool", lambda e: e.tensor_tensor(out=rsx[:], in0=msx[:], in1=nhalf[:], op=ALU.pow),
                     reads=[Tmsx, Tnhalf], writes=[Trsx])

            def load_wq(qp):
                K.dma("sp", lambda e: e.dma_start(out=wqr[qp % 2][:], in_=wqb3[:, :, qp * 256:(qp + 1) * 256]),
                      reads=[Twqb], writes=[Twqr[qp % 2]])

            def prefetch_p1(b):
                if b >= NBLK:
                    return
                t0_ = b * TB
                K.dma("sp", lambda e: e.dma_start(out=xp[:], in_=x1d[t0_:t0_ + TB, :].rearrange("(j p) d -> p j d", p=128)),
                      reads=[Tx1d[b]], writes=[Txp])
                load_wq(0)
                load_wq(1)

            def part1(b):
                pb = b % 2
                tok0 = b * TB
                H2T = h2T[pb]
                TH2T = Th2T[pb]
                if b == 0:
                    prefetch_p1(0)
                for j in range(2):
                    K.op("act", lambda e: e.activation(out=h2bf[:, j, :], in_=xp[:, j, :], func=AF.Square,
                                                       accum_out=ss1[:, j:j + 1]), reads=[Txp], writes=[Th2bf, Tss1])
                    yield 0.1
                rstd(ss1, Tss1, ms1, Tms1, rs1, Trs1)
                yield 0.1
                for j in range(2):
                    for hf in range(2):
                        cs = slice(hf * 512, (hf + 1) * 512)
                        K.op("dve", lambda e: e.scalar_tensor_tensor(out=h2bf[:, j, cs], in0=xp[:, j, cs], scalar=rs1[:, j:j + 1],
                                                                     in1=gffn[:, cs], op0=ALU.mult, op1=ALU.mult),
                             reads=[Txp, Trs1, Tgffn], writes=[Th2bf])
                        yield 0.6
                yield 3.0
                for j in range(2):
                    K.op("pe", lambda e: [e.transpose(out=tpb[:, k, :], in_=h2bf[:, j, k * 128:(k + 1) * 128],
                                                      identity=idb[:]) for k in range(8)],
                         reads=[Th2bf, Tidb], writes=[Ttpb])
                    K.op("act", lambda e: e.activation(out=H2T[:, :, j * 128:(j + 1) * 128], in_=tpb[:], func=AF.Copy),
                         writes=[TH2T, Ttpb])
                    yield 0.3
                for qp in range(8):
                    wr = wqr[qp % 2]
                    K.op("pe", lambda e: [e.matmul(MB[0][:, hh * 256:(hh + 1) * 256], lhsT=wr[:, k, hh * 128:(hh + 1) * 128],
                                                   rhs=H2T[:, k, :], start=(k == 0), stop=(k == 7))
                                          for hh in range(2) for k in range(8)],
                         reads=[Twqr[qp % 2], TH2T], writes=[TMB[0]])
                    K.op("act", lambda e: e.activation(out=qT[:, 2 * qp:2 * qp + 2, :],
                                                       in_=MB[0][:].rearrange("p (a t) -> p a t", a=2), func=AF.Copy),
                         writes=[TqT, TMB[0]])
                    if qp + 2 < 8:
                        load_wq(qp + 2)
                    yield 0.4
                prefetch_p1(b + 1)
                Sg = lambda g: S[:, g * 128:(g + 1) * 128]
                S2g = lambda g: S2[:, g * 128:(g + 1) * 128]
                Cg = lambda h: cand[:, h * 256:(h + 1) * 256]
                C2g = lambda h: S[:, h * 256:(h + 1) * 256]
                for j in range(2):
                    for pc in range(4):
                        K.op("pe", lambda e: [e.matmul(MB[0][:, i * 128:(i + 1) * 128], lhsT=qT[:, pc * 4 + i, j * 128:(j + 1) * 128],
                                                       rhs=keys_bf[:, pc * 4 + i, :], start=True, stop=True) for i in range(4)],
                             reads=[TqT, Tkeys], writes=[TMB[0]])
                        K.op("act", lambda e: e.activation(out=S[:, pc * 512:(pc + 1) * 512], in_=MB[0][:], func=AF.Copy),
                             writes=[TS, TMB[0]])
                        yield 0.4
                    for gq in range(8):
                        gs = range(2 * gq, 2 * gq + 2)
                        K.op("dve", lambda e: [e.max(out=V16[:, g, 0:8], in_=Sg(g)) for g in gs], reads=[TS], writes=[TV16])
                        yield 0.5
                        K.op("dve", lambda e: [e.max_index(out=IX[:, g, 0:8], in_max=V16[:, g, 0:8], in_values=Sg(g)) for g in gs] +
                             [e.match_replace(out=S2g(g), in_to_replace=V16[:, g, 0:8], in_values=Sg(g), imm_value=-1e30) for g in gs],
                             reads=[TS, TV16], writes=[TIX, TS2])
                        yield 1.0
                        K.op("dve", lambda e: [e.max(out=V16[:, g, 8:16], in_=S2g(g)) for g in gs], reads=[TS2], writes=[TV16])
                        yield 0.5
                        K.op("dve", lambda e: [e.max_index(out=IX[:, g, 8:16], in_max=V16[:, g, 8:16], in_values=S2g(g)) for g in gs],
                             reads=[TS2, TV16], writes=[TIX])
                        yield 0.5
                    V16r = V16[:].rearrange("p (h two) k -> p h two k", two=2)
                    cand4 = cand[:].rearrange("p (h a b) -> p h a b", h=8, a=16)
                    for hq in range(8):
                        hs = slice(hq, hq + 1)
                        K.op("dve", lambda e: e.tensor_tensor(out=cand4[:, hs], in0=V16r[:, hs, 0, :].unsqueeze(3).to_broadcast([128, 1, 16, 16]),
                                                              in1=V16r[:, hs, 1, :].unsqueeze(2).to_broadcast([128, 1, 16, 16]),
                                                              op=ALU.add), reads=[TV16], writes=[Tcand])
                        yield 0.55
                    for hq in range(8):
                        hs = range(hq, hq + 1)
                        K.op("dve", lambda e: [e.max(out=F16[:, h, 0:8], in_=Cg(h)) for h in hs], reads=[Tcand], writes=[TF16])
                        yield 0.5
                        K.op("dve", lambda e: [e.max_index(out=PS[:, h, 0:8], in_max=F16[:, h, 0:8], in_values=Cg(h)) for h in hs] +
                             [e.match_replace(out=C2g(h), in_to_replace=F16[:, h, 0:8], in_values=Cg(h), imm_value=-1e30) for h in hs],
                             reads=[Tcand, TF16], writes=[TPS, TS])
                        yield 1.0
                        K.op("dve", lambda e: [e.max(out=F16[:, h, 8:16], in_=C2g(h)) for h in hs], reads=[TS], writes=[TF16])
                        yield 0.5
                        K.op("dve", lambda e: [e.max_index(out=PS[:, h, 8:16], in_max=F16[:, h, 8:16], in_values=C2g(h)) for h in hs],
                             reads=[TS, TF16], writes=[TPS])
                        yield 0.5
                    sh3 = sh[:].rearrange("p (h k) -> p h k", k=16)
                    ex3 = ex[:].rearrange("p (h k) -> p h k", k=16)
                    K.op("dve", lambda e: e.tensor_tensor(out=sh3, in0=F16[:], in1=F16[:, :, 0:1].to_broadcast([128, 8, 16]),
                                                          op=ALU.subtract), reads=[TF16], writes=[Tsh])
                    yield 0.3
                    yield 1.5
                    yield 1.5
                    K.op("act", lambda e: e.activation(out=ex[:], in_=sh[:], func=AF.Tanh, scale=0.5), reads=[Tsh], writes=[Tex])
                    yield 0.1
                    K.op("dve", lambda e: e.tensor_scalar(out=sh[:], in0=ex[:], scalar1=-1.0, scalar2=1.0, op0=ALU.mult, op1=ALU.add),
                         reads=[Tex], writes=[Tsh])
                    yield 0.2
                    K.op("dve", lambda e: [e.reciprocal(out=sh[:], in_=sh[:])], reads=[Tsh], writes=[Tsh])
                    yield 0.2
                    K.op("dve", lambda e: e.scalar_tensor_tensor(out=ex[:], in0=ex[:], scalar=1.0, in1=sh[:], op0=ALU.add, op1=ALU.mult),
                         reads=[Tex, Tsh], writes=[Tex])
                    yield 0.2
                    PSf = PS[:].rearrange("p h k -> p (h k)")
                    K.op("dve", lambda e: [e.tensor_single_scalar(out=au[:, 0, :], in_=PSf, scalar=4, op=ALU.arith_shift_right),
                                           e.tensor_single_scalar(out=au[:, 1, :], in_=PSf, scalar=15, op=ALU.bitwise_and)],
                         reads=[TPS], writes=[Tau])
                    yield 0.3
                    K.op("dve", lambda e: [e.tensor_copy(out=apf[:], in_=au[:]), e.tensor_copy(out=IXf[:], in_=IX[:])],
                         reads=[Tau, TIX], writes=[Tapf, TIXf])
                    yield 0.5
                    IXr = IXf[:].rearrange("p (h two) k -> p h two k", two=2)
                    eqs = [S2[:].rearrange("p (h k a) -> p h k a", h=8, k=16), cand[:].rearrange("p (h k a) -> p h k a", h=8, k=16)]
                    Teqs = [TS2, Tcand]
                    for wh in range(2):
                        pos3 = apf[:, wh, :].rearrange("p (h k) -> p h k", k=16)
                        for hq in range(8):
                            hs = slice(hq, hq + 1)
                            K.op("dve", lambda e: e.tensor_tensor(
                                out=eqs[wh][:, hs], in0=pos3[:, hs].unsqueeze(3).to_broadcast([128, 1, 16, 16]),
                                in1=io128[:, 0:16].unsqueeze(1).unsqueeze(1).to_broadcast([128, 1, 16, 16]), op=ALU.is_equal),
                                 reads=[Tapf, Tio], writes=[Teqs[wh]])
                            yield 0.55
                        for hq in range(8):
                            hs = slice(hq, hq + 1)
                            K.op("dve", lambda e: e.tensor_tensor(out=eqs[wh][:, hs], in0=eqs[wh][:, hs],
                                                                  in1=IXr[:, hs, wh, :].unsqueeze(2).to_broadcast([128, 1, 16, 16]),
                                                                  op=ALU.mult), reads=[Teqs[wh], TIXf], writes=[Teqs[wh]])
                            yield 0.55
                    for wh in range(2):
                        for hq in range(4):
                            hs = slice(2 * hq, 2 * hq + 2)
                            K.op("dve", lambda e: e.reduce_sum(out=IJG[:, wh, :].rearrange("p (h k) -> p h k", k=16)[:, hs],
                                                               in_=eqs[wh][:, hs], axis=AX.X), reads=[Teqs[wh]], writes=[TIJG])
                            yield 0.55
                        if wh == 0:
                            K.op("dve", lambda e: e.reduce_sum(out=Zs[:], in_=ex3, axis=AX.X), reads=[Tex], writes=[TZs])
                            yield 0.2
                            K.op("dve", lambda e: e.reciprocal(out=rZ[:], in_=Zs[:]), reads=[TZs], writes=[TrZ])
                            yield 0.1
                            K.op("dve", lambda e: e.tensor_tensor(out=IJG[:, 2, :].rearrange("p (h k) -> p h k", k=16), in0=ex3,
                                                                  in1=rZ[:].unsqueeze(2).to_broadcast([128, 8, 16]), op=ALU.mult),
                                 reads=[Tex, TrZ], writes=[TIJG])
                            yield 0.1
                    yield 1.5
                    yield 1.5
                    K.op("pe", lambda e: [e.transpose(out=MB[0][:, w * 128:(w + 1) * 128], in_=IJG[:, w, :], identity=idf[:])
                                          for w in range(3)], reads=[TIJG, Tidf], writes=[TMB[0]])
                    K.op("act", lambda e: e.activation(out=IJGT[pb][:, :, j * 128:(j + 1) * 128],
                                                       in_=MB[0][:, 0:384].rearrange("p (w t) -> p w t", w=3),
                                                       func=AF.Copy), writes=[TIJGT[pb], TMB[0]])
                    yield 0.3

            def part2(b):
                pb = b % 2
                IT = IJGT[pb]
                for sbi in range(TB // NSB):
                    sl = sbi % 2
                    t0 = sbi * NSB
                    K.op("dve", lambda e: [e.tensor_scalar(out=Qb[sl][:, i, :], in0=iob[:], scalar1=IT[:, 1, t0 + i:t0 + i + 1],
                                                           scalar2=None, op0=ALU.is_equal) for i in range(NSB)] +
                         [e.tensor_scalar(out=Pb[sl][:, i, :], in0=iob[:], scalar1=IT[:, 0, t0 + i:t0 + i + 1],
                                          scalar2=IT[:, 2, t0 + i:t0 + i + 1], op0=ALU.is_equal, op1=ALU.mult)
                          for i in range(NSB)],
                         reads=[Tiob, TIJGT[pb]], writes=[TQb[sl], TPb[sl]])
                    for q4 in range(NSB // 4):
                        cnt = sbi * (NSB // 4) + q4
                        mi = cnt % 3
                        tt = t0 + q4 * 4
                        K.op("pe", lambda e: [e.matmul(MB[mi][:, i * 128:(i + 1) * 128], lhsT=Qb[sl][:, q4 * 4 + i, :],
                                                       rhs=Pb[sl][:, q4 * 4 + i, :], start=True, stop=True) for i in range(4)],
                             reads=[TQb[sl], TPb[sl]], writes=[TMB[mi]])
                        src = MB[mi][:].rearrange("p (t i) -> p i t", t=4)
                        K.op("act", lambda e: e.activation(out=Gbuf[:, :, tt:tt + 4], in_=src, func=AF.Copy),
                             writes=[TG, TMB[mi]])

            for gi in range(NBUF - 1):
                load_chunk(gi)
            for _ in part1(0):
                pass
            chk(51)

            def epi_adds(b):
                K.op("dve", lambda e: [e.tensor_tensor(out=xe[:, j, hf * 512:(hf + 1) * 512], in0=xe[:, j, hf * 512:(hf + 1) * 512],
                                                       in1=opsb[2 * j + hf][:], op=ALU.add)
                                       for j in range(2) for hf in range(2)], reads=[Txe], writes=[Txe] + Tops)

            def epi_rest(b):
                t0_ = b * TB
                for j in range(2):
                    K.op("act", lambda e: e.activation(out=h2bf[:, j, :], in_=xe[:, j, :], func=AF.Square,
                                                       accum_out=ss2[:, j:j + 1]), reads=[Txe], writes=[Th2bf, Tss2])
                    yield 0.1
                rstd(ss2, Tss2, ms2, Tms2, rs2, Trs2)
                yield 0.1
                for j in range(2):
                    for hf in range(2):
                        cs = slice(hf * 512, (hf + 1) * 512)
                        K.op("dve", lambda e: e.scalar_tensor_tensor(out=xe[:, j, cs], in0=xe[:, j, cs], scalar=rs2[:, j:j + 1],
                                                                     in1=gfin[:, cs], op0=ALU.mult, op1=ALU.mult),
                             reads=[Txe, Trs2, Tgfin], writes=[Txe])
                        yield 0.6
                K.dma("sp", lambda e: e.dma_start(out=y[t0_:t0_ + TB, :].rearrange("(j p) d -> p j d", p=128), in_=xe[:]),
                      reads=[Txe], sem_of=Tst[b % 2])
                yield 0.1

            def chain(*gens):
                for g in gens:
                    if g is not None:
                        for v in g:
                            yield v

            for b in range(NBLK):
                tok0 = b * TB
                pb = b % 2
                part2(b)
                chk(52)
                chk(60 + b)
                if b > 0:
                    epi_adds(b - 1)
                bg = chain(epi_rest(b - 1) if b > 0 else None, part1(b + 1) if b + 1 < NBLK else None)

                def emitA(c):
                    gi = b * NCH + c
                    sl = gi % NBUF
                    ai = c % 2
                    K.op("pe", lambda e: [e.matmul(Apsb[ai][:, 0:TB], lhsT=UT[sl][:, k * 128:(k + 1) * 128], rhs=h2T[pb][:, k, :],
                                                   start=(k == 0), stop=(k == 7)) for k in range(8)],
                         reads=[TUT[sl], Th2T[pb]], writes=[TAps[ai]])
                    K.op("act", lambda e: e.activation(out=gl[ai][:], in_=Apsb[ai][:, 0:TB], func=AF.Gelu),
                         writes=[Tgl[ai], TAps[ai]])
                    K.op("pool", lambda e: e.tensor_tensor(out=Wb[c % 4][:], in0=gl[ai][:], in1=Gbuf[:, c, :], op=ALU.mult),
                         reads=[Tgl[ai], TG], writes=[TW[c % 4]])

                def emitV(c):
                    gi = b * NCH + c
                    sl = gi % NBUF
                    K.op("pe", lambda e: [e.matmul(opsb[2 * j + hf][:], lhsT=Wb[c % 4][:, j * 128:(j + 1) * 128],
                                                   rhs=Vc[sl][:, hf * 512:(hf + 1) * 512], start=(c == 0), stop=(c == NCH - 1))
                                          for j in range(2) for hf in range(2)],
                         reads=[TW[c % 4], TVc[sl]], writes=Tops, acc=(Tops if c > 0 else ()))

                emitA(0)
                emitA(1)
                for c in range(NCH):
                    if c + 2 < NCH:
                        emitA(c + 2)
                    emitV(c)
                    load_chunk(b * NCH + c + NBUF - 1)
                    if c == NCH - 24:
                        K.dma("sp", lambda e: e.dma_start(out=xe[:], in_=x1d[tok0:tok0 + TB, :].rearrange("(j p) d -> p j d", p=128)),
                              reads=[Tx1d[b]], writes=[Txe])
                    if bg is not None and c >= 2:
                        budget = 1.45
                        while budget > 0:
                            try:
                                budget -= next(bg)
                            except StopIteration:
                                bg = None
                                break
                if bg is not None:
                    for _ in bg:
                        pass

                chk(53)
                chk(70 + b)
                chk(54)
            epi_adds(NBLK - 1)
            for _ in epi_rest(NBLK - 1):
                pass
            K.barrier()
    return nc


def _prep_weights(inputs):
    f = lambda a: np.ascontiguousarray(np.asarray(a, dtype=np.float32))
    L = 0
    eu = np.asarray(inputs["expert_u"], dtype=np.float32)[L]
    uT = np.ascontiguousarray(eu.reshape(NCH, 128, 8, 128).transpose(0, 3, 2, 1)).reshape(NEXP, D)
    sk = np.asarray(inputs["sub_keys"], dtype=np.float32)[L]
    keysT = np.ascontiguousarray(sk.reshape(16, 128, 128).transpose(2, 0, 1))
    gwt = np.ascontiguousarray(np.asarray(inputs["pool_group_w"], dtype=np.float32)[L].transpose(1, 0, 2))
    pscale = np.ascontiguousarray(np.asarray(inputs["pool_scale"], dtype=np.float32)[L].reshape(4, 128).T)
    cwt = np.ascontiguousarray(np.asarray(inputs["conv_w"], dtype=np.float32)[L].reshape(3, 4, 128).transpose(2, 1, 0))
    return dict(
        w_in=f(inputs["w_in"][L]), w_bp=f(inputs["w_branch_pool"][L]), w_bc=f(inputs["w_branch_conv"][L]),
        w_out=f(inputs["w_out"][L]), w_q=f(inputs["w_q"][L]), gw=gwt, pscale=pscale, cw=cwt,
        gmix=f(inputs["g_mix"][L]), gffn=f(inputs["g_ffn"][L]), gfin=f(inputs["g_final"]),
        keysT=keysT, uT=uT, v=f(np.asarray(inputs["expert_v"])[L]),
    )


def run(inputs, ncores, ntok, seq, phase='AB'):
    x = np.asarray(inputs["x"], dtype=np.float32)
    B, S_, _ = x.shape
    xf = np.ascontiguousarray(x.reshape(B * S_, D))
    assert B * S_ == ncores * ntok
    wts = _prep_weights(inputs)
    nc = build(ntok, seq, phase)
    in_maps = []
    for c in range(ncores):
        m = dict(wts)
        m["x"] = xf[c * ntok:(c + 1) * ntok]
        in_maps.append(m)
    res = run_bass_kernel_spmd(nc, in_maps, core_ids=list(range(ncores)))
    out = np.concatenate([np.asarray(r["y"]) for r in res.results], axis=0)
    return out.reshape(B, S_, D).astype(np.float32)


def kernel(**inputs):
    return run(inputs, 8, 8192, 4096)
```
